# Optimizing a Trainium2 kernel written in Bass

```python
import math
import jax, jax.numpy as jnp
from jax import lax
import numpy as np

D_MODEL = 1024
BATCH = 4
SEQ = 8192
DEPTH = 2

MEM_LEN = 256
DA_HEADS = 8
DA_HEAD_DIM = 64
DA_V_DIM = 2 * DA_HEAD_DIM
DA_WIDTH = DA_HEADS * DA_V_DIM
QK_WIDTH = DA_HEADS * 2 * DA_HEAD_DIM
LAMBDA_INIT_SCALE = 0.1
CONV_WIDTH = 512
CONV_K = 3
MEM_HEADS = 4
MEM_HEAD_DIM = 128
MEM_WIDTH = MEM_HEADS * MEM_HEAD_DIM
N_BRANCH = 3
ROPE_THETA = 10000.0
Q_BLOCK = 128
N_EXPERTS = 16
N_GROUPS = 4
EXPERTS_PER_GROUP = N_EXPERTS // N_GROUPS
TOP_K = 2
D_FF_EXPERT = 512
EXPERT_BLOCK = 128
LN_EPS = 1e-5
RMS_EPS = 1e-5
DEEPNORM_ALPHA = (2 * DEPTH) ** 0.25
DEEPNORM_BETA = (8 * DEPTH) ** -0.25

IN_SIZES = (QK_WIDTH, QK_WIDTH, DA_WIDTH, CONV_WIDTH, CONV_WIDTH, CONV_WIDTH, MEM_WIDTH, N_BRANCH * D_MODEL)
IN_COLS = sum(IN_SIZES)
IN_SPLITS = tuple(sum(IN_SIZES[:i + 1]) for i in range(len(IN_SIZES) - 1))

kernel_name = 'hybrid_diffattn_shortconv_memxattn_grouped_moe'


def layer_norm(x, g, b):
    x32 = x.astype(jnp.float32)
    mu = jnp.mean(x32, axis=-1, keepdims=True)
    var = jnp.mean(jnp.square(x32 - mu), axis=-1, keepdims=True)
    y = (x32 - mu) * lax.rsqrt(var + LN_EPS) * g.astype(jnp.float32) + b.astype(jnp.float32)
    return y.astype(x.dtype)


def rope(t, cos, sin):
    half = t.shape[-1] // 2
    t1, t2 = t[..., :half], t[..., half:]
    cos = cos.astype(t.dtype)
    sin = sin.astype(t.dtype)
    return jnp.concatenate([t1 * cos - t2 * sin, t2 * cos + t1 * sin], axis=-1)


def diff_attention(q, k, v, lam, lam_init, subln_g):
    B, S = q.shape[0], q.shape[1]
    qh = jnp.transpose(q, (0, 2, 3, 1, 4))
    kh = jnp.transpose(k, (0, 2, 3, 1, 4))
    vh = jnp.transpose(v, (0, 2, 1, 3))
    scale = DA_HEAD_DIM ** -0.5
    outs = []
    for blk in range(S // Q_BLOCK):
        s0 = blk * Q_BLOCK
        end = s0 + Q_BLOCK
        qb = qh[:, :, :, s0:end]
        kb = kh[:, :, :, :end]
        sc = jnp.einsum('bhcqd,bhckd->bhcqk', qb, kb).astype(jnp.float32) * scale
        causal = jnp.arange(end)[None, :] <= (s0 + jnp.arange(Q_BLOCK))[:, None]
        sc = jnp.where(causal, sc, -jnp.inf)
        p = jax.nn.softmax(sc, axis=-1)
        a = p[:, :, 0] - lam * p[:, :, 1]
        outs.append(jnp.einsum('bhqk,bhkd->bhqd', a.astype(v.dtype), vh[:, :, :end]))
    o = jnp.concatenate(outs, axis=2).astype(jnp.float32)
    o = o * lax.rsqrt(jnp.mean(jnp.square(o), axis=-1, keepdims=True) + RMS_EPS)
    o = o * subln_g.astype(jnp.float32) * (1.0 - lam_init)
    o = jnp.transpose(o, (0, 2, 1, 3)).astype(v.dtype)
    return o.reshape(B, S, DA_WIDTH)


def short_gated_conv(bg, cg, u, w):
    z = cg * u
    S = z.shape[1]
    zp = jnp.pad(z, ((0, 0), (CONV_K - 1, 0), (0, 0)))
    y = w[0] * zp[:, 0:S]
    for tap in range(1, CONV_K):
        y = y + w[tap] * zp[:, tap:tap + S]
    return bg * y


def memory_attention(qm, mem, w_kv):
    B, S = qm.shape[0], qm.shape[1]
    M = mem.shape[1]
    kv = mem @ w_kv
    km = kv[..., :MEM_WIDTH].reshape(B, M, MEM_HEADS, MEM_HEAD_DIM)
    vm = kv[..., MEM_WIDTH:].reshape(B, M, MEM_HEADS, MEM_HEAD_DIM)
    q = qm.reshape(B, S, MEM_HEADS, MEM_HEAD_DIM)
    sc = jnp.einsum('bshd,bmhd->bhsm', q, km).astype(jnp.float32) * (MEM_HEAD_DIM ** -0.5)
    p = jax.nn.softmax(sc, axis=-1)
    o = jnp.einsum('bhsm,bmhd->bshd', p.astype(vm.dtype), vm)
    return o.reshape(B, S, MEM_WIDTH)


def grouped_moe(x2d, w_router, router_bias, w_gate, w_up, w_down):
    T, D = x2d.shape
    logits = (x2d @ w_router).astype(jnp.float32) + router_bias.astype(jnp.float32)
    scores = jax.nn.softmax(logits, axis=-1)
    grouped = scores.reshape(T, N_GROUPS, EXPERTS_PER_GROUP)
    g_idx = jnp.argmax(jnp.max(grouped, axis=-1), axis=-1)
    in_group = jnp.take_along_axis(grouped, g_idx[:, None, None], axis=1)[:, 0]
    top_vals, top_idx = lax.top_k(in_group, TOP_K)
    experts = g_idx[:, None] * EXPERTS_PER_GROUP + top_idx
    weights = top_vals / jnp.sum(top_vals, axis=-1, keepdims=True)

    A = T * TOP_K
    e_flat = experts.reshape(A).astype(jnp.int32)
    tok_flat = jnp.repeat(jnp.arange(T, dtype=jnp.int32), TOP_K)
    gate_flat = weights.reshape(A).astype(x2d.dtype)
    order = jnp.argsort(e_flat)
    e_sorted = e_flat[order]
    counts = jnp.zeros((N_EXPERTS,), jnp.int32).at[e_flat].add(1)
    start = jnp.cumsum(counts) - counts
    padded = ((counts + EXPERT_BLOCK - 1) // EXPERT_BLOCK) * EXPERT_BLOCK
    pad_end = jnp.cumsum(padded)
    pad_start = pad_end - padded
    dest = pad_start[e_sorted] + (jnp.arange(A, dtype=jnp.int32) - start[e_sorted])
    P = ((A + EXPERT_BLOCK - 1) // EXPERT_BLOCK) * EXPERT_BLOCK + N_EXPERTS * EXPERT_BLOCK
    n_blocks = P // EXPERT_BLOCK
    buf_tok = jnp.full((P,), T, jnp.int32).at[dest].set(tok_flat[order])
    buf_gate = jnp.zeros((P,), x2d.dtype).at[dest].set(gate_flat[order])
    blk_start = jnp.arange(n_blocks, dtype=jnp.int32) * EXPERT_BLOCK
    blk_expert = jnp.minimum(jnp.searchsorted(pad_end, blk_start, side='right'), N_EXPERTS - 1).astype(jnp.int32)
    x_pad = jnp.concatenate([x2d, jnp.zeros((1, D), x2d.dtype)], axis=0)
    xb = x_pad[buf_tok].reshape(n_blocks, EXPERT_BLOCK, D)

    def expert_block(args):
        xblk, e = args
        h = jax.nn.silu(xblk @ w_gate[e]) * (xblk @ w_up[e])
        return h @ w_down[e]

    yb = lax.map(expert_block, (xb, blk_expert))
    y = yb.reshape(P, D) * buf_gate[:, None]
    return jax.ops.segment_sum(y, buf_tok, num_segments=T + 1)[:T]


def setup_inputs(seed: int = 0) -> dict:
    key = jax.random.key(seed)
    ks = jax.random.split(key, 26)
    f32 = jnp.float32

    def nrm(k, shape, fan_in, scale=1.0):
        return jax.random.normal(k, shape, f32) * (scale * fan_in ** -0.5)

    x = jax.random.normal(ks[0], (BATCH, SEQ, D_MODEL), f32)
    mem = jax.random.normal(ks[1], (BATCH, MEM_LEN, D_MODEL), f32)
    positions = (jax.random.randint(ks[2], (BATCH, 1), 0, 4096, jnp.int32)
                 + jnp.arange(SEQ, dtype=jnp.int32)[None, :])
    col_scale = jnp.concatenate([
        jnp.ones((2 * QK_WIDTH,), f32),
        jnp.full((DA_WIDTH,), DEEPNORM_BETA, f32),
        jnp.ones((IN_COLS - 2 * QK_WIDTH - DA_WIDTH,), f32)])
    w_in = nrm(ks[3], (DEPTH, D_MODEL, IN_COLS), D_MODEL) * col_scale
    lambda_q1 = LAMBDA_INIT_SCALE * jax.random.normal(ks[4], (DEPTH, DA_HEAD_DIM), f32)
    lambda_k1 = LAMBDA_INIT_SCALE * jax.random.normal(ks[5], (DEPTH, DA_HEAD_DIM), f32)
    lambda_q2 = LAMBDA_INIT_SCALE * jax.random.normal(ks[6], (DEPTH, DA_HEAD_DIM), f32)
    lambda_k2 = LAMBDA_INIT_SCALE * jax.random.normal(ks[7], (DEPTH, DA_HEAD_DIM), f32)
    diff_subln_g = 1.0 + 0.02 * jax.random.normal(ks[8], (DEPTH, DA_V_DIM), f32)
    conv_w = nrm(ks[9], (DEPTH, CONV_K, CONV_WIDTH), CONV_K)
    w_mem_kv = nrm(ks[10], (DEPTH, D_MODEL, 2 * MEM_WIDTH), D_MODEL)
    w_br_attn = nrm(ks[11], (DEPTH, DA_WIDTH, D_MODEL), DA_WIDTH, DEEPNORM_BETA)
    w_br_conv = nrm(ks[12], (DEPTH, CONV_WIDTH, D_MODEL), CONV_WIDTH, DEEPNORM_BETA)
    w_br_mem = nrm(ks[13], (DEPTH, MEM_WIDTH, D_MODEL), MEM_WIDTH, DEEPNORM_BETA)
    w_out = nrm(ks[14], (DEPTH, D_MODEL, D_MODEL), D_MODEL, DEEPNORM_BETA)
    ln1_g = 1.0 + 0.02 * jax.random.normal(ks[15], (DEPTH, D_MODEL), f32)
    ln1_b = 0.02 * jax.random.normal(ks[16], (DEPTH, D_MODEL), f32)
    ln2_g = 1.0 + 0.02 * jax.random.normal(ks[17], (DEPTH, D_MODEL), f32)
    ln2_b = 0.02 * jax.random.normal(ks[18], (DEPTH, D_MODEL), f32)
    w_router = nrm(ks[19], (D_MODEL, N_EXPERTS), D_MODEL)
    router_bias = 0.01 * jax.random.normal(ks[20], (N_EXPERTS,), f32)
    w_exp_gate = nrm(ks[21], (DEPTH, N_EXPERTS, D_MODEL, D_FF_EXPERT), D_MODEL, DEEPNORM_BETA)
    w_exp_up = nrm(ks[22], (DEPTH, N_EXPERTS, D_MODEL, D_FF_EXPERT), D_MODEL, DEEPNORM_BETA)
    w_exp_down = nrm(ks[23], (DEPTH, N_EXPERTS, D_FF_EXPERT, D_MODEL), D_FF_EXPERT, DEEPNORM_BETA)
    return {'x': x, 'mem': mem, 'positions': positions, 'w_in': w_in,
            'lambda_q1': lambda_q1, 'lambda_k1': lambda_k1, 'lambda_q2': lambda_q2, 'lambda_k2': lambda_k2,
            'diff_subln_g': diff_subln_g, 'conv_w': conv_w, 'w_mem_kv': w_mem_kv,
            'w_br_attn': w_br_attn, 'w_br_conv': w_br_conv, 'w_br_mem': w_br_mem, 'w_out': w_out,
            'ln1_g': ln1_g, 'ln1_b': ln1_b, 'ln2_g': ln2_g, 'ln2_b': ln2_b,
            'w_router': w_router, 'router_bias': router_bias,
            'w_exp_gate': w_exp_gate, 'w_exp_up': w_exp_up, 'w_exp_down': w_exp_down}


def reference(x, mem, positions, w_in, lambda_q1, lambda_k1, lambda_q2, lambda_k2, diff_subln_g,
              conv_w, w_mem_kv, w_br_attn, w_br_conv, w_br_mem, w_out,
              ln1_g, ln1_b, ln2_g, ln2_b, w_router, router_bias,
              w_exp_gate, w_exp_up, w_exp_down):
    B, S, D = x.shape
    inv_freq = ROPE_THETA ** (-jnp.arange(0, DA_HEAD_DIM, 2, dtype=jnp.float32) / DA_HEAD_DIM)
    ang = positions.astype(jnp.float32)[..., None] * inv_freq
    cos = jnp.cos(ang)[:, :, None, :]
    sin = jnp.sin(ang)[:, :, None, :]

    for l in range(DEPTH):
        lam_init = 0.8 - 0.6 * math.exp(-0.3 * l)
        proj = x @ w_in[l]
        q, k, v, cb, cc, cu, qm, gates = jnp.split(proj, IN_SIZES and IN_SPLITS, axis=-1)

        q = rope(q.reshape(B, S, DA_HEADS * 2, DA_HEAD_DIM), cos, sin).reshape(B, S, DA_HEADS, 2, DA_HEAD_DIM)
        k = rope(k.reshape(B, S, DA_HEADS * 2, DA_HEAD_DIM), cos, sin).reshape(B, S, DA_HEADS, 2, DA_HEAD_DIM)
        v = v.reshape(B, S, DA_HEADS, DA_V_DIM)
        lam = (jnp.exp(jnp.sum(lambda_q1[l].astype(jnp.float32) * lambda_k1[l].astype(jnp.float32)))
               - jnp.exp(jnp.sum(lambda_q2[l].astype(jnp.float32) * lambda_k2[l].astype(jnp.float32)))
               + lam_init)
        y_attn = diff_attention(q, k, v, lam, lam_init, diff_subln_g[l])

        y_conv = short_gated_conv(cb, cc, cu, conv_w[l])

        y_mem = memory_attention(qm, mem, w_mem_kv[l])

        g = jax.nn.sigmoid(gates.reshape(B, S, N_BRANCH, D))
        merged = (g[:, :, 0] * (y_attn @ w_br_attn[l])
                  + g[:, :, 1] * (y_conv @ w_br_conv[l])
                  + g[:, :, 2] * (y_mem @ w_br_mem[l]))
        mix = merged @ w_out[l]
        x = layer_norm(DEEPNORM_ALPHA * x + mix, ln1_g[l], ln1_b[l])

        f = grouped_moe(x.reshape(B * S, D), w_router, router_bias,
                        w_exp_gate[l], w_exp_up[l], w_exp_down[l]).reshape(B, S, D)
        x = layer_norm(DEEPNORM_ALPHA * x + f, ln2_g[l], ln2_b[l])
    return x
```

```python
import math
import os
import numpy as np
import ml_dtypes
import concourse.bass as bass
import concourse.mybir as mybir
from concourse.bass_utils import run_bass_kernel_spmd

F32 = mybir.dt.float32
BF16 = mybir.dt.bfloat16
I32 = mybir.dt.int32
AF = mybir.ActivationFunctionType
ALU = mybir.AluOpType
AX = mybir.AxisListType

ENGS = ['pe', 'act', 'dve', 'pool', 'sp']
EPOCH = 30000
NDSEM = 8


class Buf:
    __slots__ = ('name', 'writers', 'readers')

    def __init__(self, name=''):
        self.name = name
        self.writers = []
        self.readers = []


class Op:
    __slots__ = ('eng', 'fn', 'deps', 'seq', 'signal', 'sig_idx', 'dma', 'dma_n', 'cc')

    def __init__(self, eng, fn, dma, cc=None):
        self.eng = eng
        self.fn = fn
        self.dma = dma
        self.cc = cc
        self.deps = ()
        self.signal = False
        self.sig_idx = -1
        self.dma_n = -1


class Prog:
    def __init__(self, nc):
        self.nc = nc
        self.ops = {e: [] for e in ENGS}
        self.last = {e: None for e in ENGS}
        self.ndma = {e: 0 for e in ENGS}
        self.dmas_open = []
        self.nops = 0
        self.ncc = 0

    def add(self, eng, fn, reads=(), writes=(), partial=(), dma=False, cc=False):
        op = Op(eng, fn, dma or cc)
        if cc:
            op.cc = self.ncc
            self.ncc += 1
        deps = set()
        for b in reads:
            deps.update(b.writers)
        for b in writes:
            deps.update(b.readers)
            deps.update(b.writers)
        for b in partial:
            if b.readers:
                deps.update(b.readers)
                deps.update(b.writers)
        for b in reads:
            b.readers.append(op)
        for b in writes:
            b.writers = [op]
            b.readers = []
        for b in partial:
            if b.readers:
                b.writers = [op]
                b.readers = []
            else:
                b.writers.append(op)
        self._finish(op, deps, False)
        return op

    def _finish(self, op, deps, is_barrier):
        eng = op.eng
        op.seq = len(self.ops[eng])
        best = {}
        dl = []
        for d in deps:
            if d.dma:
                dl.append(d)
            else:
                if d.eng == eng and eng == 'pe':
                    continue
                c = best.get(d.eng)
                if c is None or d.seq > c.seq:
                    best[d.eng] = d
        for d in best.values():
            d.signal = True
            dl.append(d)
        op.deps = dl
        if not is_barrier:
            if op.dma:
                if op.cc is None:
                    op.dma_n = self.ndma[eng]
                    self.ndma[eng] += 1
                self.dmas_open.append(op)
            else:
                self.last[eng] = op
        self.ops[eng].append(op)
        self.nops += 1

    def barrier(self):
        deps = set(self.dmas_open)
        for e in ENGS:
            if self.last[e] is not None:
                deps.add(self.last[e])
        for e in ENGS:
            op = Op(e, None, False)
            self._finish(op, set(deps), True)
        self.dmas_open = []

    def emit(self):
        nc = self.nc
        nsig = {}
        for e in ENGS:
            k = 0
            for op in self.ops[e]:
                if op.dma or op.fn is None:
                    continue
                if op.signal:
                    op.sig_idx = k
                    k += 1
            nsig[e] = k
        csems = {}
        for e in ENGS:
            n_ep = (nsig[e] + EPOCH - 1) // EPOCH
            csems[e] = [nc.alloc_semaphore(name=f"c_{e}_{i}") for i in range(max(n_ep, 1))]
        dsems = {}
        for e in ENGS:
            if self.ndma[e] > 0:
                dsems[e] = [nc.alloc_semaphore(name=f"d_{e}_{i}") for i in range(NDSEM)]
        ccsems = [nc.alloc_semaphore(name=f"cc_{i}") for i in range(self.ncc)]
        engobj = {'pe': 'tensor', 'act': 'scalar', 'dve': 'vector', 'pool': 'gpsimd', 'sp': 'sync'}

        def semval(d):
            if d.cc is not None:
                return ccsems[d.cc], 1
            if d.dma:
                return dsems[d.eng][d.dma_n % NDSEM], 16 * (d.dma_n // NDSEM + 1)
            return csems[d.eng][d.sig_idx // EPOCH], (d.sig_idx % EPOCH) + 1

        def run(e, eng):
            known = {}
            for op in self.ops[e]:
                waits = []
                if op.dma and op.cc is None and op.dma_n >= NDSEM:
                    s = dsems[e][op.dma_n % NDSEM]
                    waits.append((s, 16 * (op.dma_n // NDSEM)))
                for d in op.deps:
                    waits.append(semval(d))
                for (s, v) in waits:
                    key = s.num
                    if known.get(key, 0) >= v:
                        continue
                    known[key] = v
                    eng.wait_ge(s, v)
                if op.fn is None:
                    continue
                ins = op.fn(eng)
                if op.cc is not None:
                    ins.then_inc(ccsems[op.cc], 1)
                elif op.dma:
                    ins.then_inc(dsems[e][op.dma_n % NDSEM], 16)
                elif op.signal:
                    ins.then_inc(csems[e][op.sig_idx // EPOCH], 1)

        with nc.Block() as block:
            for e in ENGS:
                if not self.ops[e]:
                    continue
                deco = getattr(block, engobj[e])

                def mk(e):
                    def f(eng):
                        run(e, eng)
                    return f
                deco(mk(e))


D = 1024
SEQ = 8192
T = 4096
NB = T // 128
NT = T // 512
DEPTH = 2
ALPHA = (2 * DEPTH) ** 0.25
LN_EPS = 1e-5
RMS_EPS = 1e-5
NEG = -30000.0
COL_Q, COL_K, COL_V, COL_CB, COL_CC, COL_CU, COL_QM, COL_G = 0, 1024, 2048, 3072, 3584, 4096, 4608, 5120
SB_BASE = 16640
SB_LIMIT = 229376 - 512


class Tile:
    __slots__ = ('t', 'b')

    def __init__(self, t, b):
        self.t = t
        self.b = b


class Builder:
    def __init__(self, nc, dbg=False):
        self.nc = nc
        self.P = Prog(nc)
        self.dbg = dbg
        self.pers_off = SB_BASE
        self.ph_off = SB_BASE
        self.ph_base = SB_BASE
        self.cnt = 0

    def _alloc(self, shape, dtype, off):
        isz = 2 if dtype == BF16 else 4
        n = 1
        for s in shape[1:]:
            n *= s
        size = (n * isz + 63) // 64 * 64
        self.cnt += 1
        t = self.nc.alloc_sbuf_tensor_at(f"t{self.cnt}", list(shape), dtype, offset=off)
        return t, size

    def pers(self, shape, dtype, name=''):
        assert self.ph_off == self.ph_base, "persistent alloc only before phases"
        t, size = self._alloc(shape, dtype, self.pers_off)
        self.pers_off += size
        self.ph_base = self.ph_off = self.pers_off
        assert self.pers_off <= SB_LIMIT
        return Tile(t, Buf(name))

    def tile(self, shape, dtype, name=''):
        t, size = self._alloc(shape, dtype, self.ph_off)
        self.ph_off += size
        assert self.ph_off <= SB_LIMIT, (name, self.ph_off)
        return Tile(t, Buf(name))

    def phase_end(self):
        self.P.barrier()
        self.ph_off = self.ph_base

    def mark(self):
        return self.ph_off

    def release(self, m):
        self.P.barrier()
        self.ph_off = m

    def red(self, out, in_, R, W=(), Wp=()):
        self.P.add('dve', lambda e: e.reduce_sum(out=out, in_=in_, axis=AX.X), reads=R, writes=W, partial=Wp)

    def rmax(self, out, in_, R, W=(), Wp=()):
        self.P.add('dve', lambda e: e.tensor_reduce(out=out, in_=in_, axis=AX.X, op=ALU.max), reads=R, writes=W, partial=Wp)

    def recip(self, out, in_, R, W=(), Wp=()):
        self.P.add('dve', lambda e: e.reciprocal(out=out, in_=in_), reads=R, writes=W, partial=Wp)

    def cc(self, src, dst, R, W):
        import os
        if os.environ.get("NOCC"):
            return
        self.P.add('pool', lambda e: e.collective_compute(
            "AllGather", ALU.bypass, replica_groups=[[2 * g, 2 * g + 1] for g in range(int(os.environ.get("NCO", "8")) // 2)],
            ins=[src], outs=[dst]), reads=R, writes=W, cc=True)

    def mm(self, out, lhsT, rhs, start, stop, R, Wp):
        self.P.add('pe', lambda e: e.matmul(out, lhsT=lhsT, rhs=rhs, start=start, stop=stop), reads=R, partial=[Wp])

    def tr(self, out, in_, ident, R, Wp):
        self.P.add('pe', lambda e: e.transpose(out=out, in_=in_, identity=ident), reads=R, partial=[Wp])

    def act(self, out, in_, func, R, W=(), Wp=(), bias=0.0, scale=1.0, accum=None):
        if accum is None:
            self.P.add('act', lambda e: e.activation(out=out, in_=in_, func=func, bias=bias, scale=scale), reads=R, writes=W, partial=Wp)
        else:
            self.P.add('act', lambda e: e.activation(out=out, in_=in_, func=func, bias=bias, scale=scale, accum_out=accum), reads=R, writes=W, partial=Wp)

    def cp(self, eng, out, in_, R, W=(), Wp=()):
        if eng == 'act':
            self.P.add('act', lambda e: e.copy(out=out, in_=in_), reads=R, writes=W, partial=Wp)
        else:
            self.P.add(eng, lambda e: e.tensor_copy(out=out, in_=in_), reads=R, writes=W, partial=Wp)

    def tt(self, eng, out, a, b, op, R, W=(), Wp=()):
        self.P.add(eng, lambda e: e.tensor_tensor(out=out, in0=a, in1=b, op=op), reads=R, writes=W, partial=Wp)

    def ts(self, eng, out, a, s1, s2, op0, op1, R, W=(), Wp=()):
        if s2 is None:
            self.P.add(eng, lambda e: e.tensor_scalar(out=out, in0=a, scalar1=s1, scalar2=None, op0=op0), reads=R, writes=W, partial=Wp)
        else:
            self.P.add(eng, lambda e: e.tensor_scalar(out=out, in0=a, scalar1=s1, scalar2=s2, op0=op0, op1=op1), reads=R, writes=W, partial=Wp)

    def stt(self, eng, out, in0, scalar, in1, op0, op1, R, W=(), Wp=()):
        self.P.add(eng, lambda e: e.scalar_tensor_tensor(out=out, in0=in0, scalar=scalar, in1=in1, op0=op0, op1=op1), reads=R, writes=W, partial=Wp)

    def dma(self, q, out, in_, R=(), W=(), Wp=(), slow=False):
        if slow:
            self.P.add(q, lambda e: e.dma_start(out=out, in_=in_, allow_slow_non_contiguous=True), reads=R, writes=W, partial=Wp, dma=True)
        else:
            self.P.add(q, lambda e: e.dma_start(out=out, in_=in_), reads=R, writes=W, partial=Wp, dma=True)

    def memset(self, eng, ap, val, W=(), Wp=()):
        self.P.add(eng, lambda e: e.memset(ap, val), writes=W, partial=Wp)


def bcast_rows(ap2d_row, nparts):
    a = ap2d_row
    n = a.shape[-1]
    return bass.AP(a.tensor, a.offset, [[0, nparts], [1, n]])


def build_program(dbg=False, stages=99, nlayers=DEPTH):
    nc = bass.Bass("TRN2", target_bir_lowering=False)
    B = Builder(nc, dbg)
    P = B.P
    skind = "ExternalOutput" if dbg else "Internal"

    def din(name, shape, dt):
        return nc.dram_tensor(name, list(shape), dt, kind="ExternalInput").ap()

    def dscr(name, shape, dt, force_internal=False):
        k = "Internal" if force_internal else skind
        if isinstance(dbg, (set, list, tuple)):
            k = "ExternalOutput" if (name in dbg and not force_internal) else "Internal"
        return nc.dram_tensor(name, list(shape), dt, kind=k).ap()

    x_in = din("x", [T, D], F32)
    pos_in = din("pos", [1, T], I32)
    mem_in = din("mem", [256, D], F32)
    cc_in = din("cc", [128, 16], F32)
    cbf_in = din("cbf", [128, 5, 128], BF16)
    sel_in = din("sel", [16, 16 * 128], BF16)
    invf_in = din("invf", [128, 1], F32)
    w_in = din("w_in", [DEPTH, D, 8192], F32)
    lq1 = din("lambda_q1", [DEPTH, 64], F32)
    lk1 = din("lambda_k1", [DEPTH, 64], F32)
    lq2 = din("lambda_q2", [DEPTH, 64], F32)
    lk2 = din("lambda_k2", [DEPTH, 64], F32)
    subg = din("diff_subln_g", [DEPTH, 128], F32)
    convw = din("conv_w", [DEPTH, 3, 512], F32)
    wkv = din("w_mem_kv", [DEPTH, D, 1024], F32)
    if stages > 3:
        wba = din("w_br_attn", [DEPTH, 1024, D], F32)
        wbc = din("w_br_conv", [DEPTH, 512, D], F32)
        wbm = din("w_br_mem", [DEPTH, 512, D], F32)
        wout = din("w_out", [DEPTH, D, D], F32)
        ln1g = din("ln1_g", [DEPTH, D], F32)
        ln1b = din("ln1_b", [DEPTH, D], F32)
        ln2g = din("ln2_g", [DEPTH, D], F32)
        ln2b = din("ln2_b", [DEPTH, D], F32)
    wr_in = din("w_router", [D, 16], F32)
    rb_in = din("router_bias", [1, 16], F32)
    if stages > 4:
        weg = din("w_exp_gate", [DEPTH, 16, D, 512], F32)
        weu = din("w_exp_up", [DEPTH, 16, D, 512], F32)
        wed = din("w_exp_down", [DEPTH, 16, 512, D], F32)
    y_out = nc.dram_tensor("y", [T, D], F32, kind="ExternalOutput").ap()

    exinK = [dscr(f"exinK{i}_", [1024, 1024], BF16, True) for i in range(4)]
    exinV = [dscr(f"exinV{i}_", [1024, 1024], BF16, True) for i in range(4)]
    exoutK = [dscr(f"exoutK{i}_", [2048, 1024], BF16, True) for i in range(4)]
    exoutV = [dscr(f"exoutV{i}_", [2048, 1024], BF16, True) for i in range(4)]
    exinT = dscr("exinT_", [128, 16], BF16, True)
    if dbg:
        dK = [dscr(f"exinK{i}", [1024, 1024], BF16) for i in range(4)]
        dV = [dscr(f"exinV{i}", [1024, 1024], BF16) for i in range(4)]
        dT = dscr("exinT", [128, 16], BF16)
        doK = [dscr(f"exoutK{i}", [2048, 1024], BF16) for i in range(4)]
        doV = [dscr(f"exoutV{i}", [2048, 1024], BF16) for i in range(4)]
    exoutT = dscr("exoutT", [256, 16], BF16, True)
    COS = dscr("COS", [128, T], F32, True)
    SIN = dscr("SIN", [128, T], F32, True)
    QT = dscr("QT", [8, 128, T], BF16)
    Gs = dscr("Gs", [24, 128, T], BF16)
    YA = dscr("YA", [8, 128, T], BF16)
    YC = dscr("YC", [4, 128, T], BF16)
    YM = dscr("YM", [4, 128, T], BF16)
    X1 = dscr("X1", [T, D], F32)
    X1T = dscr("X1T", [8, 128, T], BF16)
    XRES = dscr("XRES", [T, D], F32)
    WTD = dscr("WTD", [16, T], BF16)
    bK = [Buf(f"exinK{i}") for i in range(4)]
    bV = [Buf(f"exinV{i}") for i in range(4)]
    boK = [Buf() for i in range(4)]
    boV = [Buf() for i in range(4)]
    bT, boT = Buf(), Buf()
    bCOS, bSIN = Buf(), Buf()
    bQT = [Buf() for i in range(8)]
    bG = [Buf() for i in range(24)]
    bYA = [Buf() for i in range(8)]
    bYC = [Buf() for i in range(4)]
    bYM = [Buf() for i in range(4)]
    bX1 = [Buf() for i in range(NT)]
    bX1T = [Buf() for i in range(NT)]
    bXR = [Buf() for i in range(NT)]
    bWTD = Buf()

    ps = nc.alloc_psum_tensor("ps", [128, 8, 512], F32)
    pb = [Buf(f"ps{i}") for i in range(8)]
    psT = ps[:, 7, :].bitcast(BF16)
    psT3 = psT.rearrange("p (k t) -> p k t", k=8)

    cbf = B.pers([128, 5, 128], BF16, "cbf")
    ident = cbf.t[:, 0, :]
    Rm = cbf.t[:, 1, :]
    tri2 = B.pers([128, 2, 128], BF16, "tri2")
    ones = cbf.t[:, 3, :]
    sel = B.pers([16, 16 * 128], BF16, "sel")
    ccs = B.pers([128, 16], F32, "ccs")
    invf = B.pers([128, 1], F32, "invf")
    wT = B.pers([16, T], BF16, "wT")
    lamt = B.pers([128, 8], F32, "lam")
    gsc = B.pers([128, 128], F32, "gsc")
    cw = B.pers([128, 3, 4], F32, "cw")
    rbias = B.pers([128, 16], F32, "rbias")
    wrt = B.pers([128, 8, 16], BF16, "wrt")
    KmT = B.pers([128, 4, 256], BF16, "KmT")
    Vm = B.pers([128, 2, 512], BF16, "Vm")

    B.dma('sp', cbf.t[:], cbf_in, W=[cbf.b])
    B.dma('sp', tri2.t[:, 0, :], cbf_in[:, 2, :], Wp=[tri2.b])
    B.dma('sp', tri2.t[:, 1, :], cbf_in[:, 2, :], Wp=[tri2.b])
    B.dma('sp', sel.t[:], sel_in, W=[sel.b])
    B.dma('sp', ccs.t[:], cc_in, W=[ccs.b])
    B.dma('sp', invf.t[:], invf_in, W=[invf.b])
    B.dma('sp', rbias.t[:], bcast_rows(rb_in, 128), W=[rbias.b])
    B.dma('pool', wrt.t[:], wr_in.rearrange("(kc p) n -> p kc n", p=128), W=[wrt.b])

    posi = B.tile([128, T], I32, "posi")
    ang = B.tile([128, T], F32, "ang")
    tmp = B.tile([128, T], F32, "tmp")
    tab = B.tile([128, T], F32, "tab")
    B.dma('sp', posi.t[:], bcast_rows(pos_in, 128), W=[posi.b])
    B.cp('dve', ang.t[:], posi.t[:], R=[posi.b], W=[ang.b])
    B.ts('dve', ang.t[:], ang.t[:], invf.t[:, 0:1], 1.0 / (2 * math.pi), ALU.mult, ALU.mult, R=[ang.b, invf.b], W=[ang.b])
    for (dst, bdst, off) in ((SIN, bSIN, 0.0), (COS, bCOS, 0.25)):
        B.ts('dve', tmp.t[:], ang.t[:], off, None, ALU.add, None, R=[ang.b], W=[tmp.b])
        B.cp('dve', posi.t[:], tmp.t[:], R=[tmp.b], W=[posi.b])
        B.cp('dve', tab.t[:], posi.t[:], R=[posi.b], W=[tab.b])
        B.tt('dve', tmp.t[:], tmp.t[:], tab.t[:], ALU.subtract, R=[tmp.b, tab.b], W=[tmp.b])
        B.act(tab.t[:], tmp.t[:], AF.Sin, R=[tmp.b], W=[tab.b], scale=6.28318)
        B.dma('sp', dst, tab.t[:], R=[tab.b], W=[bdst])
    B.phase_end()
    if stages <= 0.1:
        nlayers = 0

    for l in range(nlayers):
        lam_init = 0.8 - 0.6 * math.exp(-0.3 * l)
        x_src = x_in if l == 0 else XRES
        bxs = [Buf() for _ in range(NT)] if l == 0 else bXR
        y_dst = y_out if l == nlayers - 1 else XRES
        by = [Buf() for _ in range(NT)] if l == nlayers - 1 else bXR

        lt = B.tile([128, 4, 64], F32, "lt")
        for i, a in enumerate((lq1, lk1, lq2, lk2)):
            B.dma('sp', lt.t[:, i, :], bcast_rows(a[l:l + 1, :], 128), Wp=[lt.b])
        lp = B.tile([128, 2, 64], F32, "lp")
        lsum = B.tile([128, 4], F32, "lsum")
        B.tt('dve', lp.t[:, 0, :], lt.t[:, 0, :], lt.t[:, 1, :], ALU.mult, R=[lt.b], Wp=[lp.b])
        B.tt('dve', lp.t[:, 1, :], lt.t[:, 2, :], lt.t[:, 3, :], ALU.mult, R=[lt.b], Wp=[lp.b])
        B.red(lsum.t[:, 0:2], lp.t[:], R=[lp.b], W=[lsum.b])
        B.act(lsum.t[:, 2:4], lsum.t[:, 0:2], AF.Exp, R=[lsum.b], W=[lsum.b])
        B.tt('dve', lamt.t[:, 0:1], lsum.t[:, 3:4], lsum.t[:, 2:3], ALU.subtract, R=[lsum.b], W=[lamt.b])
        B.ts('dve', lamt.t[:, 0:1], lamt.t[:, 0:1], -lam_init, None, ALU.add, None, R=[lamt.b], W=[lamt.b])
        B.dma('sp', gsc.t[:], bcast_rows(subg[l:l + 1, :], 128), W=[gsc.b])
        B.ts('dve', gsc.t[:], gsc.t[:], 1.0 - lam_init, None, ALU.mult, None, R=[gsc.b], W=[gsc.b])
        for tap in range(3):
            B.dma('sp', cw.t[:, tap, :], convw[l, tap, :].rearrange("(c p) -> p c", p=128), Wp=[cw.b], slow=True)

        memT = B.tile([128, 8, 256], BF16, "memT")
        wkvs = B.tile([128, 8, 1024], BF16, "wkvs")
        B.dma('pool', wkvs.t[:], wkv[l].rearrange("(kc p) n -> p kc n", p=128), W=[wkvs.b])
        mf = B.tile([128, D], F32, "mf")
        mb = B.tile([128, D], BF16, "mb")
        for mc in range(2):
            B.dma('sp', mf.t[:], mem_in[mc * 128:(mc + 1) * 128, :], W=[mf.b])
            B.cp('dve', mb.t[:], mf.t[:], R=[mf.b], W=[mb.b])
            for kc in range(8):
                B.tr(psT[:, kc * 128:(kc + 1) * 128], mb.t[:, kc * 128:(kc + 1) * 128], ident, R=[mb.b, cbf.b], Wp=pb[7])
            B.cp('dve', memT.t[:, :, mc * 128:(mc + 1) * 128], psT3, R=[pb[7]], Wp=[memT.b])
        for hd in range(4):
            for kc in range(8):
                B.mm(ps[:, 0, 0:256], wkvs.t[:, kc, hd * 128:(hd + 1) * 128], memT.t[:, kc, :], kc == 0, kc == 7, R=[wkvs.b, memT.b], Wp=pb[0])
            B.cp('dve', KmT.t[:, hd, :], ps[:, 0, 0:256], R=[pb[0]], Wp=[KmT.b])
        for mc in range(2):
            for kc in range(8):
                B.mm(ps[:, 1, :], memT.t[:, kc, mc * 128:(mc + 1) * 128], wkvs.t[:, kc, 512:1024], kc == 0, kc == 7, R=[wkvs.b, memT.b], Wp=pb[1])
            B.cp('dve', Vm.t[:, mc, :], ps[:, 1, :], R=[pb[1]], Wp=[Vm.b])
        B.phase_end()
        if stages <= 0.2:
            continue

        xT = B.tile([128, 8, T], BF16, "xT")
        bxT = [Buf() for _ in range(NT)]
        m0 = B.mark()
        xf = [B.tile([128, D], F32, "xf") for _ in range(2)]
        xb = [B.tile([128, D], BF16, "xb") for _ in range(2)]
        for blk in range(NB):
            i = blk % 2
            B.dma('sp', xf[i].t[:], x_src[blk * 128:(blk + 1) * 128, :], R=[bxs[blk // 4]], W=[xf[i].b])
            B.cp('act', xb[i].t[:], xf[i].t[:], R=[xf[i].b], W=[xb[i].b])
            for kc in range(8):
                B.tr(psT[:, kc * 128:(kc + 1) * 128], xb[i].t[:, kc * 128:(kc + 1) * 128], ident, R=[xb[i].b, cbf.b], Wp=pb[7])
            B.cp('dve', xT.t[:, :, blk * 128:(blk + 1) * 128], psT3, R=[pb[7]], Wp=[bxT[blk // 4]])
        B.release(m0)
        if stages <= 0.3:
            B.phase_end()
            continue

        NSLAB = 4
        slabs = [B.tile([128, 8, 512], BF16, f"slab{i}") for i in range(NSLAB)]
        plan = [COL_K, COL_K + 512, COL_V, COL_V + 512, COL_CC, COL_CU, COL_Q, COL_Q + 512, COL_QM] + \
               [COL_G + i * 512 for i in range(6)] + [COL_CB, COL_CC, COL_CU]
        if stages <= 1:
            plan = plan[:6]
        st_ = {'issued': 0, 'taken': 0}

        def issue_slab():
            n = st_['issued']
            if n >= len(plan):
                return
            s = slabs[n % NSLAB]
            c0 = plan[n]
            B.dma('pool', s.t[:], w_in[l, :, c0:c0 + 512].rearrange("(kc p) n -> p kc n", p=128), W=[s.b])
            st_['issued'] += 1

        def next_slab(c0):
            n = st_['taken']
            assert plan[n] == c0, (plan[n], c0)
            while st_['issued'] <= n + 1:
                if st_['issued'] >= len(plan):
                    break
                issue_slab()
            st_['taken'] += 1
            return slabs[n % NSLAB]

        pctr = [0]

        def proj_fm(s, j, t):
            bi = pctr[0] % 4
            pctr[0] += 1
            for kc in range(8):
                B.mm(ps[:, bi, :], s.t[:, kc, j * 128:(j + 1) * 128], xT.t[:, kc, t * 512:(t + 1) * 512], kc == 0, kc == 7,
                     R=[s.b, bxT[t]], Wp=pb[bi])
            return bi

        hbuf = [B.tile([128, T], BF16, "hbuf") for _ in range(2)]
        hctr = [0]

        def next_hb():
            hb = hbuf[hctr[0] % 2]
            hctr[0] += 1
            return hb

        ma_ = B.mark()
        cosT = B.tile([128, T], F32, "cos")
        sinT = B.tile([128, T], F32, "sin")
        B.dma('sp', cosT.t[:], COS, R=[bCOS], W=[cosT.b])
        B.dma('sp', sinT.t[:], SIN, R=[bSIN], W=[sinT.b])
        kb_ = [B.tile([128, 512], BF16, "kb") for _ in range(2)]
        t1_ = [B.tile([128, 512], F32, "t1") for _ in range(2)]
        t2_ = [B.tile([128, 512], F32, "t2") for _ in range(2)]
        rctr = [0]

        import os
        RH = int(os.environ.get("RH", "9"))

        def rope_head(s, j, hb):
            for t in range(NT):
                bi = proj_fm(s, j, t)
                r = rctr[0] % 2
                rctr[0] += 1
                if RH < 2:
                    continue
                B.cp('act', kb_[r].t[:], ps[:, bi, :], R=[pb[bi]], W=[kb_[r].b])
                B.mm(ps[:, 4 + r, :], Rm, kb_[r].t[:], True, True, R=[kb_[r].b, cbf.b], Wp=pb[4 + r])
                if RH < 3:
                    continue
                B.tt('dve', t1_[r].t[:], ps[:, bi, :], cosT.t[:, t * 512:(t + 1) * 512], ALU.mult, R=[pb[bi], cosT.b, kb_[r].b], W=[t1_[r].b])
                B.tt('dve', t2_[r].t[:], ps[:, 4 + r, :], sinT.t[:, t * 512:(t + 1) * 512], ALU.mult, R=[pb[4 + r], sinT.b], W=[t2_[r].b])
                B.tt('dve', hb.t[:, t * 512:(t + 1) * 512], t1_[r].t[:], t2_[r].t[:], ALU.add, R=[t1_[r].b, t2_[r].b], Wp=[hb.b])

        for sl in range(2):
            s = next_slab(COL_K + sl * 512)
            for j in range(4):
                hd = sl * 4 + j
                hb = next_hb()
                rope_head(s, j, hb)
                i, hh = hd // 2, hd % 2
                if RH >= 4:
                    B.dma('sp', exinK[i].rearrange("(h p a) c -> h p (a c)", h=2, a=4)[hh], hb.t[:], R=[hb.b], Wp=[bK[i]])
                if hh == 1:
                    B.cc(exinK[i], exoutK[i], R=[bK[i]], W=[boK[i]])
        if stages <= 0.5:
            B.phase_end()
            continue
        vt = [B.tile([128, 512], BF16, "vt") for _ in range(2)]
        vctr = 0
        for half in range(2):
            s = next_slab(COL_V + half * 512)
            for blk in range(NB):
                bi = pctr[0] % 4
                pctr[0] += 1
                for kc in range(8):
                    B.mm(ps[:, bi, :], xT.t[:, kc, blk * 128:(blk + 1) * 128], s.t[:, kc, :], kc == 0, kc == 7, R=[s.b, bxT[blk // 4]], Wp=pb[bi])
                v = vt[vctr % 2]
                vctr += 1
                B.cp('act', v.t[:], ps[:, bi, :], R=[pb[bi]], W=[v.b])
                for pr in range(2):
                    i = half * 2 + pr
                    dst = exinV[i].rearrange("(h tt) (tr d) -> h (tt tr) d", h=2, tr=8)[:, blk * 128:(blk + 1) * 128, :].rearrange("h t d -> t h d")
                    B.dma('sp', dst, v.t[:, pr * 256:(pr + 1) * 256].rearrange("p (h d) -> p h d", h=2), R=[v.b], Wp=[bV[i]])
            for pr in range(2):
                i = half * 2 + pr
                B.cc(exinV[i], exoutV[i], R=[bV[i]], W=[boV[i]])
        if stages <= 0.7:
            B.phase_end()
            continue
        s_cc = next_slab(COL_CC)
        s_cu = next_slab(COL_CU)
        tl = B.tile([128, 2, 4, 4], F32, "tl")
        tlb = B.tile([128, 4, 4], BF16, "tlb")
        xtail = xT.t[:, :, :].rearrange("p k (c t) -> p k c t", c=2)
        for which, s in enumerate((s_cc, s_cu)):
            for chc in range(4):
                c0 = (which * 4 + chc) * 4
                for kc in range(8):
                    B.mm(ps[:, 6, c0:c0 + 4].rearrange("p (c t) -> p c t", c=2),
                         s.t[:, kc, chc * 128:(chc + 1) * 128], xtail[:, kc, :, 2046:2048], kc == 0, kc == 7,
                         R=[s.b, bxT[3], bxT[7]], Wp=pb[6])
        B.cp('dve', tl.t[:].rearrange("p a b c -> p (a b c)"), ps[:, 6, 0:32], R=[pb[6]], W=[tl.b])
        B.tt('dve', tlb.t[:], tl.t[:, 0, :, :], tl.t[:, 1, :, :], ALU.mult, R=[tl.b], W=[tlb.b])
        B.dma('sp', exinT, tlb.t[:].rearrange("p a b -> p (a b)"), R=[tlb.b], W=[bT])
        B.cc(exinT, exoutT, R=[bT], W=[boT])
        if dbg:
            for i in range(4):
                B.dma('sp', dK[i], exinK[i], R=[bK[i]])
                B.dma('sp', dV[i], exinV[i], R=[bV[i]])
            B.dma('sp', dT, exinT, R=[bT])
            for i in range(4):
                B.dma('sp', doK[i], exoutK[i], R=[boK[i]])
                B.dma('sp', doV[i], exoutV[i], R=[boV[i]])
        if stages <= 1:
            B.phase_end()
            continue

        for sl in range(2):
            s = next_slab(COL_Q + sl * 512)
            for j in range(4):
                hd = sl * 4 + j
                hb = next_hb()
                rope_head(s, j, hb)
                B.dma('sp', QT[hd], hb.t[:], R=[hb.b], W=[bQT[hd]])
        B.release(ma_)

        s = next_slab(COL_QM)
        qmb = [B.tile([128, 512], BF16, "qmb") for _ in range(2)]
        pmt = [B.tile([128, 2, 512], BF16, "pmt") for _ in range(2)]
        rl = [B.tile([128, 512], F32, "rl") for _ in range(2)]
        mctr = 0
        for mh in range(4):
            hb = next_hb()
            for t in range(NT):
                bi = proj_fm(s, mh, t)
                r = mctr % 2
                mctr += 1
                B.cp('dve', qmb[r].t[:], ps[:, bi, :], R=[pb[bi]], W=[qmb[r].b])
                for mc in range(2):
                    B.mm(ps[:, 4 + mc, :], KmT.t[:, mh, mc * 128:(mc + 1) * 128], qmb[r].t[:], True, True, R=[KmT.b, qmb[r].b], Wp=pb[4 + mc])
                    B.act(pmt[r].t[:, mc, :], ps[:, 4 + mc, :], AF.Exp, R=[pb[4 + mc]], Wp=[pmt[r].b], scale=128 ** -0.5)
                for mc in range(2):
                    B.mm(ps[:, 6, :], Vm.t[:, mc, mh * 128:(mh + 1) * 128], pmt[r].t[:, mc, :], mc == 0, mc == 1, R=[Vm.b, pmt[r].b], Wp=pb[6])
                bl = pctr[0] % 4
                pctr[0] += 1
                for mc in range(2):
                    B.mm(ps[:, bl, :], ones, pmt[r].t[:, mc, :], mc == 0, mc == 1, R=[cbf.b, pmt[r].b], Wp=pb[bl])
                B.recip(rl[r].t[:], ps[:, bl, :], R=[pb[bl]], W=[rl[r].b])
                B.tt('dve', hb.t[:, t * 512:(t + 1) * 512], ps[:, 6, :], rl[r].t[:], ALU.mult, R=[pb[6], rl[r].b], Wp=[hb.b])
            B.dma('sp', YM[mh], hb.t[:], R=[hb.b], W=[bYM[mh]])
        B.release(ma_)

        for sl in range(6):
            s = next_slab(COL_G + sl * 512)
            for j in range(4):
                gi = sl * 4 + j
                hb = next_hb()
                for t in range(NT):
                    bi = proj_fm(s, j, t)
                    B.act(hb.t[:, t * 512:(t + 1) * 512], ps[:, bi, :], AF.Sigmoid, R=[pb[bi]], Wp=[hb.b])
                B.dma('sp', Gs[gi], hb.t[:], R=[hb.b], W=[bG[gi]])

        s_cb = next_slab(COL_CB)
        s_cc = next_slab(COL_CC)
        s_cu = next_slab(COL_CU)
        tg = B.tile([128, 2, 16], BF16, "tg")
        tgf = B.tile([128, 2, 16], F32, "tgf")
        for r in range(2):
            B.dma('sp', tg.t[:, r, :], exoutT[r * 128:(r + 1) * 128, :], R=[boT], Wp=[tg.b])
        B.cp('dve', tgf.t[:], tg.t[:], R=[tg.b], W=[tgf.b])
        halo = B.tile([128, 2, 4, 2], F32, "halo")
        for c in range(2):
            for k in range(4):
                r_, c_ = k // 2, k % 2
                src = tgf.t[:, r_, :].rearrange("p (a c t) -> p a c t", a=4, c=2)[:, :, c_, :]
                col = 4 + c * 4 + k
                if k == 0:
                    B.ts('dve', halo.t[:, c, :, :], src, ccs.t[:, col:col + 1], None, ALU.mult, None, R=[tgf.b, ccs.b], W=[halo.b] if c == 0 else (), Wp=[halo.b] if c == 1 else ())
                else:
                    B.stt('dve', halo.t[:, c, :, :], src, ccs.t[:, col:col + 1], halo.t[:, c, :, :], ALU.mult, ALU.add,
                          R=[tgf.b, ccs.b, halo.b], W=[halo.b])
        zrow = B.tile([128, 2, 2050], F32, "zrow")
        ccs_ = [B.tile([128, 512], F32, "ccsb") for _ in range(2)]
        yv = [B.tile([128, 512], F32, "yv") for _ in range(2)]
        cctr = 0
        for chc in range(4):
            hb = next_hb()
            for c in range(2):
                B.cp('dve', zrow.t[:, c, 0:2], halo.t[:, c, chc, :], R=[halo.b], Wp=[zrow.b])
            for t in range(NT):
                c, tt_ = t // 4, t % 4
                bc = proj_fm(s_cc, chc, t)
                r = cctr % 2
                cctr += 1
                B.cp('act', ccs_[r].t[:], ps[:, bc, :], R=[pb[bc]], W=[ccs_[r].b])
                bu = proj_fm(s_cu, chc, t)
                B.tt('dve', zrow.t[:, c, 2 + tt_ * 512:2 + (tt_ + 1) * 512], ps[:, bu, :], ccs_[r].t[:], ALU.mult, R=[pb[bu], ccs_[r].b], Wp=[zrow.b])
            for t in range(NT):
                c, tt_ = t // 4, t % 4
                r = cctr % 2
                cctr += 1
                bb = proj_fm(s_cb, chc, t)
                B.ts('dve', yv[r].t[:], zrow.t[:, c, tt_ * 512:tt_ * 512 + 512], cw.t[:, 0, chc:chc + 1], None, ALU.mult, None, R=[zrow.b, cw.b], W=[yv[r].b])
                B.stt('dve', yv[r].t[:], zrow.t[:, c, tt_ * 512 + 1:tt_ * 512 + 513], cw.t[:, 1, chc:chc + 1], yv[r].t[:], ALU.mult, ALU.add, R=[zrow.b, cw.b, yv[r].b], W=[yv[r].b])
                B.stt('dve', yv[r].t[:], zrow.t[:, c, tt_ * 512 + 2:tt_ * 512 + 514], cw.t[:, 2, chc:chc + 1], yv[r].t[:], ALU.mult, ALU.add, R=[zrow.b, cw.b, yv[r].b], W=[yv[r].b])
                B.tt('dve', hb.t[:, t * 512:(t + 1) * 512], ps[:, bb, :], yv[r].t[:], ALU.mult, R=[pb[bb], yv[r].b], Wp=[hb.b])
            B.dma('sp', YC[chc], hb.t[:], R=[hb.b], W=[bYC[chc]])
        B.phase_end()
        if stages <= 2:
            continue

        NBUF = 2
        Kg = [B.tile([128, 3 * 2048], BF16, "Kg") for _ in range(NBUF)]
        Ko = [B.tile([128, T], BF16, "Ko") for _ in range(NBUF)]
        Vg = [B.tile([128, 48, 129], BF16, "Vg") for _ in range(NBUF)]
        Vo = [B.tile([128, 32, 129], BF16, "Vo") for _ in range(NBUF)]
        Qh = [B.tile([128, T], BF16, "Qh") for _ in range(NBUF)]
        yaT = [B.tile([128, T], BF16, "yaT") for _ in range(2)]
        for i in range(NBUF):
            B.memset('dve', Vg[i].t[:, :, 128:129], 1.0, Wp=[Vg[i].b])
            B.memset('dve', Vo[i].t[:, :, 128:129], 1.0, Wp=[Vo[i].b])
        ptile = [B.tile([128, 2, 2, 256], BF16, "pt") for _ in range(3)]
        osb = [B.tile([128, 128], F32, "osb") for _ in range(2)]
        osq = [B.tile([128, 128], F32, "osq") for _ in range(2)]
        ybf = [B.tile([128, 128], BF16, "ybf") for _ in range(2)]
        sm = [B.tile([128, 8], F32, "sm") for _ in range(2)]
        slots = [(0, 0), (1, 0), (1, 1)]

        def load_head(hd):
            i, hh = hd // 2, hd % 2
            bfi = hd % NBUF
            kg, ko, vg, vo, qh = Kg[bfi], Ko[bfi], Vg[bfi], Vo[bfi], Qh[bfi]
            gK = exoutK[i].rearrange("(r h p a) c -> r h p (a c)", r=2, h=2, a=4)
            gV = exoutV[i].rearrange("(r h tt) (tr d) -> r h (tt tr) d", r=2, h=2, tr=8)
            for si, (rk, ch) in enumerate(slots):
                B.dma('sp', kg.t[:, si * 2048:(si + 1) * 2048], gK[rk, hh, :, ch * 2048:(ch + 1) * 2048], R=[boK[i]], Wp=[kg.b])
                B.dma('sp', vg.t[:, si * 16:(si + 1) * 16, 0:128],
                      gV[rk, hh, ch * 2048:(ch + 1) * 2048, :].rearrange("(n p) d -> p n d", p=128), R=[boV[i]], Wp=[vg.b])
            B.dma('sp', ko.t[:], exinK[i].rearrange("(h p a) c -> h p (a c)", h=2, a=4)[hh], R=[bK[i]], W=[ko.b])
            B.dma('sp', vo.t[:, :, 0:128], exinV[i].rearrange("(h tt) (tr d) -> h (tt tr) d", h=2, tr=8)[hh].rearrange("(n p) d -> p n d", p=128),
                  R=[bV[i]], Wp=[vo.b])
            B.dma('sp', qh.t[:], QT[hd], R=[bQT[hd]], W=[qh.b])

        stepctr = 0
        cmbctr = 0
        load_head(0)
        for hd in range(8):
            if hd + 1 < 8:
                load_head(hd + 1)
            bfi = hd % NBUF
            kg, ko, vg, vo, qh = Kg[bfi], Ko[bfi], Vg[bfi], Vo[bfi], Qh[bfi]
            ya = yaT[hd % 2]
            for c in range(2):
                pref = [(0, 0)] if c == 0 else [(0, 1), (1, 2), (2, 3)]
                for qt in range(8):
                    q0 = c * 2048 + qt * 256
                    steps = []
                    for (si, bcol) in pref:
                        for kb in range(16):
                            steps.append((kg.t[:, si * 2048 + kb * 128: si * 2048 + (kb + 1) * 128], vg.t[:, si * 16 + kb, :], bcol, None, kg.b, vg.b))
                    for kb in range(2 * qt + 2):
                        mk = None
                        if kb == 2 * qt:
                            mk = 0
                        elif kb == 2 * qt + 1:
                            mk = 1
                        steps.append((ko.t[:, c * 2048 + kb * 128: c * 2048 + (kb + 1) * 128], vo.t[:, c * 16 + kb, :], None, mk, ko.b, vo.b))
                    nsteps = len(steps)
                    assert nsteps % 2 == 0
                    for comp in range(2):
                        B.mm(ps[:, 4 + comp, 0:258], cbf.t[:, 4, :], cbf.t[:, 0:3, :].rearrange("p a b -> p (a b)")[:, 0:258], True, False,
                             R=[cbf.b], Wp=pb[4 + comp])
                    for pidx in range(nsteps // 2):
                        pc = stepctr
                        stepctr += 1
                        pt = ptile[pc % 3]
                        bA = 2 * (pc % 2)
                        bcol = steps[2 * pidx][2]
                        assert steps[2 * pidx + 1][2] == bcol
                        for j in range(2):
                            kap, vap, _, mk, kbuf, vbuf = steps[2 * pidx + j]
                            for comp in range(2):
                                B.mm(ps[:, bA + comp, j * 256:(j + 1) * 256], kap[comp * 64:(comp + 1) * 64, :],
                                     qh.t[comp * 64:(comp + 1) * 64, q0:q0 + 256], True, True, R=[kbuf, qh.b], Wp=pb[bA + comp])
                        bias = 0.0 if bcol is None else ccs.t[:, bcol:bcol + 1]
                        for comp in range(2):
                            B.act(pt.t[:, comp, :, :].rearrange("p j q -> p (j q)"), ps[:, bA + comp, :], AF.Exp, R=[pb[bA + comp], ccs.b],
                                  Wp=[pt.b], bias=bias, scale=0.125)
                        for j in range(2):
                            mk = steps[2 * pidx + j][3]
                            if mk is not None:
                                B.tt('dve', pt.t[:, :, j, mk * 128:(mk + 1) * 128], pt.t[:, :, j, mk * 128:(mk + 1) * 128], tri2.t[:], ALU.mult,
                                     R=[pt.b, tri2.b], W=[pt.b])
                        for j in range(2):
                            kap, vap, _, mk, kbuf, vbuf = steps[2 * pidx + j]
                            sidx = 2 * pidx + j
                            for comp in range(2):
                                for sub in range(2):
                                    if mk == 1 and sub == 0:
                                        continue
                                    last = (sidx == nsteps - 1) if sub == 1 else (sidx == nsteps - 2)
                                    B.mm(ps[:, 4 + comp, sub * 129:(sub + 1) * 129], pt.t[:, comp, j, sub * 128:(sub + 1) * 128], vap, False, last,
                                         R=[pt.b, vbuf], Wp=pb[4 + comp])
                    for sub in range(2):
                        k = cmbctr % 2
                        cmbctr += 1
                        o1 = ps[:, 4, sub * 129:(sub + 1) * 129]
                        o2 = ps[:, 5, sub * 129:(sub + 1) * 129]
                        b1, b2 = pb[4], pb[5]
                        s_ = sm[k]
                        B.recip(s_.t[:, 0:1], o1[:, 128:129], R=[b1], W=[s_.b])
                        B.recip(s_.t[:, 1:2], o2[:, 128:129], R=[b2, s_.b], W=[s_.b])
                        B.tt('dve', s_.t[:, 2:3], s_.t[:, 1:2], lamt.t[:, 0:1], ALU.mult, R=[s_.b, lamt.b], W=[s_.b])
                        B.ts('dve', osb[k].t[:], o1[:, 0:128], s_.t[:, 0:1], None, ALU.mult, None, R=[b1, s_.b], W=[osb[k].b])
                        B.stt('dve', osb[k].t[:], o2[:, 0:128], s_.t[:, 2:3], osb[k].t[:], ALU.mult, ALU.add, R=[b2, s_.b, osb[k].b], W=[osb[k].b])
                        B.tt('dve', osq[k].t[:], osb[k].t[:], osb[k].t[:], ALU.mult, R=[osb[k].b], W=[osq[k].b])
                        B.red(s_.t[:, 3:4], osq[k].t[:], R=[osq[k].b, s_.b], W=[s_.b])
                        B.ts('dve', s_.t[:, 4:5], s_.t[:, 3:4], 1.0 / 128, RMS_EPS, ALU.mult, ALU.add, R=[s_.b], W=[s_.b])
                        B.act(s_.t[:, 6:7], s_.t[:, 4:5], AF.Ln, R=[s_.b], W=[s_.b])
                        B.act(s_.t[:, 5:6], s_.t[:, 6:7], AF.Exp, R=[s_.b], W=[s_.b], scale=-0.5)
                        B.stt('dve', ybf[k].t[:], osb[k].t[:], s_.t[:, 5:6], gsc.t[:], ALU.mult, ALU.mult, R=[osb[k].b, s_.b, gsc.b], W=[ybf[k].b])
                        B.tr(psT[:, k * 128:(k + 1) * 128], ybf[k].t[:], ident, R=[ybf[k].b, cbf.b], Wp=pb[7])
                        B.cp('dve', ya.t[:, q0 + sub * 128:q0 + (sub + 1) * 128], psT[:, k * 128:(k + 1) * 128], R=[pb[7]], Wp=[ya.b])
            B.dma('sp', YA[hd], ya.t[:], R=[ya.b], W=[bYA[hd]])
        B.phase_end()
        if stages <= 3:
            continue

        wba_s = B.tile([128, 8, D], BF16, "wba")
        wbc_s = B.tile([128, 4, D], BF16, "wbc")
        wbm_s = B.tile([128, 4, D], BF16, "wbm")
        wo_s = B.tile([128, 8, D], BF16, "wo")
        B.dma('pool', wba_s.t[:], wba[l].rearrange("(kc p) n -> p kc n", p=128), W=[wba_s.b])
        B.dma('pool', wbc_s.t[:], wbc[l].rearrange("(kc p) n -> p kc n", p=128), W=[wbc_s.b])
        B.dma('pool', wbm_s.t[:], wbm[l].rearrange("(kc p) n -> p kc n", p=128), W=[wbm_s.b])
        B.dma('pool', wo_s.t[:], wout[l].rearrange("(kc p) n -> p kc n", p=128), W=[wo_s.b])
        lng = B.tile([128, D], F32, "lng")
        lnb = B.tile([128, D], F32, "lnb")
        B.dma('sp', lng.t[:], bcast_rows(ln1g[l:l + 1, :], 128), W=[lng.b])
        B.dma('sp', lnb.t[:], bcast_rows(ln1b[l:l + 1, :], 128), W=[lnb.b])
        yat = [B.tile([128, 8, 512], BF16, "yat") for _ in range(2)]
        yct = [B.tile([128, 4, 512], BF16, "yct") for _ in range(2)]
        ymt = [B.tile([128, 4, 512], BF16, "ymt") for _ in range(2)]
        gt = [B.tile([128, 3, 512], BF16, "gt") for _ in range(2)]
        mT = [B.tile([128, 8, 512], BF16, "mT") for _ in range(2)]
        ma = [B.tile([128, 512], F32, "ma") for _ in range(2)]
        mb2 = [B.tile([128, 512], F32, "mb2") for _ in range(2)]
        xr = [B.tile([128, D], F32, "xr") for _ in range(2)]
        rr = [B.tile([128, D], F32, "rr") for _ in range(2)]
        x1b = [B.tile([128, D], BF16, "x1b") for _ in range(2)]
        x1T = [B.tile([128, 8, 512], BF16, "x1T") for _ in range(2)]
        st = [B.tile([128, 16], F32, "st") for _ in range(2)]
        rt = [B.tile([128, 16, 8], F32, "rt") for _ in range(2)]
        wtb = [B.tile([128, 16], BF16, "wtb") for _ in range(2)]

        def layer_norm(r_, g_, b_, s_):
            P.add('dve', lambda e: e.bn_stats(out=s_.t[:, 0:6], in_=r_.t[:, 0:512]), reads=[r_.b], writes=[s_.b])
            P.add('dve', lambda e: e.bn_stats(out=s_.t[:, 6:12], in_=r_.t[:, 512:1024]), reads=[r_.b, s_.b], writes=[s_.b])
            P.add('dve', lambda e: e.bn_aggr(out=s_.t[:, 12:14], in_=s_.t[:, 0:12]), reads=[s_.b], writes=[s_.b])
            B.ts('dve', s_.t[:, 14:15], s_.t[:, 13:14], LN_EPS, None, ALU.add, None, R=[s_.b], W=[s_.b])
            B.act(s_.t[:, 15:16], s_.t[:, 14:15], AF.Ln, R=[s_.b], W=[s_.b])
            B.act(s_.t[:, 14:15], s_.t[:, 15:16], AF.Exp, R=[s_.b], W=[s_.b], scale=-0.5)
            B.ts('dve', r_.t[:], r_.t[:], s_.t[:, 12:13], s_.t[:, 14:15], ALU.subtract, ALU.mult, R=[r_.b, s_.b], W=[r_.b])
            B.tt('dve', r_.t[:], r_.t[:], g_.t[:], ALU.mult, R=[r_.b, g_.b], W=[r_.b])
            B.tt('dve', r_.t[:], r_.t[:], b_.t[:], ALU.add, R=[r_.b, b_.b], W=[r_.b])

        gctr2 = 0
        for t in range(NT):
            k = t % 2
            for hd in range(8):
                B.dma('sp', yat[k].t[:, hd, :], YA[hd, :, t * 512:(t + 1) * 512], R=[bYA[hd]], Wp=[yat[k].b])
            for c4 in range(4):
                B.dma('sp', yct[k].t[:, c4, :], YC[c4, :, t * 512:(t + 1) * 512], R=[bYC[c4]], Wp=[yct[k].b])
                B.dma('sp', ymt[k].t[:, c4, :], YM[c4, :, t * 512:(t + 1) * 512], R=[bYM[c4]], Wp=[ymt[k].b])
            for j in range(8):
                jj = j % 2
                g_ = gt[gctr2 % 2]
                gctr2 += 1
                for br in range(3):
                    B.dma('sp', g_.t[:, br, :], Gs[br * 8 + j, :, t * 512:(t + 1) * 512], R=[bG[br * 8 + j]], Wp=[g_.b])
                for kc in range(8):
                    B.mm(ps[:, 0, :], wba_s.t[:, kc, j * 128:(j + 1) * 128], yat[k].t[:, kc, :], kc == 0, kc == 7, R=[wba_s.b, yat[k].b], Wp=pb[0])
                for kc in range(4):
                    B.mm(ps[:, 1, :], wbc_s.t[:, kc, j * 128:(j + 1) * 128], yct[k].t[:, kc, :], kc == 0, kc == 3, R=[wbc_s.b, yct[k].b], Wp=pb[1])
                for kc in range(4):
                    B.mm(ps[:, 2, :], wbm_s.t[:, kc, j * 128:(j + 1) * 128], ymt[k].t[:, kc, :], kc == 0, kc == 3, R=[wbm_s.b, ymt[k].b], Wp=pb[2])
                B.tt('dve', ma[jj].t[:], ps[:, 0, :], g_.t[:, 0, :], ALU.mult, R=[pb[0], g_.b], W=[ma[jj].b])
                B.tt('dve', mb2[jj].t[:], ps[:, 1, :], g_.t[:, 1, :], ALU.mult, R=[pb[1], g_.b], W=[mb2[jj].b])
                B.tt('dve', ma[jj].t[:], ma[jj].t[:], mb2[jj].t[:], ALU.add, R=[ma[jj].b, mb2[jj].b], W=[ma[jj].b])
                B.tt('dve', mb2[jj].t[:], ps[:, 2, :], g_.t[:, 2, :], ALU.mult, R=[pb[2], g_.b], W=[mb2[jj].b])
                B.tt('dve', mT[k].t[:, j, :], ma[jj].t[:], mb2[jj].t[:], ALU.add, R=[ma[jj].b, mb2[jj].b], Wp=[mT[k].b])
            for b4 in range(4):
                blk = t * 4 + b4
                kk = blk % 2
                B.dma('sp', xr[kk].t[:], x_src[blk * 128:(blk + 1) * 128, :], R=[bxs[t]], W=[xr[kk].b])
                for half in range(2):
                    bank = 3 + half
                    for kc in range(8):
                        B.mm(ps[:, bank, :], mT[k].t[:, kc, b4 * 128:(b4 + 1) * 128], wo_s.t[:, kc, half * 512:(half + 1) * 512], kc == 0, kc == 7,
                             R=[mT[k].b, wo_s.b], Wp=pb[bank])
                    B.stt('dve', rr[kk].t[:, half * 512:(half + 1) * 512], xr[kk].t[:, half * 512:(half + 1) * 512], ALPHA, ps[:, bank, :],
                          ALU.mult, ALU.add, R=[xr[kk].b, pb[bank]], Wp=[rr[kk].b])
                layer_norm(rr[kk], lng, lnb, st[kk])
                B.dma('sp', X1[blk * 128:(blk + 1) * 128, :], rr[kk].t[:], R=[rr[kk].b], Wp=[bX1[t]])
                B.cp('act', x1b[kk].t[:], rr[kk].t[:], R=[rr[kk].b], W=[x1b[kk].b])
                for kc in range(8):
                    B.tr(psT[:, kc * 128:(kc + 1) * 128], x1b[kk].t[:, kc * 128:(kc + 1) * 128], ident, R=[x1b[kk].b, cbf.b], Wp=pb[7])
                B.cp('dve', x1T[k].t[:, :, b4 * 128:(b4 + 1) * 128], psT3, R=[pb[7]], Wp=[x1T[k].b])
                for kc in range(8):
                    B.mm(ps[:, 5, 0:16], x1T[k].t[:, kc, b4 * 128:(b4 + 1) * 128], wrt.t[:, kc, :], kc == 0, kc == 7, R=[x1T[k].b, wrt.b], Wp=pb[5])
                R_ = rt[kk]
                rb_ = R_.b
                col = lambda j: R_.t[:, :, j]
                g44 = lambda ap: ap.rearrange("p (g e) -> p g e", g=4)
                B.tt('dve', col(0), ps[:, 5, 0:16], rbias.t[:], ALU.add, R=[pb[5], rbias.b], W=[rb_])
                B.rmax(R_.t[:, 0:4, 1], g44(col(0)), R=[rb_], W=[rb_])
                B.rmax(R_.t[:, 4:5, 1], R_.t[:, 0:4, 1], R=[rb_], W=[rb_])
                B.ts('dve', R_.t[:, 0:4, 2], R_.t[:, 0:4, 1], R_.t[:, 4:5, 1], NEG, ALU.is_lt, ALU.mult, R=[rb_], W=[rb_])
                for g in range(4):
                    B.ts('dve', R_.t[:, g * 4:(g + 1) * 4, 3], R_.t[:, g * 4:(g + 1) * 4, 0], R_.t[:, g:g + 1, 2], None, ALU.add, None, R=[rb_], W=[rb_])
                B.rmax(R_.t[:, 5:6, 1], col(3), R=[rb_], W=[rb_])
                B.ts('dve', col(4), col(3), R_.t[:, 5:6, 1], NEG, ALU.is_ge, ALU.mult, R=[rb_], W=[rb_])
                B.tt('dve', col(4), col(4), col(3), ALU.add, R=[rb_], W=[rb_])
                B.rmax(R_.t[:, 6:7, 1], col(4), R=[rb_], W=[rb_])
                B.ts('dve', col(5), col(3), R_.t[:, 6:7, 1], None, ALU.is_ge, None, R=[rb_], W=[rb_])
                B.ts('dve', col(6), col(3), R_.t[:, 5:6, 1], -80.0, ALU.subtract, ALU.max, R=[rb_], W=[rb_])
                B.act(col(7), col(6), AF.Exp, R=[rb_], W=[rb_])
                B.tt('dve', col(7), col(7), col(5), ALU.mult, R=[rb_], W=[rb_])
                B.red(R_.t[:, 7:8, 1], col(7), R=[rb_], W=[rb_])
                B.recip(R_.t[:, 8:9, 1], R_.t[:, 7:8, 1], R=[rb_], W=[rb_])
                B.ts('dve', wtb[kk].t[:], col(7), R_.t[:, 8:9, 1], None, ALU.mult, None, R=[rb_], W=[wtb[kk].b])
                B.mm(ps[0:16, 6, 0:128], wtb[kk].t[:], ident, True, True, R=[wtb[kk].b, cbf.b], Wp=pb[6])
                B.cp('dve', wT.t[:, blk * 128:(blk + 1) * 128], ps[0:16, 6, 0:128], R=[pb[6]], Wp=[wT.b])
            for kc in range(8):
                B.dma('sp', X1T[kc, :, t * 512:(t + 1) * 512], x1T[k].t[:, kc, :], R=[x1T[k].b], Wp=[bX1T[t]])
        if dbg:
            B.dma('sp', WTD, wT.t[:], R=[wT.b], W=[bWTD])
        B.phase_end()
        if stages <= 4:
            continue

        lng2 = B.tile([128, D], F32, "lng2")
        lnb2 = B.tile([128, D], F32, "lnb2")
        B.dma('sp', lng2.t[:], bcast_rows(ln2g[l:l + 1, :], 128), W=[lng2.b])
        B.dma('sp', lnb2.t[:], bcast_rows(ln2b[l:l + 1, :], 128), W=[lnb2.b])
        NPT = 2048
        xp = B.tile([128, 8, NPT], BF16, "xp")
        facc = B.tile([128, 16, D], F32, "facc")
        bfacc = [Buf() for _ in range(16)]
        wg_ = [B.tile([128, 8, 512], BF16, "wg") for _ in range(2)]
        wu_ = [B.tile([128, 8, 512], BF16, "wu") for _ in range(2)]
        wd_ = [B.tile([128, 4, D], BF16, "wd") for _ in range(2)]
        hT = [B.tile([128, 4, 512], BF16, "hT") for _ in range(2)]
        wbs = [B.tile([128, 512], BF16, "wbs") for _ in range(2)]
        sg = [B.tile([128, 512], F32, "sg") for _ in range(2)]
        tu = [B.tile([128, 512], F32, "tu") for _ in range(2)]
        xr2 = [B.tile([128, D], F32, "xr2") for _ in range(2)]
        st2 = [B.tile([128, 16], F32, "st2") for _ in range(2)]
        gctr = 0
        dctr = 0
        hctr2 = 0
        for ps_ in range(T // NPT):
            tok0 = ps_ * NPT
            bxp = [Buf() for _ in range(4)]
            for tt_ in range(4):
                for kc in range(8):
                    B.dma('sp', xp.t[:, kc, tt_ * 512:(tt_ + 1) * 512], X1T[kc, :, tok0 + tt_ * 512: tok0 + (tt_ + 1) * 512], R=[bX1T[ps_ * 4 + tt_]], Wp=[bxp[tt_]])
            for ex in range(16):
                w = ex % 2
                B.dma('pool', wg_[w].t[:], weg[l, ex].rearrange("(kc p) n -> p kc n", p=128), W=[wg_[w].b])
                B.dma('pool', wu_[w].t[:], weu[l, ex].rearrange("(kc p) n -> p kc n", p=128), W=[wu_[w].b])
                B.dma('pool', wd_[w].t[:], wed[l, ex].rearrange("(kc p) n -> p kc n", p=128), W=[wd_[w].b])
                for tt_ in range(4):
                    hk = hctr2 % 2
                    hctr2 += 1
                    B.mm(ps[:, 6, :], sel.t[:, ex * 128:(ex + 1) * 128], wT.t[:, tok0 + tt_ * 512: tok0 + (tt_ + 1) * 512], True, True, R=[sel.b, wT.b], Wp=pb[6])
                    B.cp('act', wbs[hk].t[:], ps[:, 6, :], R=[pb[6]], W=[wbs[hk].b])
                    for fc in range(4):
                        g2 = gctr % 2
                        gctr += 1
                        bg, bu = g2 * 2, g2 * 2 + 1
                        for kc in range(8):
                            B.mm(ps[:, bg, :], wg_[w].t[:, kc, fc * 128:(fc + 1) * 128], xp.t[:, kc, tt_ * 512:(tt_ + 1) * 512], kc == 0, kc == 7,
                                 R=[wg_[w].b, bxp[tt_]], Wp=pb[bg])
                        for kc in range(8):
                            B.mm(ps[:, bu, :], wu_[w].t[:, kc, fc * 128:(fc + 1) * 128], xp.t[:, kc, tt_ * 512:(tt_ + 1) * 512], kc == 0, kc == 7,
                                 R=[wu_[w].b, bxp[tt_]], Wp=pb[bu])
                        B.act(sg[g2].t[:], ps[:, bg, :], AF.Silu, R=[pb[bg]], W=[sg[g2].b])
                        B.tt('dve', tu[g2].t[:], ps[:, bu, :], sg[g2].t[:], ALU.mult, R=[pb[bu], sg[g2].b], W=[tu[g2].b])
                        B.tt('dve', hT[hk].t[:, fc, :], tu[g2].t[:], wbs[hk].t[:], ALU.mult, R=[tu[g2].b, wbs[hk].b], Wp=[hT[hk].b])
                    for b4 in range(4):
                        lb = tt_ * 4 + b4
                        for half in range(2):
                            bank = 4 + (dctr % 2)
                            dctr += 1
                            for fc in range(4):
                                B.mm(ps[:, bank, :], hT[hk].t[:, fc, b4 * 128:(b4 + 1) * 128], wd_[w].t[:, fc, half * 512:(half + 1) * 512], fc == 0, fc == 3,
                                     R=[hT[hk].b, wd_[w].b], Wp=pb[bank])
                            dst = facc.t[:, lb, half * 512:(half + 1) * 512]
                            if ex == 0:
                                B.cp('dve', dst, ps[:, bank, :], R=[pb[bank]], Wp=[bfacc[lb]])
                            else:
                                B.tt('dve', dst, dst, ps[:, bank, :], ALU.add, R=[pb[bank], bfacc[lb]], Wp=[bfacc[lb]])
            for lb in range(16):
                blk = ps_ * 16 + lb
                kk = lb % 2
                B.dma('sp', xr2[kk].t[:], X1[blk * 128:(blk + 1) * 128, :], R=[bX1[blk // 4]], W=[xr2[kk].b])
                B.stt('dve', xr2[kk].t[:], xr2[kk].t[:], ALPHA, facc.t[:, lb, :], ALU.mult, ALU.add, R=[xr2[kk].b, bfacc[lb]], W=[xr2[kk].b])
                layer_norm(xr2[kk], lng2, lnb2, st2[kk])
                B.dma('sp', y_dst[blk * 128:(blk + 1) * 128, :], xr2[kk].t[:], R=[xr2[kk].b], Wp=[by[blk // 4]])
        B.phase_end()

    P.barrier()
    print("nops", P.nops, {e: len(P.ops[e]) for e in ENGS})
    P.emit()
    return nc


def host_consts():
    ident = np.eye(128, dtype=np.float32)
    Rm = np.zeros((128, 128), np.float32)
    for m in range(128):
        if (m % 64) < 32:
            Rm[m + 32, m] = -1.0
        else:
            Rm[m - 32, m] = 1.0
    tri = (np.arange(128)[None, :] >= np.arange(128)[:, None]).astype(np.float32)
    ones = np.ones((128, 128), np.float32)
    cbf = np.stack([ident, Rm, tri, ones, np.zeros_like(ones)], axis=1).astype(ml_dtypes.bfloat16)
    sel = np.zeros((16, 16, 128), np.float32)
    for e in range(16):
        sel[e, e, :] = 1.0
    sel = sel.reshape(16, 16 * 128).astype(ml_dtypes.bfloat16)
    inv_freq = (10000.0 ** (-np.arange(0, 64, 2, dtype=np.float32) / 64)).astype(np.float32)
    invf = inv_freq[np.arange(128) % 32].reshape(128, 1).astype(np.float32)
    return cbf, sel, invf


def core_tokens(h):
    qa, qb = (0, 3) if h == 0 else (1, 2)
    return np.concatenate([np.arange(qa * 2048, (qa + 1) * 2048), np.arange(qb * 2048, (qb + 1) * 2048)])


def core_cc(h):
    cc = np.zeros((128, 16), np.float32)
    if h == 0:
        cc[:, 0] = NEG
        cc[:, 8 + 3] = 1.0
    else:
        cc[:, 3] = NEG
        cc[:, 4 + 0] = 1.0
        cc[:, 8 + 2] = 1.0
    return cc


_NC_CACHE = {}


def make_in_maps(inputs):
    cbf, sel, invf = host_consts()
    x = np.asarray(inputs['x'])
    mem = np.asarray(inputs['mem'])
    pos = np.asarray(inputs['positions'])
    shared = {}
    for k in ['w_in', 'lambda_q1', 'lambda_k1', 'lambda_q2', 'lambda_k2', 'diff_subln_g', 'conv_w', 'w_mem_kv',
              'w_br_attn', 'w_br_conv', 'w_br_mem', 'w_out', 'ln1_g', 'ln1_b', 'ln2_g', 'ln2_b', 'w_router',
              'w_exp_gate', 'w_exp_up', 'w_exp_down']:
        shared[k] = np.ascontiguousarray(np.asarray(inputs[k]), dtype=np.float32)
    shared['router_bias'] = np.ascontiguousarray(np.asarray(inputs['router_bias'], dtype=np.float32).reshape(1, 16))
    shared['cbf'] = cbf
    shared['sel'] = sel
    shared['invf'] = invf
    in_maps = []
    for c in range(8):
        b, h = c // 2, c % 2
        tok = core_tokens(h)
        m = dict(shared)
        m['x'] = np.ascontiguousarray(x[b, tok, :], dtype=np.float32)
        m['pos'] = np.ascontiguousarray(pos[b, tok].reshape(1, T).astype(np.int32))
        m['mem'] = np.ascontiguousarray(mem[b], dtype=np.float32)
        m['cc'] = core_cc(h)
        in_maps.append(m)
    return in_maps


def kernel(**inputs):
    in_maps = make_in_maps(inputs)
    if 'nc' not in _NC_CACHE:
        _NC_CACHE['nc'] = build_program()
    nc = _NC_CACHE['nc']
    res = run_bass_kernel_spmd(nc, in_maps, core_ids=list(range(8)))
    out = np.zeros((4, SEQ, D), np.float32)
    for c in range(8):
        b, h = c // 2, c % 2
        out[b, core_tokens(h), :] = np.asarray(res.results[c]['y'], dtype=np.float32)
    return out
```

```python
import math
import os
import numpy as np
import ml_dtypes
import concourse.bass as bass
import concourse.mybir as mybir
from concourse.bass_utils import run_bass_kernel_spmd

F32 = mybir.dt.float32
BF16 = mybir.dt.bfloat16
I32 = mybir.dt.int32
AF = mybir.ActivationFunctionType
ALU = mybir.AluOpType
AX = mybir.AxisListType

ENGS = ['pe', 'act', 'dve', 'pool', 'sp']
EPOCH = 30000
NDSEM = 8


class Buf:
    __slots__ = ('name', 'writers', 'readers')

    def __init__(self, name=''):
        self.name = name
        self.writers = []
        self.readers = []


class Op:
    __slots__ = ('eng', 'fn', 'deps', 'seq', 'signal', 'sig_idx', 'dma', 'dma_n', 'cc')

    def __init__(self, eng, fn, dma, cc=None):
        self.eng = eng
        self.fn = fn
        self.dma = dma
        self.cc = cc
        self.deps = ()
        self.signal = False
        self.sig_idx = -1
        self.dma_n = -1


class Prog:
    def __init__(self, nc):
        self.nc = nc
        self.ops = {e: [] for e in ENGS}
        self.last = {e: None for e in ENGS}
        self.ndma = {e: 0 for e in ENGS}
        self.dmas_open = []
        self.nops = 0
        self.ncc = 0

    def add(self, eng, fn, reads=(), writes=(), partial=(), dma=False, cc=False):
        op = Op(eng, fn, dma or cc)
        if cc:
            op.cc = self.ncc
            self.ncc += 1
        deps = set()
        for b in reads:
            deps.update(b.writers)
        for b in writes:
            deps.update(b.readers)
            deps.update(b.writers)
        for b in partial:
            if b.readers:
                deps.update(b.readers)
                deps.update(b.writers)
        for b in reads:
            b.readers.append(op)
        for b in writes:
            b.writers = [op]
            b.readers = []
        for b in partial:
            if b.readers:
                b.writers = [op]
                b.readers = []
            else:
                b.writers.append(op)
        self._finish(op, deps, False)
        return op

    def _finish(self, op, deps, is_barrier):
        eng = op.eng
        op.seq = len(self.ops[eng])
        best = {}
        dl = []
        for d in deps:
            if d.dma:
                dl.append(d)
            else:
                if d.eng == eng and eng == 'pe':
                    continue
                c = best.get(d.eng)
                if c is None or d.seq > c.seq:
                    best[d.eng] = d
        for d in best.values():
            d.signal = True
            dl.append(d)
        op.deps = dl
        if not is_barrier:
            if op.dma:
                if op.cc is None:
                    op.dma_n = self.ndma[eng]
                    self.ndma[eng] += 1
                self.dmas_open.append(op)
            else:
                self.last[eng] = op
        self.ops[eng].append(op)
        self.nops += 1

    def barrier(self):
        deps = set(self.dmas_open)
        for e in ENGS:
            if self.last[e] is not None:
                deps.add(self.last[e])
        for e in ENGS:
            op = Op(e, None, False)
            self._finish(op, set(deps), True)
        self.dmas_open = []

    def emit(self):
        nc = self.nc
        nsig = {}
        for e in ENGS:
            k = 0
            for op in self.ops[e]:
                if op.dma or op.fn is None:
                    continue
                if op.signal:
                    op.sig_idx = k
                    k += 1
            nsig[e] = k
        csems = {}
        for e in ENGS:
            n_ep = (nsig[e] + EPOCH - 1) // EPOCH
            csems[e] = [nc.alloc_semaphore(name=f"c_{e}_{i}") for i in range(max(n_ep, 1))]
        dsems = {}
        for e in ENGS:
            if self.ndma[e] > 0:
                dsems[e] = [nc.alloc_semaphore(name=f"d_{e}_{i}") for i in range(NDSEM)]
        ccsems = [nc.alloc_semaphore(name=f"cc_{i}") for i in range(self.ncc)]
        engobj = {'pe': 'tensor', 'act': 'scalar', 'dve': 'vector', 'pool': 'gpsimd', 'sp': 'sync'}

        def semval(d):
            if d.cc is not None:
                return ccsems[d.cc], 1
            if d.dma:
                return dsems[d.eng][d.dma_n % NDSEM], 16 * (d.dma_n // NDSEM + 1)
            return csems[d.eng][d.sig_idx // EPOCH], (d.sig_idx % EPOCH) + 1

        def run(e, eng):
            known = {}
            for op in self.ops[e]:
                waits = []
                if op.dma and op.cc is None and op.dma_n >= NDSEM:
                    s = dsems[e][op.dma_n % NDSEM]
                    waits.append((s, 16 * (op.dma_n // NDSEM)))
                for d in op.deps:
                    waits.append(semval(d))
                for (s, v) in waits:
                    key = s.num
                    if known.get(key, 0) >= v:
                        continue
                    known[key] = v
                    eng.wait_ge(s, v)
                if op.fn is None:
                    continue
                ins = op.fn(eng)
                if op.cc is not None:
                    ins.then_inc(ccsems[op.cc], 1)
                elif op.dma:
                    ins.then_inc(dsems[e][op.dma_n % NDSEM], 16)
                elif op.signal:
                    ins.then_inc(csems[e][op.sig_idx // EPOCH], 1)

        with nc.Block() as block:
            for e in ENGS:
                if not self.ops[e]:
                    continue
                deco = getattr(block, engobj[e])

                def mk(e):
                    def f(eng):
                        run(e, eng)
                    return f
                deco(mk(e))


D = 1024
SEQ = 8192
T = 4096
NB = T // 128
NT = T // 512
DEPTH = 2
ALPHA = (2 * DEPTH) ** 0.25
LN_EPS = 1e-5
RMS_EPS = 1e-5
NEG = -30000.0
COL_Q, COL_K, COL_V, COL_CB, COL_CC, COL_CU, COL_QM, COL_G = 0, 1024, 2048, 3072, 3584, 4096, 4608, 5120
SB_BASE = 16640
SB_LIMIT = 229376 - 512


class Tile:
    __slots__ = ('t', 'b')

    def __init__(self, t, b):
        self.t = t
        self.b = b


class Builder:
    def __init__(self, nc, dbg=False):
        self.nc = nc
        self.P = Prog(nc)
        self.dbg = dbg
        self.pers_off = SB_BASE
        self.ph_off = SB_BASE
        self.ph_base = SB_BASE
        self.cnt = 0

    def _alloc(self, shape, dtype, off):
        isz = 2 if dtype == BF16 else 4
        n = 1
        for s in shape[1:]:
            n *= s
        size = (n * isz + 63) // 64 * 64
        self.cnt += 1
        t = self.nc.alloc_sbuf_tensor_at(f"t{self.cnt}", list(shape), dtype, offset=off)
        return t, size

    def pers(self, shape, dtype, name=''):
        assert self.ph_off == self.ph_base, "persistent alloc only before phases"
        t, size = self._alloc(shape, dtype, self.pers_off)
        self.pers_off += size
        self.ph_base = self.ph_off = self.pers_off
        assert self.pers_off <= SB_LIMIT
        return Tile(t, Buf(name))

    def tile(self, shape, dtype, name=''):
        t, size = self._alloc(shape, dtype, self.ph_off)
        self.ph_off += size
        assert self.ph_off <= SB_LIMIT, (name, self.ph_off)
        return Tile(t, Buf(name))

    def phase_end(self):
        self.P.barrier()
        self.ph_off = self.ph_base

    def mark(self):
        return self.ph_off

    def release(self, m):
        self.P.barrier()
        self.ph_off = m

    def red(self, out, in_, R, W=(), Wp=()):
        self.P.add('dve', lambda e: e.reduce_sum(out=out, in_=in_, axis=AX.X), reads=R, writes=W, partial=Wp)

    def rmax(self, out, in_, R, W=(), Wp=()):
        self.P.add('dve', lambda e: e.tensor_reduce(out=out, in_=in_, axis=AX.X, op=ALU.max), reads=R, writes=W, partial=Wp)

    def recip(self, out, in_, R, W=(), Wp=()):
        self.P.add('dve', lambda e: e.reciprocal(out=out, in_=in_), reads=R, writes=W, partial=Wp)

    def cc(self, src, dst, R, W):
        import os
        if os.environ.get("NOCC"):
            return
        self.P.add('pool', lambda e: e.collective_compute(
            "AllGather", ALU.bypass, replica_groups=[[2 * g, 2 * g + 1] for g in range(int(os.environ.get("NCO", "8")) // 2)],
            ins=[src], outs=[dst]), reads=R, writes=W, cc=True)

    def mm(self, out, lhsT, rhs, start, stop, R, Wp):
        self.P.add('pe', lambda e: e.matmul(out, lhsT=lhsT, rhs=rhs, start=start, stop=stop), reads=R, partial=[Wp])

    def tr(self, out, in_, ident, R, Wp):
        self.P.add('pe', lambda e: e.transpose(out=out, in_=in_, identity=ident), reads=R, partial=[Wp])

    def act(self, out, in_, func, R, W=(), Wp=(), bias=0.0, scale=1.0, accum=None):
        if accum is None:
            self.P.add('act', lambda e: e.activation(out=out, in_=in_, func=func, bias=bias, scale=scale), reads=R, writes=W, partial=Wp)
        else:
            self.P.add('act', lambda e: e.activation(out=out, in_=in_, func=func, bias=bias, scale=scale, accum_out=accum), reads=R, writes=W, partial=Wp)

    def cp(self, eng, out, in_, R, W=(), Wp=()):
        if eng == 'act':
            self.P.add('act', lambda e: e.copy(out=out, in_=in_), reads=R, writes=W, partial=Wp)
        else:
            self.P.add(eng, lambda e: e.tensor_copy(out=out, in_=in_), reads=R, writes=W, partial=Wp)

    def tt(self, eng, out, a, b, op, R, W=(), Wp=()):
        self.P.add(eng, lambda e: e.tensor_tensor(out=out, in0=a, in1=b, op=op), reads=R, writes=W, partial=Wp)

    def ts(self, eng, out, a, s1, s2, op0, op1, R, W=(), Wp=()):
        if s2 is None:
            self.P.add(eng, lambda e: e.tensor_scalar(out=out, in0=a, scalar1=s1, scalar2=None, op0=op0), reads=R, writes=W, partial=Wp)
        else:
            self.P.add(eng, lambda e: e.tensor_scalar(out=out, in0=a, scalar1=s1, scalar2=s2, op0=op0, op1=op1), reads=R, writes=W, partial=Wp)

    def stt(self, eng, out, in0, scalar, in1, op0, op1, R, W=(), Wp=()):
        self.P.add(eng, lambda e: e.scalar_tensor_tensor(out=out, in0=in0, scalar=scalar, in1=in1, op0=op0, op1=op1), reads=R, writes=W, partial=Wp)

    def dma(self, q, out, in_, R=(), W=(), Wp=(), slow=False):
        if slow:
            self.P.add(q, lambda e: e.dma_start(out=out, in_=in_, allow_slow_non_contiguous=True), reads=R, writes=W, partial=Wp, dma=True)
        else:
            self.P.add(q, lambda e: e.dma_start(out=out, in_=in_), reads=R, writes=W, partial=Wp, dma=True)

    def memset(self, eng, ap, val, W=(), Wp=()):
        self.P.add(eng, lambda e: e.memset(ap, val), writes=W, partial=Wp)


def bcast_rows(ap2d_row, nparts):
    a = ap2d_row
    n = a.shape[-1]
    return bass.AP(a.tensor, a.offset, [[0, nparts], [1, n]])


def build_program(dbg=False, stages=99, nlayers=DEPTH):
    nc = bass.Bass("TRN2", target_bir_lowering=False)
    B = Builder(nc, dbg)
    P = B.P
    skind = "ExternalOutput" if dbg else "Internal"

    def din(name, shape, dt):
        return nc.dram_tensor(name, list(shape), dt, kind="ExternalInput").ap()

    def dscr(name, shape, dt, force_internal=False):
        k = "Internal" if force_internal else skind
        if isinstance(dbg, (set, list, tuple)):
            k = "ExternalOutput" if (name in dbg and not force_internal) else "Internal"
        return nc.dram_tensor(name, list(shape), dt, kind=k).ap()

    x_in = din("x", [T, D], F32)
    pos_in = din("pos", [1, T], I32)
    mem_in = din("mem", [256, D], F32)
    cc_in = din("cc", [128, 16], F32)
    cbf_in = din("cbf", [128, 5, 128], BF16)
    sel_in = din("sel", [16, 16 * 128], BF16)
    invf_in = din("invf", [128, 1], F32)
    w_in = din("w_in", [DEPTH, D, 8192], F32)
    lq1 = din("lambda_q1", [DEPTH, 64], F32)
    lk1 = din("lambda_k1", [DEPTH, 64], F32)
    lq2 = din("lambda_q2", [DEPTH, 64], F32)
    lk2 = din("lambda_k2", [DEPTH, 64], F32)
    subg = din("diff_subln_g", [DEPTH, 128], F32)
    convw = din("conv_w", [DEPTH, 3, 512], F32)
    wkv = din("w_mem_kv", [DEPTH, D, 1024], F32)
    if stages > 3:
        wba = din("w_br_attn", [DEPTH, 1024, D], F32)
        wbc = din("w_br_conv", [DEPTH, 512, D], F32)
        wbm = din("w_br_mem", [DEPTH, 512, D], F32)
        wout = din("w_out", [DEPTH, D, D], F32)
        ln1g = din("ln1_g", [DEPTH, D], F32)
        ln1b = din("ln1_b", [DEPTH, D], F32)
        ln2g = din("ln2_g", [DEPTH, D], F32)
        ln2b = din("ln2_b", [DEPTH, D], F32)
    wr_in = din("w_router", [D, 16], F32)
    rb_in = din("router_bias", [1, 16], F32)
    if stages > 4:
        weg = din("w_exp_gate", [DEPTH, 16, D, 512], F32)
        weu = din("w_exp_up", [DEPTH, 16, D, 512], F32)
        wed = din("w_exp_down", [DEPTH, 16, 512, D], F32)
    y_out = nc.dram_tensor("y", [T, D], F32, kind="ExternalOutput").ap()

    exinK = [dscr(f"exinK{i}_", [1024, 1024], BF16, True) for i in range(4)]
    exinV = [dscr(f"exinV{i}_", [1024, 1024], BF16, True) for i in range(4)]
    exoutK = [dscr(f"exoutK{i}_", [2048, 1024], BF16, True) for i in range(4)]
    exoutV = [dscr(f"exoutV{i}_", [2048, 1024], BF16, True) for i in range(4)]
    exinT = dscr("exinT_", [128, 16], BF16, True)
    if dbg:
        dK = [dscr(f"exinK{i}", [1024, 1024], BF16) for i in range(4)]
        dV = [dscr(f"exinV{i}", [1024, 1024], BF16) for i in range(4)]
        dT = dscr("exinT", [128, 16], BF16)
        doK = [dscr(f"exoutK{i}", [2048, 1024], BF16) for i in range(4)]
        doV = [dscr(f"exoutV{i}", [2048, 1024], BF16) for i in range(4)]
    exoutT = dscr("exoutT", [256, 16], BF16, True)
    COS = dscr("COS", [128, T], F32, True)
    SIN = dscr("SIN", [128, T], F32, True)
    QT = dscr("QT", [8, 128, T], BF16)
    Gs = dscr("Gs", [24, 128, T], BF16)
    YA = dscr("YA", [8, 128, T], BF16)
    YC = dscr("YC", [4, 128, T], BF16)
    YM = dscr("YM", [4, 128, T], BF16)
    X1 = dscr("X1", [T, D], F32)
    X1T = dscr("X1T", [8, 128, T], BF16)
    XRES = dscr("XRES", [T, D], F32)
    WTD = dscr("WTD", [16, T], BF16)
    bK = [Buf(f"exinK{i}") for i in range(4)]
    bV = [Buf(f"exinV{i}") for i in range(4)]
    boK = [Buf() for i in range(4)]
    boV = [Buf() for i in range(4)]
    bT, boT = Buf(), Buf()
    bCOS, bSIN = Buf(), Buf()
    bQT = [Buf() for i in range(8)]
    bG = [Buf() for i in range(24)]
    bYA = [Buf() for i in range(8)]
    bYC = [Buf() for i in range(4)]
    bYM = [Buf() for i in range(4)]
    bX1 = [Buf() for i in range(NT)]
    bX1T = [Buf() for i in range(NT)]
    bXR = [Buf() for i in range(NT)]
    bWTD = Buf()

    ps = nc.alloc_psum_tensor("ps", [128, 8, 512], F32)
    pb = [Buf(f"ps{i}") for i in range(8)]
    psT = ps[:, 7, :].bitcast(BF16)
    psT3 = psT.rearrange("p (k t) -> p k t", k=8)

    cbf = B.pers([128, 5, 128], BF16, "cbf")
    ident = cbf.t[:, 0, :]
    Rm = cbf.t[:, 1, :]
    tri2 = B.pers([128, 2, 128], BF16, "tri2")
    ones = cbf.t[:, 3, :]
    sel = B.pers([16, 16 * 128], BF16, "sel")
    ccs = B.pers([128, 16], F32, "ccs")
    invf = B.pers([128, 1], F32, "invf")
    wT = B.pers([16, T], BF16, "wT")
    lamt = B.pers([128, 8], F32, "lam")
    gsc = B.pers([128, 128], F32, "gsc")
    cw = B.pers([128, 3, 4], F32, "cw")
    rbias = B.pers([128, 16], F32, "rbias")
    wrt = B.pers([128, 8, 16], BF16, "wrt")
    KmT = B.pers([128, 4, 256], BF16, "KmT")
    Vm = B.pers([128, 2, 512], BF16, "Vm")

    B.dma('sp', cbf.t[:], cbf_in, W=[cbf.b])
    B.dma('sp', tri2.t[:, 0, :], cbf_in[:, 2, :], Wp=[tri2.b])
    B.dma('sp', tri2.t[:, 1, :], cbf_in[:, 2, :], Wp=[tri2.b])
    B.dma('sp', sel.t[:], sel_in, W=[sel.b])
    B.dma('sp', ccs.t[:], cc_in, W=[ccs.b])
    B.dma('sp', invf.t[:], invf_in, W=[invf.b])
    B.dma('sp', rbias.t[:], bcast_rows(rb_in, 128), W=[rbias.b])
    B.dma('pool', wrt.t[:], wr_in.rearrange("(kc p) n -> p kc n", p=128), W=[wrt.b])

    posi = B.tile([128, T], I32, "posi")
    ang = B.tile([128, T], F32, "ang")
    tmp = B.tile([128, T], F32, "tmp")
    tab = B.tile([128, T], F32, "tab")
    B.dma('sp', posi.t[:], bcast_rows(pos_in, 128), W=[posi.b])
    B.cp('dve', ang.t[:], posi.t[:], R=[posi.b], W=[ang.b])
    B.ts('dve', ang.t[:], ang.t[:], invf.t[:, 0:1], 1.0 / (2 * math.pi), ALU.mult, ALU.mult, R=[ang.b, invf.b], W=[ang.b])
    for (dst, bdst, off) in ((SIN, bSIN, 0.0), (COS, bCOS, 0.25)):
        B.ts('dve', tmp.t[:], ang.t[:], off, None, ALU.add, None, R=[ang.b], W=[tmp.b])
        B.cp('dve', posi.t[:], tmp.t[:], R=[tmp.b], W=[posi.b])
        B.cp('dve', tab.t[:], posi.t[:], R=[posi.b], W=[tab.b])
        B.tt('dve', tmp.t[:], tmp.t[:], tab.t[:], ALU.subtract, R=[tmp.b, tab.b], W=[tmp.b])
        B.act(tab.t[:], tmp.t[:], AF.Sin, R=[tmp.b], W=[tab.b], scale=6.28318)
        B.dma('sp', dst, tab.t[:], R=[tab.b], W=[bdst])
    B.phase_end()
    if stages <= 0.1:
        nlayers = 0

    for l in range(nlayers):
        lam_init = 0.8 - 0.6 * math.exp(-0.3 * l)
        x_src = x_in if l == 0 else XRES
        bxs = [Buf() for _ in range(NT)] if l == 0 else bXR
        y_dst = y_out if l == nlayers - 1 else XRES
        by = [Buf() for _ in range(NT)] if l == nlayers - 1 else bXR

        lt = B.tile([128, 4, 64], F32, "lt")
        for i, a in enumerate((lq1, lk1, lq2, lk2)):
            B.dma('sp', lt.t[:, i, :], bcast_rows(a[l:l + 1, :], 128), Wp=[lt.b])
        lp = B.tile([128, 2, 64], F32, "lp")
        lsum = B.tile([128, 4], F32, "lsum")
        B.tt('dve', lp.t[:, 0, :], lt.t[:, 0, :], lt.t[:, 1, :], ALU.mult, R=[lt.b], Wp=[lp.b])
        B.tt('dve', lp.t[:, 1, :], lt.t[:, 2, :], lt.t[:, 3, :], ALU.mult, R=[lt.b], Wp=[lp.b])
        B.red(lsum.t[:, 0:2], lp.t[:], R=[lp.b], W=[lsum.b])
        B.act(lsum.t[:, 2:4], lsum.t[:, 0:2], AF.Exp, R=[lsum.b], W=[lsum.b])
        B.tt('dve', lamt.t[:, 0:1], lsum.t[:, 3:4], lsum.t[:, 2:3], ALU.subtract, R=[lsum.b], W=[lamt.b])
        B.ts('dve', lamt.t[:, 0:1], lamt.t[:, 0:1], -lam_init, None, ALU.add, None, R=[lamt.b], W=[lamt.b])
        B.dma('sp', gsc.t[:], bcast_rows(subg[l:l + 1, :], 128), W=[gsc.b])
        B.ts('dve', gsc.t[:], gsc.t[:], 1.0 - lam_init, None, ALU.mult, None, R=[gsc.b], W=[gsc.b])
        for tap in range(3):
            B.dma('sp', cw.t[:, tap, :], convw[l, tap, :].rearrange("(c p) -> p c", p=128), Wp=[cw.b], slow=True)

        memT = B.tile([128, 8, 256], BF16, "memT")
        wkvs = B.tile([128, 8, 1024], BF16, "wkvs")
        B.dma('pool', wkvs.t[:], wkv[l].rearrange("(kc p) n -> p kc n", p=128), W=[wkvs.b])
        mf = B.tile([128, D], F32, "mf")
        mb = B.tile([128, D], BF16, "mb")
        for mc in range(2):
            B.dma('sp', mf.t[:], mem_in[mc * 128:(mc + 1) * 128, :], W=[mf.b])
            B.cp('dve', mb.t[:], mf.t[:], R=[mf.b], W=[mb.b])
            for kc in range(8):
                B.tr(psT[:, kc * 128:(kc + 1) * 128], mb.t[:, kc * 128:(kc + 1) * 128], ident, R=[mb.b, cbf.b], Wp=pb[7])
            B.cp('dve', memT.t[:, :, mc * 128:(mc + 1) * 128], psT3, R=[pb[7]], Wp=[memT.b])
        for hd in range(4):
            for kc in range(8):
                B.mm(ps[:, 0, 0:256], wkvs.t[:, kc, hd * 128:(hd + 1) * 128], memT.t[:, kc, :], kc == 0, kc == 7, R=[wkvs.b, memT.b], Wp=pb[0])
            B.cp('dve', KmT.t[:, hd, :], ps[:, 0, 0:256], R=[pb[0]], Wp=[KmT.b])
        for mc in range(2):
            for kc in range(8):
                B.mm(ps[:, 1, :], memT.t[:, kc, mc * 128:(mc + 1) * 128], wkvs.t[:, kc, 512:1024], kc == 0, kc == 7, R=[wkvs.b, memT.b], Wp=pb[1])
            B.cp('dve', Vm.t[:, mc, :], ps[:, 1, :], R=[pb[1]], Wp=[Vm.b])
        B.phase_end()
        if stages <= 0.2:
            continue

        xT = B.tile([128, 8, T], BF16, "xT")
        bxT = [Buf() for _ in range(NT)]
        m0 = B.mark()
        xf = [B.tile([128, D], F32, "xf") for _ in range(2)]
        xb = [B.tile([128, D], BF16, "xb") for _ in range(2)]
        for blk in range(NB):
            i = blk % 2
            B.dma('sp', xf[i].t[:], x_src[blk * 128:(blk + 1) * 128, :], R=[bxs[blk // 4]], W=[xf[i].b])
            B.cp('act', xb[i].t[:], xf[i].t[:], R=[xf[i].b], W=[xb[i].b])
            for kc in range(8):
                B.tr(psT[:, kc * 128:(kc + 1) * 128], xb[i].t[:, kc * 128:(kc + 1) * 128], ident, R=[xb[i].b, cbf.b], Wp=pb[7])
            B.cp('dve', xT.t[:, :, blk * 128:(blk + 1) * 128], psT3, R=[pb[7]], Wp=[bxT[blk // 4]])
        B.release(m0)
        if stages <= 0.3:
            B.phase_end()
            continue

        NSLAB = 4
        slabs = [B.tile([128, 8, 512], BF16, f"slab{i}") for i in range(NSLAB)]
        plan = [COL_K, COL_K + 512, COL_V, COL_V + 512, COL_CC, COL_CU, COL_Q, COL_Q + 512, COL_QM] + \
               [COL_G + i * 512 for i in range(6)] + [COL_CB, COL_CC, COL_CU]
        if stages <= 1:
            plan = plan[:6]
        st_ = {'issued': 0, 'taken': 0}

        def issue_slab():
            n = st_['issued']
            if n >= len(plan):
                return
            s = slabs[n % NSLAB]
            c0 = plan[n]
            B.dma('pool', s.t[:], w_in[l, :, c0:c0 + 512].rearrange("(kc p) n -> p kc n", p=128), W=[s.b])
            st_['issued'] += 1

        def next_slab(c0):
            n = st_['taken']
            assert plan[n] == c0, (plan[n], c0)
            while st_['issued'] <= n + 1:
                if st_['issued'] >= len(plan):
                    break
                issue_slab()
            st_['taken'] += 1
            return slabs[n % NSLAB]

        pctr = [0]

        def proj_fm(s, j, t):
            bi = pctr[0] % 4
            pctr[0] += 1
            for kc in range(8):
                B.mm(ps[:, bi, :], s.t[:, kc, j * 128:(j + 1) * 128], xT.t[:, kc, t * 512:(t + 1) * 512], kc == 0, kc == 7,
                     R=[s.b, bxT[t]], Wp=pb[bi])
            return bi

        hbuf = [B.tile([128, T], BF16, "hbuf") for _ in range(2)]
        hctr = [0]

        def next_hb():
            hb = hbuf[hctr[0] % 2]
            hctr[0] += 1
            return hb

        ma_ = B.mark()
        cosT = B.tile([128, T], F32, "cos")
        sinT = B.tile([128, T], F32, "sin")
        B.dma('sp', cosT.t[:], COS, R=[bCOS], W=[cosT.b])
        B.dma('sp', sinT.t[:], SIN, R=[bSIN], W=[sinT.b])
        kb_ = [B.tile([128, 512], BF16, "kb") for _ in range(2)]
        t1_ = [B.tile([128, 512], F32, "t1") for _ in range(2)]
        t2_ = [B.tile([128, 512], F32, "t2") for _ in range(2)]
        rctr = [0]

        import os
        RH = int(os.environ.get("RH", "9"))

        def rope_head(s, j, hb):
            for t in range(NT):
                bi = proj_fm(s, j, t)
                r = rctr[0] % 2
                rctr[0] += 1
                if RH < 2:
                    continue
                B.cp('act', kb_[r].t[:], ps[:, bi, :], R=[pb[bi]], W=[kb_[r].b])
                B.mm(ps[:, 4 + r, :], Rm, kb_[r].t[:], True, True, R=[kb_[r].b, cbf.b], Wp=pb[4 + r])
                if RH < 3:
                    continue
                B.tt('dve', t1_[r].t[:], ps[:, bi, :], cosT.t[:, t * 512:(t + 1) * 512], ALU.mult, R=[pb[bi], cosT.b, kb_[r].b], W=[t1_[r].b])
                B.tt('dve', t2_[r].t[:], ps[:, 4 + r, :], sinT.t[:, t * 512:(t + 1) * 512], ALU.mult, R=[pb[4 + r], sinT.b], W=[t2_[r].b])
                B.tt('dve', hb.t[:, t * 512:(t + 1) * 512], t1_[r].t[:], t2_[r].t[:], ALU.add, R=[t1_[r].b, t2_[r].b], Wp=[hb.b])

        for sl in range(2):
            s = next_slab(COL_K + sl * 512)
            for j in range(4):
                hd = sl * 4 + j
                hb = next_hb()
                rope_head(s, j, hb)
                i, hh = hd // 2, hd % 2
                if RH >= 4:
                    B.dma('sp', exinK[i].rearrange("(h p a) c -> h p (a c)", h=2, a=4)[hh], hb.t[:], R=[hb.b], Wp=[bK[i]])
                if hh == 1:
                    B.cc(exinK[i], exoutK[i], R=[bK[i]], W=[boK[i]])
        if stages <= 0.5:
            B.phase_end()
            continue
        vt = [B.tile([128, 512], BF16, "vt") for _ in range(2)]
        vctr = 0
        for half in range(2):
            s = next_slab(COL_V + half * 512)
            for blk in range(NB):
                bi = pctr[0] % 4
                pctr[0] += 1
                for kc in range(8):
                    B.mm(ps[:, bi, :], xT.t[:, kc, blk * 128:(blk + 1) * 128], s.t[:, kc, :], kc == 0, kc == 7, R=[s.b, bxT[blk // 4]], Wp=pb[bi])
                v = vt[vctr % 2]
                vctr += 1
                B.cp('act', v.t[:], ps[:, bi, :], R=[pb[bi]], W=[v.b])
                for pr in range(2):
                    i = half * 2 + pr
                    dst = exinV[i].rearrange("(h tt) (tr d) -> h (tt tr) d", h=2, tr=8)[:, blk * 128:(blk + 1) * 128, :].rearrange("h t d -> t h d")
                    B.dma('sp', dst, v.t[:, pr * 256:(pr + 1) * 256].rearrange("p (h d) -> p h d", h=2), R=[v.b], Wp=[bV[i]])
            for pr in range(2):
                i = half * 2 + pr
                B.cc(exinV[i], exoutV[i], R=[bV[i]], W=[boV[i]])
        if stages <= 0.7:
            B.phase_end()
            continue
        s_cc = next_slab(COL_CC)
        s_cu = next_slab(COL_CU)
        tl = B.tile([128, 2, 4, 4], F32, "tl")
        tlb = B.tile([128, 4, 4], BF16, "tlb")
        xtail = xT.t[:, :, :].rearrange("p k (c t) -> p k c t", c=2)
        for which, s in enumerate((s_cc, s_cu)):
            for chc in range(4):
                c0 = (which * 4 + chc) * 4
                for kc in range(8):
                    B.mm(ps[:, 6, c0:c0 + 4].rearrange("p (c t) -> p c t", c=2),
                         s.t[:, kc, chc * 128:(chc + 1) * 128], xtail[:, kc, :, 2046:2048], kc == 0, kc == 7,
                         R=[s.b, bxT[3], bxT[7]], Wp=pb[6])
        B.cp('dve', tl.t[:].rearrange("p a b c -> p (a b c)"), ps[:, 6, 0:32], R=[pb[6]], W=[tl.b])
        B.tt('dve', tlb.t[:], tl.t[:, 0, :, :], tl.t[:, 1, :, :], ALU.mult, R=[tl.b], W=[tlb.b])
        B.dma('sp', exinT, tlb.t[:].rearrange("p a b -> p (a b)"), R=[tlb.b], W=[bT])
        B.cc(exinT, exoutT, R=[bT], W=[boT])
        if dbg:
            for i in range(4):
                B.dma('sp', dK[i], exinK[i], R=[bK[i]])
                B.dma('sp', dV[i], exinV[i], R=[bV[i]])
            B.dma('sp', dT, exinT, R=[bT])
            for i in range(4):
                B.dma('sp', doK[i], exoutK[i], R=[boK[i]])
                B.dma('sp', doV[i], exoutV[i], R=[boV[i]])
        if stages <= 1:
            B.phase_end()
            continue

        for sl in range(2):
            s = next_slab(COL_Q + sl * 512)
            for j in range(4):
                hd = sl * 4 + j
                hb = next_hb()
                rope_head(s, j, hb)
                B.dma('sp', QT[hd], hb.t[:], R=[hb.b], W=[bQT[hd]])
        B.release(ma_)

        s = next_slab(COL_QM)
        qmb = [B.tile([128, 512], BF16, "qmb") for _ in range(2)]
        pmt = [B.tile([128, 2, 512], BF16, "pmt") for _ in range(2)]
        rl = [B.tile([128, 512], F32, "rl") for _ in range(2)]
        mctr = 0
        for mh in range(4):
            hb = next_hb()
            for t in range(NT):
                bi = proj_fm(s, mh, t)
                r = mctr % 2
                mctr += 1
                B.cp('dve', qmb[r].t[:], ps[:, bi, :], R=[pb[bi]], W=[qmb[r].b])
                for mc in range(2):
                    B.mm(ps[:, 4 + mc, :], KmT.t[:, mh, mc * 128:(mc + 1) * 128], qmb[r].t[:], True, True, R=[KmT.b, qmb[r].b], Wp=pb[4 + mc])
                    B.act(pmt[r].t[:, mc, :], ps[:, 4 + mc, :], AF.Exp, R=[pb[4 + mc]], Wp=[pmt[r].b], scale=128 ** -0.5)
                for mc in range(2):
                    B.mm(ps[:, 6, :], Vm.t[:, mc, mh * 128:(mh + 1) * 128], pmt[r].t[:, mc, :], mc == 0, mc == 1, R=[Vm.b, pmt[r].b], Wp=pb[6])
                bl = pctr[0] % 4
                pctr[0] += 1
                for mc in range(2):
                    B.mm(ps[:, bl, :], ones, pmt[r].t[:, mc, :], mc == 0, mc == 1, R=[cbf.b, pmt[r].b], Wp=pb[bl])
                B.recip(rl[r].t[:], ps[:, bl, :], R=[pb[bl]], W=[rl[r].b])
                B.tt('dve', hb.t[:, t * 512:(t + 1) * 512], ps[:, 6, :], rl[r].t[:], ALU.mult, R=[pb[6], rl[r].b], Wp=[hb.b])
            B.dma('sp', YM[mh], hb.t[:], R=[hb.b], W=[bYM[mh]])
        B.release(ma_)

        for sl in range(6):
            s = next_slab(COL_G + sl * 512)
            for j in range(4):
                gi = sl * 4 + j
                hb = next_hb()
                for t in range(NT):
                    bi = proj_fm(s, j, t)
                    B.act(hb.t[:, t * 512:(t + 1) * 512], ps[:, bi, :], AF.Sigmoid, R=[pb[bi]], Wp=[hb.b])
                B.dma('sp', Gs[gi], hb.t[:], R=[hb.b], W=[bG[gi]])

        s_cb = next_slab(COL_CB)
        s_cc = next_slab(COL_CC)
        s_cu = next_slab(COL_CU)
        tg = B.tile([128, 2, 16], BF16, "tg")
        tgf = B.tile([128, 2, 16], F32, "tgf")
        for r in range(2):
            B.dma('sp', tg.t[:, r, :], exoutT[r * 128:(r + 1) * 128, :], R=[boT], Wp=[tg.b])
        B.cp('dve', tgf.t[:], tg.t[:], R=[tg.b], W=[tgf.b])
        halo = B.tile([128, 2, 4, 2], F32, "halo")
        for c in range(2):
            for k in range(4):
                r_, c_ = k // 2, k % 2
                src = tgf.t[:, r_, :].rearrange("p (a c t) -> p a c t", a=4, c=2)[:, :, c_, :]
                col = 4 + c * 4 + k
                if k == 0:
                    B.ts('dve', halo.t[:, c, :, :], src, ccs.t[:, col:col + 1], None, ALU.mult, None, R=[tgf.b, ccs.b], W=[halo.b] if c == 0 else (), Wp=[halo.b] if c == 1 else ())
                else:
                    B.stt('dve', halo.t[:, c, :, :], src, ccs.t[:, col:col + 1], halo.t[:, c, :, :], ALU.mult, ALU.add,
                          R=[tgf.b, ccs.b, halo.b], W=[halo.b])
        zrow = B.tile([128, 2, 2050], F32, "zrow")
        ccs_ = [B.tile([128, 512], F32, "ccsb") for _ in range(2)]
        yv = [B.tile([128, 512], F32, "yv") for _ in range(2)]
        cctr = 0
        for chc in range(4):
            hb = next_hb()
            for c in range(2):
                B.cp('dve', zrow.t[:, c, 0:2], halo.t[:, c, chc, :], R=[halo.b], Wp=[zrow.b])
            for t in range(NT):
                c, tt_ = t // 4, t % 4
                bc = proj_fm(s_cc, chc, t)
                r = cctr % 2
                cctr += 1
                B.cp('act', ccs_[r].t[:], ps[:, bc, :], R=[pb[bc]], W=[ccs_[r].b])
                bu = proj_fm(s_cu, chc, t)
                B.tt('dve', zrow.t[:, c, 2 + tt_ * 512:2 + (tt_ + 1) * 512], ps[:, bu, :], ccs_[r].t[:], ALU.mult, R=[pb[bu], ccs_[r].b], Wp=[zrow.b])
            for t in range(NT):
                c, tt_ = t // 4, t % 4
                r = cctr % 2
                cctr += 1
                bb = proj_fm(s_cb, chc, t)
                B.ts('dve', yv[r].t[:], zrow.t[:, c, tt_ * 512:tt_ * 512 + 512], cw.t[:, 0, chc:chc + 1], None, ALU.mult, None, R=[zrow.b, cw.b], W=[yv[r].b])
                B.stt('dve', yv[r].t[:], zrow.t[:, c, tt_ * 512 + 1:tt_ * 512 + 513], cw.t[:, 1, chc:chc + 1], yv[r].t[:], ALU.mult, ALU.add, R=[zrow.b, cw.b, yv[r].b], W=[yv[r].b])
                B.stt('dve', yv[r].t[:], zrow.t[:, c, tt_ * 512 + 2:tt_ * 512 + 514], cw.t[:, 2, chc:chc + 1], yv[r].t[:], ALU.mult, ALU.add, R=[zrow.b, cw.b, yv[r].b], W=[yv[r].b])
                B.tt('dve', hb.t[:, t * 512:(t + 1) * 512], ps[:, bb, :], yv[r].t[:], ALU.mult, R=[pb[bb], yv[r].b], Wp=[hb.b])
            B.dma('sp', YC[chc], hb.t[:], R=[hb.b], W=[bYC[chc]])
        B.phase_end()
        if stages <= 2:
            continue

        NBUF = 2
        Kg = [B.tile([128, 3 * 2048], BF16, "Kg") for _ in range(NBUF)]
        Ko = [B.tile([128, T], BF16, "Ko") for _ in range(NBUF)]
        Vg = [B.tile([128, 48, 129], BF16, "Vg") for _ in range(NBUF)]
        Vo = [B.tile([128, 32, 129], BF16, "Vo") for _ in range(NBUF)]
        Qh = [B.tile([128, T], BF16, "Qh") for _ in range(NBUF)]
        yaT = [B.tile([128, T], BF16, "yaT") for _ in range(2)]
        for i in range(NBUF):
            B.memset('dve', Vg[i].t[:, :, 128:129], 1.0, Wp=[Vg[i].b])
            B.memset('dve', Vo[i].t[:, :, 128:129], 1.0, Wp=[Vo[i].b])
        ptile = [B.tile([128, 2, 2, 256], BF16, "pt") for _ in range(3)]
        osb = [B.tile([128, 128], F32, "osb") for _ in range(2)]
        osq = [B.tile([128, 128], F32, "osq") for _ in range(2)]
        ybf = [B.tile([128, 128], BF16, "ybf") for _ in range(2)]
        sm = [B.tile([128, 8], F32, "sm") for _ in range(2)]
        slots = [(0, 0), (1, 0), (1, 1)]

        def load_head(hd):
            i, hh = hd // 2, hd % 2
            bfi = hd % NBUF
            kg, ko, vg, vo, qh = Kg[bfi], Ko[bfi], Vg[bfi], Vo[bfi], Qh[bfi]
            gK = exoutK[i].rearrange("(r h p a) c -> r h p (a c)", r=2, h=2, a=4)
            gV = exoutV[i].rearrange("(r h tt) (tr d) -> r h (tt tr) d", r=2, h=2, tr=8)
            for si, (rk, ch) in enumerate(slots):
                B.dma('sp', kg.t[:, si * 2048:(si + 1) * 2048], gK[rk, hh, :, ch * 2048:(ch + 1) * 2048], R=[boK[i]], Wp=[kg.b])
                B.dma('sp', vg.t[:, si * 16:(si + 1) * 16, 0:128],
                      gV[rk, hh, ch * 2048:(ch + 1) * 2048, :].rearrange("(n p) d -> p n d", p=128), R=[boV[i]], Wp=[vg.b])
            B.dma('sp', ko.t[:], exinK[i].rearrange("(h p a) c -> h p (a c)", h=2, a=4)[hh], R=[bK[i]], W=[ko.b])
            B.dma('sp', vo.t[:, :, 0:128], exinV[i].rearrange("(h tt) (tr d) -> h (tt tr) d", h=2, tr=8)[hh].rearrange("(n p) d -> p n d", p=128),
                  R=[bV[i]], Wp=[vo.b])
            B.dma('sp', qh.t[:], QT[hd], R=[bQT[hd]], W=[qh.b])

        oc = [[B.tile([128, 258], F32, "oc") for _ in range(2)] for _ in range(2)]
        ybf4 = [B.tile([128, 128], BF16, "ybf4") for _ in range(4)]
        stepctr = 0
        cmbctr = 0
        qtctr = 0
        TR_DELAY = 6
        load_head(0)
        for hd in range(8):
            if hd + 1 < 8:
                load_head(hd + 1)
            bfi = hd % NBUF
            kg, ko, vg, vo, qh = Kg[bfi], Ko[bfi], Vg[bfi], Vo[bfi], Qh[bfi]
            ya = yaT[hd % 2]
            items = []
            for c in range(2):
                pref = [(0, 0)] if c == 0 else [(0, 1), (1, 2), (2, 3)]
                for qt in range(8):
                    q0 = c * 2048 + qt * 256
                    steps = []
                    for (si, bcol) in pref:
                        for kb in range(16):
                            steps.append((kg.t[:, si * 2048 + kb * 128: si * 2048 + (kb + 1) * 128], vg.t[:, si * 16 + kb, :], bcol, None, kg.b, vg.b))
                    for kb in range(2 * qt + 2):
                        mk = None
                        if kb == 2 * qt:
                            mk = 0
                        elif kb == 2 * qt + 1:
                            mk = 1
                        steps.append((ko.t[:, c * 2048 + kb * 128: c * 2048 + (kb + 1) * 128], vo.t[:, c * 16 + kb, :], None, mk, ko.b, vo.b))
                    nsteps = len(steps)
                    assert nsteps % 2 == 0
                    for pidx in range(nsteps // 2):
                        items.append({'q0': q0, 'pidx': pidx, 'np': nsteps // 2, 'nsteps': nsteps, 's': (steps[2 * pidx], steps[2 * pidx + 1])})

            def emit_scores(it):
                nonlocal stepctr
                pc = stepctr
                stepctr += 1
                pt = ptile[pc % 3]
                bA = 2 * (pc % 2)
                it['pt'] = pt
                q0 = it['q0']
                bcol = it['s'][0][2]
                assert it['s'][1][2] == bcol
                for j in range(2):
                    kap, vap, _, mk, kbuf, vbuf = it['s'][j]
                    for comp in range(2):
                        B.mm(ps[:, bA + comp, j * 256:(j + 1) * 256], kap[comp * 64:(comp + 1) * 64, :],
                             qh.t[comp * 64:(comp + 1) * 64, q0:q0 + 256], True, True, R=[kbuf, qh.b], Wp=pb[bA + comp])
                bias = 0.0 if bcol is None else ccs.t[:, bcol:bcol + 1]
                for comp in range(2):
                    B.act(pt.t[:, comp, :, :].rearrange("p j q -> p (j q)"), ps[:, bA + comp, :], AF.Exp, R=[pb[bA + comp], ccs.b],
                          Wp=[pt.b], bias=bias, scale=0.125)

            pending = []

            def emit_pv(it, now):
                nonlocal cmbctr, qtctr
                pt = it['pt']
                q0 = it['q0']
                pidx, nsteps = it['pidx'], it['nsteps']
                if pidx == 0:
                    for comp in range(2):
                        B.mm(ps[:, 4 + comp, 0:258], cbf.t[:, 4, :], cbf.t[:, 0:3, :].rearrange("p a b -> p (a b)")[:, 0:258], True, False,
                             R=[cbf.b], Wp=pb[4 + comp])
                for j in range(2):
                    mk = it['s'][j][3]
                    if mk is not None:
                        B.tt('dve', pt.t[:, :, j, mk * 128:(mk + 1) * 128], pt.t[:, :, j, mk * 128:(mk + 1) * 128], tri2.t[:], ALU.mult,
                             R=[pt.b, tri2.b], W=[pt.b])
                for j in range(2):
                    kap, vap, _, mk, kbuf, vbuf = it['s'][j]
                    sidx = 2 * pidx + j
                    for comp in range(2):
                        for sub in range(2):
                            if mk == 1 and sub == 0:
                                continue
                            last = (sidx == nsteps - 1) if sub == 1 else (sidx == nsteps - 2)
                            B.mm(ps[:, 4 + comp, sub * 129:(sub + 1) * 129], pt.t[:, comp, j, sub * 128:(sub + 1) * 128], vap, False, last,
                                 R=[pt.b, vbuf], Wp=pb[4 + comp])
                if pidx != it['np'] - 1:
                    return
                par = qtctr % 2
                qtctr += 1
                o_ = oc[par]
                for comp in range(2):
                    B.cp('dve', o_[comp].t[:], ps[:, 4 + comp, 0:258], R=[pb[4 + comp]], W=[o_[comp].b])
                for sub in range(2):
                    k = cmbctr % 2
                    cmbctr += 1
                    o1 = o_[0].t[:, sub * 129:(sub + 1) * 129]
                    o2 = o_[1].t[:, sub * 129:(sub + 1) * 129]
                    b1, b2 = o_[0].b, o_[1].b
                    s_ = sm[k]
                    yb = ybf4[par * 2 + sub]
                    B.recip(s_.t[:, 0:1], o1[:, 128:129], R=[b1], W=[s_.b])
                    B.recip(s_.t[:, 1:2], o2[:, 128:129], R=[b2, s_.b], W=[s_.b])
                    B.tt('dve', s_.t[:, 2:3], s_.t[:, 1:2], lamt.t[:, 0:1], ALU.mult, R=[s_.b, lamt.b], W=[s_.b])
                    B.ts('dve', osb[k].t[:], o1[:, 0:128], s_.t[:, 0:1], None, ALU.mult, None, R=[b1, s_.b], W=[osb[k].b])
                    B.stt('dve', osb[k].t[:], o2[:, 0:128], s_.t[:, 2:3], osb[k].t[:], ALU.mult, ALU.add, R=[b2, s_.b, osb[k].b], W=[osb[k].b])
                    B.tt('dve', osq[k].t[:], osb[k].t[:], osb[k].t[:], ALU.mult, R=[osb[k].b], W=[osq[k].b])
                    B.red(s_.t[:, 3:4], osq[k].t[:], R=[osq[k].b, s_.b], W=[s_.b])
                    B.ts('dve', s_.t[:, 4:5], s_.t[:, 3:4], 1.0 / 128, RMS_EPS, ALU.mult, ALU.add, R=[s_.b], W=[s_.b])
                    B.act(s_.t[:, 6:7], s_.t[:, 4:5], AF.Ln, R=[s_.b], W=[s_.b])
                    B.act(s_.t[:, 5:6], s_.t[:, 6:7], AF.Exp, R=[s_.b], W=[s_.b], scale=-0.5)
                    B.stt('dve', yb.t[:], osb[k].t[:], s_.t[:, 5:6], gsc.t[:], ALU.mult, ALU.mult, R=[osb[k].b, s_.b, gsc.b], W=[yb.b])
                    pending.append((now + TR_DELAY, yb, par * 2 + sub, q0 + sub * 128))

            def flush_tr(now, force=False):
                while pending and (force or pending[0][0] <= now):
                    _, yb, kk, col = pending.pop(0)
                    B.tr(psT[:, kk * 128:(kk + 1) * 128], yb.t[:], ident, R=[yb.b, cbf.b], Wp=pb[7])
                    B.cp('dve', ya.t[:, col:col + 128], psT[:, kk * 128:(kk + 1) * 128], R=[pb[7]], Wp=[ya.b])

            n_it = len(items)
            for i in range(n_it + 1):
                if i < n_it:
                    emit_scores(items[i])
                if i >= 1:
                    emit_pv(items[i - 1], i)
                flush_tr(i)
            flush_tr(0, force=True)
            B.dma('sp', YA[hd], ya.t[:], R=[ya.b], W=[bYA[hd]])
        B.phase_end()
        if stages <= 3:
            continue

        wba_s = B.tile([128, 8, D], BF16, "wba")
        wbc_s = B.tile([128, 4, D], BF16, "wbc")
        wbm_s = B.tile([128, 4, D], BF16, "wbm")
        wo_s = B.tile([128, 8, D], BF16, "wo")
        B.dma('pool', wba_s.t[:], wba[l].rearrange("(kc p) n -> p kc n", p=128), W=[wba_s.b])
        B.dma('pool', wbc_s.t[:], wbc[l].rearrange("(kc p) n -> p kc n", p=128), W=[wbc_s.b])
        B.dma('pool', wbm_s.t[:], wbm[l].rearrange("(kc p) n -> p kc n", p=128), W=[wbm_s.b])
        B.dma('pool', wo_s.t[:], wout[l].rearrange("(kc p) n -> p kc n", p=128), W=[wo_s.b])
        lng = B.tile([128, D], F32, "lng")
        lnb = B.tile([128, D], F32, "lnb")
        B.dma('sp', lng.t[:], bcast_rows(ln1g[l:l + 1, :], 128), W=[lng.b])
        B.dma('sp', lnb.t[:], bcast_rows(ln1b[l:l + 1, :], 128), W=[lnb.b])
        yat = [B.tile([128, 8, 512], BF16, "yat") for _ in range(2)]
        yct = [B.tile([128, 4, 512], BF16, "yct") for _ in range(2)]
        ymt = [B.tile([128, 4, 512], BF16, "ymt") for _ in range(2)]
        gt = [B.tile([128, 3, 512], BF16, "gt") for _ in range(2)]
        mT = [B.tile([128, 8, 512], BF16, "mT") for _ in range(2)]
        ma = [B.tile([128, 512], F32, "ma") for _ in range(2)]
        mb2 = [B.tile([128, 512], F32, "mb2") for _ in range(2)]
        xr = [B.tile([128, D], F32, "xr") for _ in range(2)]
        rr = [B.tile([128, D], F32, "rr") for _ in range(2)]
        x1b = [B.tile([128, D], BF16, "x1b") for _ in range(2)]
        x1T = [B.tile([128, 8, 512], BF16, "x1T") for _ in range(2)]
        st = [B.tile([128, 16], F32, "st") for _ in range(2)]
        rt = [B.tile([128, 16, 8], F32, "rt") for _ in range(2)]
        wtb = [B.tile([128, 16], BF16, "wtb") for _ in range(2)]

        def layer_norm(r_, g_, b_, s_):
            P.add('dve', lambda e: e.bn_stats(out=s_.t[:, 0:6], in_=r_.t[:, 0:512]), reads=[r_.b], writes=[s_.b])
            P.add('dve', lambda e: e.bn_stats(out=s_.t[:, 6:12], in_=r_.t[:, 512:1024]), reads=[r_.b, s_.b], writes=[s_.b])
            P.add('dve', lambda e: e.bn_aggr(out=s_.t[:, 12:14], in_=s_.t[:, 0:12]), reads=[s_.b], writes=[s_.b])
            B.ts('dve', s_.t[:, 14:15], s_.t[:, 13:14], LN_EPS, None, ALU.add, None, R=[s_.b], W=[s_.b])
            B.act(s_.t[:, 15:16], s_.t[:, 14:15], AF.Ln, R=[s_.b], W=[s_.b])
            B.act(s_.t[:, 14:15], s_.t[:, 15:16], AF.Exp, R=[s_.b], W=[s_.b], scale=-0.5)
            B.ts('dve', r_.t[:], r_.t[:], s_.t[:, 12:13], s_.t[:, 14:15], ALU.subtract, ALU.mult, R=[r_.b, s_.b], W=[r_.b])
            B.tt('dve', r_.t[:], r_.t[:], g_.t[:], ALU.mult, R=[r_.b, g_.b], W=[r_.b])
            B.tt('dve', r_.t[:], r_.t[:], b_.t[:], ALU.add, R=[r_.b, b_.b], W=[r_.b])

        gctr2 = 0
        for t in range(NT):
            k = t % 2
            for hd in range(8):
                B.dma('sp', yat[k].t[:, hd, :], YA[hd, :, t * 512:(t + 1) * 512], R=[bYA[hd]], Wp=[yat[k].b])
            for c4 in range(4):
                B.dma('sp', yct[k].t[:, c4, :], YC[c4, :, t * 512:(t + 1) * 512], R=[bYC[c4]], Wp=[yct[k].b])
                B.dma('sp', ymt[k].t[:, c4, :], YM[c4, :, t * 512:(t + 1) * 512], R=[bYM[c4]], Wp=[ymt[k].b])
            for j in range(8):
                jj = j % 2
                g_ = gt[gctr2 % 2]
                gctr2 += 1
                for br in range(3):
                    B.dma('sp', g_.t[:, br, :], Gs[br * 8 + j, :, t * 512:(t + 1) * 512], R=[bG[br * 8 + j]], Wp=[g_.b])
                for kc in range(8):
                    B.mm(ps[:, 0, :], wba_s.t[:, kc, j * 128:(j + 1) * 128], yat[k].t[:, kc, :], kc == 0, kc == 7, R=[wba_s.b, yat[k].b], Wp=pb[0])
                for kc in range(4):
                    B.mm(ps[:, 1, :], wbc_s.t[:, kc, j * 128:(j + 1) * 128], yct[k].t[:, kc, :], kc == 0, kc == 3, R=[wbc_s.b, yct[k].b], Wp=pb[1])
                for kc in range(4):
                    B.mm(ps[:, 2, :], wbm_s.t[:, kc, j * 128:(j + 1) * 128], ymt[k].t[:, kc, :], kc == 0, kc == 3, R=[wbm_s.b, ymt[k].b], Wp=pb[2])
                B.tt('dve', ma[jj].t[:], ps[:, 0, :], g_.t[:, 0, :], ALU.mult, R=[pb[0], g_.b], W=[ma[jj].b])
                B.tt('dve', mb2[jj].t[:], ps[:, 1, :], g_.t[:, 1, :], ALU.mult, R=[pb[1], g_.b], W=[mb2[jj].b])
                B.tt('dve', ma[jj].t[:], ma[jj].t[:], mb2[jj].t[:], ALU.add, R=[ma[jj].b, mb2[jj].b], W=[ma[jj].b])
                B.tt('dve', mb2[jj].t[:], ps[:, 2, :], g_.t[:, 2, :], ALU.mult, R=[pb[2], g_.b], W=[mb2[jj].b])
                B.tt('dve', mT[k].t[:, j, :], ma[jj].t[:], mb2[jj].t[:], ALU.add, R=[ma[jj].b, mb2[jj].b], Wp=[mT[k].b])
            for b4 in range(4):
                blk = t * 4 + b4
                kk = blk % 2
                B.dma('sp', xr[kk].t[:], x_src[blk * 128:(blk + 1) * 128, :], R=[bxs[t]], W=[xr[kk].b])
                for half in range(2):
                    bank = 3 + half
                    for kc in range(8):
                        B.mm(ps[:, bank, :], mT[k].t[:, kc, b4 * 128:(b4 + 1) * 128], wo_s.t[:, kc, half * 512:(half + 1) * 512], kc == 0, kc == 7,
                             R=[mT[k].b, wo_s.b], Wp=pb[bank])
                    B.stt('dve', rr[kk].t[:, half * 512:(half + 1) * 512], xr[kk].t[:, half * 512:(half + 1) * 512], ALPHA, ps[:, bank, :],
                          ALU.mult, ALU.add, R=[xr[kk].b, pb[bank]], Wp=[rr[kk].b])
                layer_norm(rr[kk], lng, lnb, st[kk])
                B.dma('sp', X1[blk * 128:(blk + 1) * 128, :], rr[kk].t[:], R=[rr[kk].b], Wp=[bX1[t]])
                B.cp('act', x1b[kk].t[:], rr[kk].t[:], R=[rr[kk].b], W=[x1b[kk].b])
                for kc in range(8):
                    B.tr(psT[:, kc * 128:(kc + 1) * 128], x1b[kk].t[:, kc * 128:(kc + 1) * 128], ident, R=[x1b[kk].b, cbf.b], Wp=pb[7])
                B.cp('dve', x1T[k].t[:, :, b4 * 128:(b4 + 1) * 128], psT3, R=[pb[7]], Wp=[x1T[k].b])
                for kc in range(8):
                    B.mm(ps[:, 5, 0:16], x1T[k].t[:, kc, b4 * 128:(b4 + 1) * 128], wrt.t[:, kc, :], kc == 0, kc == 7, R=[x1T[k].b, wrt.b], Wp=pb[5])
                R_ = rt[kk]
                rb_ = R_.b
                col = lambda j: R_.t[:, :, j]
                g44 = lambda ap: ap.rearrange("p (g e) -> p g e", g=4)
                B.tt('dve', col(0), ps[:, 5, 0:16], rbias.t[:], ALU.add, R=[pb[5], rbias.b], W=[rb_])
                B.rmax(R_.t[:, 0:4, 1], g44(col(0)), R=[rb_], W=[rb_])
                B.rmax(R_.t[:, 4:5, 1], R_.t[:, 0:4, 1], R=[rb_], W=[rb_])
                B.ts('dve', R_.t[:, 0:4, 2], R_.t[:, 0:4, 1], R_.t[:, 4:5, 1], NEG, ALU.is_lt, ALU.mult, R=[rb_], W=[rb_])
                for g in range(4):
                    B.ts('dve', R_.t[:, g * 4:(g + 1) * 4, 3], R_.t[:, g * 4:(g + 1) * 4, 0], R_.t[:, g:g + 1, 2], None, ALU.add, None, R=[rb_], W=[rb_])
                B.rmax(R_.t[:, 5:6, 1], col(3), R=[rb_], W=[rb_])
                B.ts('dve', col(4), col(3), R_.t[:, 5:6, 1], NEG, ALU.is_ge, ALU.mult, R=[rb_], W=[rb_])
                B.tt('dve', col(4), col(4), col(3), ALU.add, R=[rb_], W=[rb_])
                B.rmax(R_.t[:, 6:7, 1], col(4), R=[rb_], W=[rb_])
                B.ts('dve', col(5), col(3), R_.t[:, 6:7, 1], None, ALU.is_ge, None, R=[rb_], W=[rb_])
                B.ts('dve', col(6), col(3), R_.t[:, 5:6, 1], -80.0, ALU.subtract, ALU.max, R=[rb_], W=[rb_])
                B.act(col(7), col(6), AF.Exp, R=[rb_], W=[rb_])
                B.tt('dve', col(7), col(7), col(5), ALU.mult, R=[rb_], W=[rb_])
                B.red(R_.t[:, 7:8, 1], col(7), R=[rb_], W=[rb_])
                B.recip(R_.t[:, 8:9, 1], R_.t[:, 7:8, 1], R=[rb_], W=[rb_])
                B.ts('dve', wtb[kk].t[:], col(7), R_.t[:, 8:9, 1], None, ALU.mult, None, R=[rb_], W=[wtb[kk].b])
                B.mm(ps[0:16, 6, 0:128], wtb[kk].t[:], ident, True, True, R=[wtb[kk].b, cbf.b], Wp=pb[6])
                B.cp('dve', wT.t[:, blk * 128:(blk + 1) * 128], ps[0:16, 6, 0:128], R=[pb[6]], Wp=[wT.b])
            for kc in range(8):
                B.dma('sp', X1T[kc, :, t * 512:(t + 1) * 512], x1T[k].t[:, kc, :], R=[x1T[k].b], Wp=[bX1T[t]])
        if dbg:
            B.dma('sp', WTD, wT.t[:], R=[wT.b], W=[bWTD])
        B.phase_end()
        if stages <= 4:
            continue

        lng2 = B.tile([128, D], F32, "lng2")
        lnb2 = B.tile([128, D], F32, "lnb2")
        B.dma('sp', lng2.t[:], bcast_rows(ln2g[l:l + 1, :], 128), W=[lng2.b])
        B.dma('sp', lnb2.t[:], bcast_rows(ln2b[l:l + 1, :], 128), W=[lnb2.b])
        NPT = 2048
        xp = B.tile([128, 8, NPT], BF16, "xp")
        facc = B.tile([128, 16, D], F32, "facc")
        bfacc = [Buf() for _ in range(16)]
        wg_ = [B.tile([128, 8, 512], BF16, "wg") for _ in range(2)]
        wu_ = [B.tile([128, 8, 512], BF16, "wu") for _ in range(2)]
        wd_ = [B.tile([128, 4, D], BF16, "wd") for _ in range(2)]
        hT = [B.tile([128, 4, 512], BF16, "hT") for _ in range(2)]
        wbs = [B.tile([128, 512], BF16, "wbs") for _ in range(2)]
        sg = [B.tile([128, 512], F32, "sg") for _ in range(2)]
        tu = [B.tile([128, 512], F32, "tu") for _ in range(2)]
        xr2 = [B.tile([128, D], F32, "xr2") for _ in range(2)]
        st2 = [B.tile([128, 16], F32, "st2") for _ in range(2)]
        gctr = 0
        dctr = 0
        hctr2 = 0
        prev_down = [None]
        for ps_ in range(T // NPT):
            tok0 = ps_ * NPT
            bxp = [Buf() for _ in range(4)]
            for tt_ in range(4):
                for kc in range(8):
                    B.dma('sp', xp.t[:, kc, tt_ * 512:(tt_ + 1) * 512], X1T[kc, :, tok0 + tt_ * 512: tok0 + (tt_ + 1) * 512], R=[bX1T[ps_ * 4 + tt_]], Wp=[bxp[tt_]])
            for ex in range(16):
                w = ex % 2
                B.dma('pool', wg_[w].t[:], weg[l, ex].rearrange("(kc p) n -> p kc n", p=128), W=[wg_[w].b])
                B.dma('pool', wu_[w].t[:], weu[l, ex].rearrange("(kc p) n -> p kc n", p=128), W=[wu_[w].b])
                B.dma('pool', wd_[w].t[:], wed[l, ex].rearrange("(kc p) n -> p kc n", p=128), W=[wd_[w].b])
                for tt_ in range(4):
                    hk = hctr2 % 2
                    hctr2 += 1
                    B.mm(ps[:, 6, :], sel.t[:, ex * 128:(ex + 1) * 128], wT.t[:, tok0 + tt_ * 512: tok0 + (tt_ + 1) * 512], True, True, R=[sel.b, wT.b], Wp=pb[6])
                    B.cp('act', wbs[hk].t[:], ps[:, 6, :], R=[pb[6]], W=[wbs[hk].b])
                    for fc in range(4):
                        g2 = gctr % 2
                        gctr += 1
                        bg, bu = g2 * 2, g2 * 2 + 1
                        for kc in range(8):
                            B.mm(ps[:, bg, :], wg_[w].t[:, kc, fc * 128:(fc + 1) * 128], xp.t[:, kc, tt_ * 512:(tt_ + 1) * 512], kc == 0, kc == 7,
                                 R=[wg_[w].b, bxp[tt_]], Wp=pb[bg])
                        for kc in range(8):
                            B.mm(ps[:, bu, :], wu_[w].t[:, kc, fc * 128:(fc + 1) * 128], xp.t[:, kc, tt_ * 512:(tt_ + 1) * 512], kc == 0, kc == 7,
                                 R=[wu_[w].b, bxp[tt_]], Wp=pb[bu])
                        B.act(sg[g2].t[:], ps[:, bg, :], AF.Silu, R=[pb[bg]], W=[sg[g2].b])
                        B.tt('dve', tu[g2].t[:], ps[:, bu, :], sg[g2].t[:], ALU.mult, R=[pb[bu], sg[g2].b], W=[tu[g2].b])
                        B.tt('dve', hT[hk].t[:, fc, :], tu[g2].t[:], wbs[hk].t[:], ALU.mult, R=[tu[g2].b, wbs[hk].b], Wp=[hT[hk].b])
                    def down(tt_=tt_, hk=hk, w=w, ex=ex):
                        nonlocal dctr
                        for b4 in range(4):
                            lb = tt_ * 4 + b4
                            for half in range(2):
                                bank = 4 + (dctr % 2)
                                dctr += 1
                                for fc in range(4):
                                    B.mm(ps[:, bank, :], hT[hk].t[:, fc, b4 * 128:(b4 + 1) * 128], wd_[w].t[:, fc, half * 512:(half + 1) * 512], fc == 0, fc == 3,
                                         R=[hT[hk].b, wd_[w].b], Wp=pb[bank])
                                dst = facc.t[:, lb, half * 512:(half + 1) * 512]
                                if ex == 0:
                                    B.cp('dve', dst, ps[:, bank, :], R=[pb[bank]], Wp=[bfacc[lb]])
                                else:
                                    B.tt('dve', dst, dst, ps[:, bank, :], ALU.add, R=[pb[bank], bfacc[lb]], Wp=[bfacc[lb]])
                    if prev_down[0] is not None:
                        prev_down[0]()
                    prev_down[0] = down
            if prev_down[0] is not None:
                prev_down[0]()
                prev_down[0] = None
            for lb in range(16):
                blk = ps_ * 16 + lb
                kk = lb % 2
                B.dma('sp', xr2[kk].t[:], X1[blk * 128:(blk + 1) * 128, :], R=[bX1[blk // 4]], W=[xr2[kk].b])
                B.stt('dve', xr2[kk].t[:], xr2[kk].t[:], ALPHA, facc.t[:, lb, :], ALU.mult, ALU.add, R=[xr2[kk].b, bfacc[lb]], W=[xr2[kk].b])
                layer_norm(xr2[kk], lng2, lnb2, st2[kk])
                B.dma('sp', y_dst[blk * 128:(blk + 1) * 128, :], xr2[kk].t[:], R=[xr2[kk].b], Wp=[by[blk // 4]])
        B.phase_end()

    P.barrier()
    print("nops", P.nops, {e: len(P.ops[e]) for e in ENGS})
    P.emit()
    return nc


def host_consts():
    ident = np.eye(128, dtype=np.float32)
    Rm = np.zeros((128, 128), np.float32)
    for m in range(128):
        if (m % 64) < 32:
            Rm[m + 32, m] = -1.0
        else:
            Rm[m - 32, m] = 1.0
    tri = (np.arange(128)[None, :] >= np.arange(128)[:, None]).astype(np.float32)
    ones = np.ones((128, 128), np.float32)
    cbf = np.stack([ident, Rm, tri, ones, np.zeros_like(ones)], axis=1).astype(ml_dtypes.bfloat16)
    sel = np.zeros((16, 16, 128), np.float32)
    for e in range(16):
        sel[e, e, :] = 1.0
    sel = sel.reshape(16, 16 * 128).astype(ml_dtypes.bfloat16)
    inv_freq = (10000.0 ** (-np.arange(0, 64, 2, dtype=np.float32) / 64)).astype(np.float32)
    invf = inv_freq[np.arange(128) % 32].reshape(128, 1).astype(np.float32)
    return cbf, sel, invf


def core_tokens(h):
    qa, qb = (0, 3) if h == 0 else (1, 2)
    return np.concatenate([np.arange(qa * 2048, (qa + 1) * 2048), np.arange(qb * 2048, (qb + 1) * 2048)])


def core_cc(h):
    cc = np.zeros((128, 16), np.float32)
    if h == 0:
        cc[:, 0] = NEG
        cc[:, 8 + 3] = 1.0
    else:
        cc[:, 3] = NEG
        cc[:, 4 + 0] = 1.0
        cc[:, 8 + 2] = 1.0
    return cc


_NC_CACHE = {}


def make_in_maps(inputs):
    cbf, sel, invf = host_consts()
    x = np.asarray(inputs['x'])
    mem = np.asarray(inputs['mem'])
    pos = np.asarray(inputs['positions'])
    shared = {}
    for k in ['w_in', 'lambda_q1', 'lambda_k1', 'lambda_q2', 'lambda_k2', 'diff_subln_g', 'conv_w', 'w_mem_kv',
              'w_br_attn', 'w_br_conv', 'w_br_mem', 'w_out', 'ln1_g', 'ln1_b', 'ln2_g', 'ln2_b', 'w_router',
              'w_exp_gate', 'w_exp_up', 'w_exp_down']:
        shared[k] = np.ascontiguousarray(np.asarray(inputs[k]), dtype=np.float32)
    shared['router_bias'] = np.ascontiguousarray(np.asarray(inputs['router_bias'], dtype=np.float32).reshape(1, 16))
    shared['cbf'] = cbf
    shared['sel'] = sel
    shared['invf'] = invf
    in_maps = []
    for c in range(8):
        b, h = c // 2, c % 2
        tok = core_tokens(h)
        m = dict(shared)
        m['x'] = np.ascontiguousarray(x[b, tok, :], dtype=np.float32)
        m['pos'] = np.ascontiguousarray(pos[b, tok].reshape(1, T).astype(np.int32))
        m['mem'] = np.ascontiguousarray(mem[b], dtype=np.float32)
        m['cc'] = core_cc(h)
        in_maps.append(m)
    return in_maps


def kernel(**inputs):
    in_maps = make_in_maps(inputs)
    if 'nc' not in _NC_CACHE:
        _NC_CACHE['nc'] = build_program()
    nc = _NC_CACHE['nc']
    res = run_bass_kernel_spmd(nc, in_maps, core_ids=list(range(8)))
    out = np.zeros((4, SEQ, D), np.float32)
    for c in range(8):
        b, h = c // 2, c % 2
        out[b, core_tokens(h), :] = np.asarray(res.results[c]['y'], dtype=np.float32)
    return out
```

```python
import math
import os
import numpy as np
import ml_dtypes
import concourse.bass as bass
import concourse.mybir as mybir
from concourse.bass_utils import run_bass_kernel_spmd

F32 = mybir.dt.float32
BF16 = mybir.dt.bfloat16
I32 = mybir.dt.int32
AF = mybir.ActivationFunctionType
ALU = mybir.AluOpType
AX = mybir.AxisListType

ENGS = ['pe', 'act', 'dve', 'pool', 'sp']
EPOCH = 30000
NDSEM = 8


class Buf:
    __slots__ = ('name', 'writers', 'readers')

    def __init__(self, name=''):
        self.name = name
        self.writers = []
        self.readers = []


class Op:
    __slots__ = ('eng', 'fn', 'deps', 'seq', 'signal', 'sig_idx', 'dma', 'dma_n', 'cc')

    def __init__(self, eng, fn, dma, cc=None):
        self.eng = eng
        self.fn = fn
        self.dma = dma
        self.cc = cc
        self.deps = ()
        self.signal = False
        self.sig_idx = -1
        self.dma_n = -1


class Prog:
    def __init__(self, nc):
        self.nc = nc
        self.ops = {e: [] for e in ENGS}
        self.last = {e: None for e in ENGS}
        self.ndma = {e: 0 for e in ENGS}
        self.dmas_open = []
        self.nops = 0
        self.ncc = 0

    def add(self, eng, fn, reads=(), writes=(), partial=(), dma=False, cc=False):
        op = Op(eng, fn, dma or cc)
        if cc:
            op.cc = self.ncc
            self.ncc += 1
        deps = set()
        for b in reads:
            deps.update(b.writers)
        for b in writes:
            deps.update(b.readers)
            deps.update(b.writers)
        for b in partial:
            if b.readers:
                deps.update(b.readers)
                deps.update(b.writers)
        for b in reads:
            b.readers.append(op)
        for b in writes:
            b.writers = [op]
            b.readers = []
        for b in partial:
            if b.readers:
                b.writers = [op]
                b.readers = []
            else:
                b.writers.append(op)
        self._finish(op, deps, False)
        return op

    def _finish(self, op, deps, is_barrier):
        eng = op.eng
        op.seq = len(self.ops[eng])
        best = {}
        dl = []
        for d in deps:
            if d.dma:
                dl.append(d)
            else:
                if d.eng == eng and eng == 'pe':
                    continue
                c = best.get(d.eng)
                if c is None or d.seq > c.seq:
                    best[d.eng] = d
        for d in best.values():
            d.signal = True
            dl.append(d)
        op.deps = dl
        if not is_barrier:
            if op.dma:
                if op.cc is None:
                    op.dma_n = self.ndma[eng]
                    self.ndma[eng] += 1
                self.dmas_open.append(op)
            else:
                self.last[eng] = op
        self.ops[eng].append(op)
        self.nops += 1

    def barrier(self):
        deps = set(self.dmas_open)
        for e in ENGS:
            if self.last[e] is not None:
                deps.add(self.last[e])
        for e in ENGS:
            op = Op(e, None, False)
            self._finish(op, set(deps), True)
        self.dmas_open = []

    def emit(self):
        nc = self.nc
        nsig = {}
        for e in ENGS:
            k = 0
            for op in self.ops[e]:
                if op.dma or op.fn is None:
                    continue
                if op.signal:
                    op.sig_idx = k
                    k += 1
            nsig[e] = k
        csems = {}
        for e in ENGS:
            n_ep = (nsig[e] + EPOCH - 1) // EPOCH
            csems[e] = [nc.alloc_semaphore(name=f"c_{e}_{i}") for i in range(max(n_ep, 1))]
        dsems = {}
        for e in ENGS:
            if self.ndma[e] > 0:
                dsems[e] = [nc.alloc_semaphore(name=f"d_{e}_{i}") for i in range(NDSEM)]
        ccsems = [nc.alloc_semaphore(name=f"cc_{i}") for i in range(self.ncc)]
        engobj = {'pe': 'tensor', 'act': 'scalar', 'dve': 'vector', 'pool': 'gpsimd', 'sp': 'sync'}

        def semval(d):
            if d.cc is not None:
                return ccsems[d.cc], 1
            if d.dma:
                return dsems[d.eng][d.dma_n % NDSEM], 16 * (d.dma_n // NDSEM + 1)
            return csems[d.eng][d.sig_idx // EPOCH], (d.sig_idx % EPOCH) + 1

        def run(e, eng):
            known = {}
            for op in self.ops[e]:
                waits = []
                if op.dma and op.cc is None and op.dma_n >= NDSEM:
                    s = dsems[e][op.dma_n % NDSEM]
                    waits.append((s, 16 * (op.dma_n // NDSEM)))
                for d in op.deps:
                    waits.append(semval(d))
                for (s, v) in waits:
                    key = s.num
                    if known.get(key, 0) >= v:
                        continue
                    known[key] = v
                    eng.wait_ge(s, v)
                if op.fn is None:
                    continue
                ins = op.fn(eng)
                if op.cc is not None:
                    ins.then_inc(ccsems[op.cc], 1)
                elif op.dma:
                    ins.then_inc(dsems[e][op.dma_n % NDSEM], 16)
                elif op.signal:
                    ins.then_inc(csems[e][op.sig_idx // EPOCH], 1)

        with nc.Block() as block:
            for e in ENGS:
                if not self.ops[e]:
                    continue
                deco = getattr(block, engobj[e])

                def mk(e):
                    def f(eng):
                        run(e, eng)
                    return f
                deco(mk(e))


D = 1024
SEQ = 8192
T = 4096
NB = T // 128
NT = T // 512
DEPTH = 2
ALPHA = (2 * DEPTH) ** 0.25
LN_EPS = 1e-5
RMS_EPS = 1e-5
NEG = -30000.0
COL_Q, COL_K, COL_V, COL_CB, COL_CC, COL_CU, COL_QM, COL_G = 0, 1024, 2048, 3072, 3584, 4096, 4608, 5120
SB_BASE = 16640
SB_LIMIT = 229376 - 512


class Tile:
    __slots__ = ('t', 'b')

    def __init__(self, t, b):
        self.t = t
        self.b = b


class Builder:
    def __init__(self, nc, dbg=False):
        self.nc = nc
        self.P = Prog(nc)
        self.dbg = dbg
        self.pers_off = SB_BASE
        self.ph_off = SB_BASE
        self.ph_base = SB_BASE
        self.cnt = 0

    def _alloc(self, shape, dtype, off):
        isz = 2 if dtype == BF16 else 4
        n = 1
        for s in shape[1:]:
            n *= s
        size = (n * isz + 63) // 64 * 64
        self.cnt += 1
        t = self.nc.alloc_sbuf_tensor_at(f"t{self.cnt}", list(shape), dtype, offset=off)
        return t, size

    def pers(self, shape, dtype, name=''):
        assert self.ph_off == self.ph_base, "persistent alloc only before phases"
        t, size = self._alloc(shape, dtype, self.pers_off)
        self.pers_off += size
        self.ph_base = self.ph_off = self.pers_off
        assert self.pers_off <= SB_LIMIT
        return Tile(t, Buf(name))

    def tile(self, shape, dtype, name=''):
        t, size = self._alloc(shape, dtype, self.ph_off)
        self.ph_off += size
        assert self.ph_off <= SB_LIMIT, (name, self.ph_off)
        return Tile(t, Buf(name))

    def phase_end(self):
        self.P.barrier()
        self.ph_off = self.ph_base

    def mark(self):
        return self.ph_off

    def release(self, m):
        self.P.barrier()
        self.ph_off = m

    def red(self, out, in_, R, W=(), Wp=()):
        self.P.add('dve', lambda e: e.reduce_sum(out=out, in_=in_, axis=AX.X), reads=R, writes=W, partial=Wp)

    def rmax(self, out, in_, R, W=(), Wp=()):
        self.P.add('dve', lambda e: e.tensor_reduce(out=out, in_=in_, axis=AX.X, op=ALU.max), reads=R, writes=W, partial=Wp)

    def recip(self, out, in_, R, W=(), Wp=()):
        self.P.add('dve', lambda e: e.reciprocal(out=out, in_=in_), reads=R, writes=W, partial=Wp)

    def cc(self, src, dst, R, W):
        import os
        if os.environ.get("NOCC"):
            return
        self.P.add('pool', lambda e: e.collective_compute(
            "AllGather", ALU.bypass, replica_groups=[[2 * g, 2 * g + 1] for g in range(int(os.environ.get("NCO", "8")) // 2)],
            ins=[src], outs=[dst]), reads=R, writes=W, cc=True)

    def mm(self, out, lhsT, rhs, start, stop, R, Wp):
        self.P.add('pe', lambda e: e.matmul(out, lhsT=lhsT, rhs=rhs, start=start, stop=stop), reads=R, partial=[Wp])

    def tr(self, out, in_, ident, R, Wp):
        self.P.add('pe', lambda e: e.transpose(out=out, in_=in_, identity=ident), reads=R, partial=[Wp])

    def act(self, out, in_, func, R, W=(), Wp=(), bias=0.0, scale=1.0, accum=None):
        if accum is None:
            self.P.add('act', lambda e: e.activation(out=out, in_=in_, func=func, bias=bias, scale=scale), reads=R, writes=W, partial=Wp)
        else:
            self.P.add('act', lambda e: e.activation(out=out, in_=in_, func=func, bias=bias, scale=scale, accum_out=accum), reads=R, writes=W, partial=Wp)

    def cp(self, eng, out, in_, R, W=(), Wp=()):
        if eng == 'act':
            self.P.add('act', lambda e: e.copy(out=out, in_=in_), reads=R, writes=W, partial=Wp)
        else:
            self.P.add(eng, lambda e: e.tensor_copy(out=out, in_=in_), reads=R, writes=W, partial=Wp)

    def tt(self, eng, out, a, b, op, R, W=(), Wp=()):
        self.P.add(eng, lambda e: e.tensor_tensor(out=out, in0=a, in1=b, op=op), reads=R, writes=W, partial=Wp)

    def ts(self, eng, out, a, s1, s2, op0, op1, R, W=(), Wp=()):
        if s2 is None:
            self.P.add(eng, lambda e: e.tensor_scalar(out=out, in0=a, scalar1=s1, scalar2=None, op0=op0), reads=R, writes=W, partial=Wp)
        else:
            self.P.add(eng, lambda e: e.tensor_scalar(out=out, in0=a, scalar1=s1, scalar2=s2, op0=op0, op1=op1), reads=R, writes=W, partial=Wp)

    def stt(self, eng, out, in0, scalar, in1, op0, op1, R, W=(), Wp=()):
        self.P.add(eng, lambda e: e.scalar_tensor_tensor(out=out, in0=in0, scalar=scalar, in1=in1, op0=op0, op1=op1), reads=R, writes=W, partial=Wp)

    def dma(self, q, out, in_, R=(), W=(), Wp=(), slow=False):
        if slow:
            self.P.add(q, lambda e: e.dma_start(out=out, in_=in_, allow_slow_non_contiguous=True), reads=R, writes=W, partial=Wp, dma=True)
        else:
            self.P.add(q, lambda e: e.dma_start(out=out, in_=in_), reads=R, writes=W, partial=Wp, dma=True)

    def memset(self, eng, ap, val, W=(), Wp=()):
        self.P.add(eng, lambda e: e.memset(ap, val), writes=W, partial=Wp)


def bcast_rows(ap2d_row, nparts):
    a = ap2d_row
    n = a.shape[-1]
    return bass.AP(a.tensor, a.offset, [[0, nparts], [1, n]])


def build_program(dbg=False, stages=99, nlayers=DEPTH):
    nc = bass.Bass("TRN2", target_bir_lowering=False)
    B = Builder(nc, dbg)
    P = B.P
    skind = "ExternalOutput" if dbg else "Internal"

    def din(name, shape, dt):
        return nc.dram_tensor(name, list(shape), dt, kind="ExternalInput").ap()

    def dscr(name, shape, dt, force_internal=False):
        k = "Internal" if force_internal else skind
        if isinstance(dbg, (set, list, tuple)):
            k = "ExternalOutput" if (name in dbg and not force_internal) else "Internal"
        return nc.dram_tensor(name, list(shape), dt, kind=k).ap()

    x_in = din("x", [T, D], F32)
    pos_in = din("pos", [1, T], I32)
    mem_in = din("mem", [256, D], F32)
    cc_in = din("cc", [128, 16], F32)
    cbf_in = din("cbf", [128, 5, 128], BF16)
    sel_in = din("sel", [16, 16 * 128], BF16)
    invf_in = din("invf", [128, 1], F32)
    w_in = din("w_in", [DEPTH, D, 8192], F32)
    lq1 = din("lambda_q1", [DEPTH, 64], F32)
    lk1 = din("lambda_k1", [DEPTH, 64], F32)
    lq2 = din("lambda_q2", [DEPTH, 64], F32)
    lk2 = din("lambda_k2", [DEPTH, 64], F32)
    subg = din("diff_subln_g", [DEPTH, 128], F32)
    convw = din("conv_w", [DEPTH, 3, 512], F32)
    wkv = din("w_mem_kv", [DEPTH, D, 1024], F32)
    if stages > 3:
        wba = din("w_br_attn", [DEPTH, 1024, D], F32)
        wbc = din("w_br_conv", [DEPTH, 512, D], F32)
        wbm = din("w_br_mem", [DEPTH, 512, D], F32)
        wout = din("w_out", [DEPTH, D, D], F32)
        ln1g = din("ln1_g", [DEPTH, D], F32)
        ln1b = din("ln1_b", [DEPTH, D], F32)
        ln2g = din("ln2_g", [DEPTH, D], F32)
        ln2b = din("ln2_b", [DEPTH, D], F32)
    wr_in = din("w_router", [D, 16], F32)
    rb_in = din("router_bias", [1, 16], F32)
    if stages > 4:
        weg = din("w_exp_gate", [DEPTH, 16, D, 512], F32)
        weu = din("w_exp_up", [DEPTH, 16, D, 512], F32)
        wed = din("w_exp_down", [DEPTH, 16, 512, D], F32)
    y_out = nc.dram_tensor("y", [T, D], F32, kind="ExternalOutput").ap()

    exinK = [dscr(f"exinK{i}_", [1024, 1024], BF16, True) for i in range(4)]
    exinV = [dscr(f"exinV{i}_", [1024, 1024], BF16, True) for i in range(4)]
    exoutK = [dscr(f"exoutK{i}_", [2048, 1024], BF16, True) for i in range(4)]
    exoutV = [dscr(f"exoutV{i}_", [2048, 1024], BF16, True) for i in range(4)]
    exinT = dscr("exinT_", [128, 16], BF16, True)
    if dbg:
        dK = [dscr(f"exinK{i}", [1024, 1024], BF16) for i in range(4)]
        dV = [dscr(f"exinV{i}", [1024, 1024], BF16) for i in range(4)]
        dT = dscr("exinT", [128, 16], BF16)
        doK = [dscr(f"exoutK{i}", [2048, 1024], BF16) for i in range(4)]
        doV = [dscr(f"exoutV{i}", [2048, 1024], BF16) for i in range(4)]
    exoutT = dscr("exoutT", [256, 16], BF16, True)
    COS = dscr("COS", [128, T], F32, True)
    SIN = dscr("SIN", [128, T], F32, True)
    QT = dscr("QT", [8, 128, T], BF16)
    Gs = dscr("Gs", [24, 128, T], BF16)
    YA = dscr("YA", [8, 128, T], BF16)
    YC = dscr("YC", [4, 128, T], BF16)
    YM = dscr("YM", [4, 128, T], BF16)
    X1 = dscr("X1", [T, D], F32)
    X1T = dscr("X1T", [8, 128, T], BF16)
    XRES = dscr("XRES", [T, D], F32)
    WTD = dscr("WTD", [16, T], BF16)
    bK = [Buf(f"exinK{i}") for i in range(4)]
    bV = [Buf(f"exinV{i}") for i in range(4)]
    boK = [Buf() for i in range(4)]
    boV = [Buf() for i in range(4)]
    bT, boT = Buf(), Buf()
    bCOS, bSIN = Buf(), Buf()
    bQT = [Buf() for i in range(8)]
    bG = [Buf() for i in range(24)]
    bYA = [Buf() for i in range(8)]
    bYC = [Buf() for i in range(4)]
    bYM = [Buf() for i in range(4)]
    bX1 = [Buf() for i in range(NT)]
    bX1T = [Buf() for i in range(NT)]
    bXR = [Buf() for i in range(NT)]
    bWTD = Buf()

    ps = nc.alloc_psum_tensor("ps", [128, 8, 512], F32)
    pb = [Buf(f"ps{i}") for i in range(8)]
    psT = ps[:, 7, :].bitcast(BF16)
    psT3 = psT.rearrange("p (k t) -> p k t", k=8)

    cbf = B.pers([128, 5, 128], BF16, "cbf")
    ident = cbf.t[:, 0, :]
    Rm = cbf.t[:, 1, :]
    tri2 = B.pers([128, 2, 128], BF16, "tri2")
    ones = cbf.t[:, 3, :]
    sel = B.pers([16, 16 * 128], BF16, "sel")
    ccs = B.pers([128, 16], F32, "ccs")
    invf = B.pers([128, 1], F32, "invf")
    wT = B.pers([16, T], BF16, "wT")
    lamt = B.pers([128, 8], F32, "lam")
    gsc = B.pers([128, 128], F32, "gsc")
    cw = B.pers([128, 3, 4], F32, "cw")
    rbias = B.pers([128, 16], F32, "rbias")
    wrt = B.pers([128, 8, 16], BF16, "wrt")
    KmT = B.pers([128, 4, 256], BF16, "KmT")
    Vm = B.pers([128, 2, 512], BF16, "Vm")

    B.dma('sp', cbf.t[:], cbf_in, W=[cbf.b])
    B.dma('sp', tri2.t[:, 0, :], cbf_in[:, 2, :], Wp=[tri2.b])
    B.dma('sp', tri2.t[:, 1, :], cbf_in[:, 2, :], Wp=[tri2.b])
    B.dma('sp', sel.t[:], sel_in, W=[sel.b])
    B.dma('sp', ccs.t[:], cc_in, W=[ccs.b])
    B.dma('sp', invf.t[:], invf_in, W=[invf.b])
    B.dma('sp', rbias.t[:], bcast_rows(rb_in, 128), W=[rbias.b])
    B.dma('pool', wrt.t[:], wr_in.rearrange("(kc p) n -> p kc n", p=128), W=[wrt.b])

    posi = B.tile([128, T], I32, "posi")
    ang = B.tile([128, T], F32, "ang")
    tmp = B.tile([128, T], F32, "tmp")
    tab = B.tile([128, T], F32, "tab")
    B.dma('sp', posi.t[:], bcast_rows(pos_in, 128), W=[posi.b])
    B.cp('dve', ang.t[:], posi.t[:], R=[posi.b], W=[ang.b])
    B.ts('dve', ang.t[:], ang.t[:], invf.t[:, 0:1], 1.0 / (2 * math.pi), ALU.mult, ALU.mult, R=[ang.b, invf.b], W=[ang.b])
    for (dst, bdst, off) in ((SIN, bSIN, 0.0), (COS, bCOS, 0.25)):
        B.ts('dve', tmp.t[:], ang.t[:], off, None, ALU.add, None, R=[ang.b], W=[tmp.b])
        B.cp('dve', posi.t[:], tmp.t[:], R=[tmp.b], W=[posi.b])
        B.cp('dve', tab.t[:], posi.t[:], R=[posi.b], W=[tab.b])
        B.tt('dve', tmp.t[:], tmp.t[:], tab.t[:], ALU.subtract, R=[tmp.b, tab.b], W=[tmp.b])
        B.act(tab.t[:], tmp.t[:], AF.Sin, R=[tmp.b], W=[tab.b], scale=6.28318)
        B.dma('sp', dst, tab.t[:], R=[tab.b], W=[bdst])
    B.phase_end()
    if stages <= 0.1:
        nlayers = 0

    for l in range(nlayers):
        lam_init = 0.8 - 0.6 * math.exp(-0.3 * l)
        x_src = x_in if l == 0 else XRES
        bxs = [Buf() for _ in range(NT)] if l == 0 else bXR
        y_dst = y_out if l == nlayers - 1 else XRES
        by = [Buf() for _ in range(NT)] if l == nlayers - 1 else bXR

        lt = B.tile([128, 4, 64], F32, "lt")
        for i, a in enumerate((lq1, lk1, lq2, lk2)):
            B.dma('sp', lt.t[:, i, :], bcast_rows(a[l:l + 1, :], 128), Wp=[lt.b])
        lp = B.tile([128, 2, 64], F32, "lp")
        lsum = B.tile([128, 4], F32, "lsum")
        B.tt('dve', lp.t[:, 0, :], lt.t[:, 0, :], lt.t[:, 1, :], ALU.mult, R=[lt.b], Wp=[lp.b])
        B.tt('dve', lp.t[:, 1, :], lt.t[:, 2, :], lt.t[:, 3, :], ALU.mult, R=[lt.b], Wp=[lp.b])
        B.red(lsum.t[:, 0:2], lp.t[:], R=[lp.b], W=[lsum.b])
        B.act(lsum.t[:, 2:4], lsum.t[:, 0:2], AF.Exp, R=[lsum.b], W=[lsum.b])
        B.tt('dve', lamt.t[:, 0:1], lsum.t[:, 3:4], lsum.t[:, 2:3], ALU.subtract, R=[lsum.b], W=[lamt.b])
        B.ts('dve', lamt.t[:, 0:1], lamt.t[:, 0:1], -lam_init, None, ALU.add, None, R=[lamt.b], W=[lamt.b])
        B.dma('sp', gsc.t[:], bcast_rows(subg[l:l + 1, :], 128), W=[gsc.b])
        B.ts('dve', gsc.t[:], gsc.t[:], 1.0 - lam_init, None, ALU.mult, None, R=[gsc.b], W=[gsc.b])
        for tap in range(3):
            B.dma('sp', cw.t[:, tap, :], convw[l, tap, :].rearrange("(c p) -> p c", p=128), Wp=[cw.b], slow=True)

        memT = B.tile([128, 8, 256], BF16, "memT")
        wkvs = B.tile([128, 8, 1024], BF16, "wkvs")
        B.dma('pool', wkvs.t[:], wkv[l].rearrange("(kc p) n -> p kc n", p=128), W=[wkvs.b])
        mf = B.tile([128, D], F32, "mf")
        mb = B.tile([128, D], BF16, "mb")
        for mc in range(2):
            B.dma('sp', mf.t[:], mem_in[mc * 128:(mc + 1) * 128, :], W=[mf.b])
            B.cp('dve', mb.t[:], mf.t[:], R=[mf.b], W=[mb.b])
            for kc in range(8):
                B.tr(psT[:, kc * 128:(kc + 1) * 128], mb.t[:, kc * 128:(kc + 1) * 128], ident, R=[mb.b, cbf.b], Wp=pb[7])
            B.cp('dve', memT.t[:, :, mc * 128:(mc + 1) * 128], psT3, R=[pb[7]], Wp=[memT.b])
        for hd in range(4):
            for kc in range(8):
                B.mm(ps[:, 0, 0:256], wkvs.t[:, kc, hd * 128:(hd + 1) * 128], memT.t[:, kc, :], kc == 0, kc == 7, R=[wkvs.b, memT.b], Wp=pb[0])
            B.cp('dve', KmT.t[:, hd, :], ps[:, 0, 0:256], R=[pb[0]], Wp=[KmT.b])
        for mc in range(2):
            for kc in range(8):
                B.mm(ps[:, 1, :], memT.t[:, kc, mc * 128:(mc + 1) * 128], wkvs.t[:, kc, 512:1024], kc == 0, kc == 7, R=[wkvs.b, memT.b], Wp=pb[1])
            B.cp('dve', Vm.t[:, mc, :], ps[:, 1, :], R=[pb[1]], Wp=[Vm.b])
        B.phase_end()
        if stages <= 0.2:
            continue

        xT = B.tile([128, 8, T], BF16, "xT")
        bxT = [Buf() for _ in range(NT)]
        m0 = B.mark()
        xf = [B.tile([128, D], F32, "xf") for _ in range(2)]
        xb = [B.tile([128, D], BF16, "xb") for _ in range(2)]
        for blk in range(NB):
            i = blk % 2
            B.dma('sp', xf[i].t[:], x_src[blk * 128:(blk + 1) * 128, :], R=[bxs[blk // 4]], W=[xf[i].b])
            B.cp('act', xb[i].t[:], xf[i].t[:], R=[xf[i].b], W=[xb[i].b])
            for kc in range(8):
                B.tr(psT[:, kc * 128:(kc + 1) * 128], xb[i].t[:, kc * 128:(kc + 1) * 128], ident, R=[xb[i].b, cbf.b], Wp=pb[7])
            B.cp('dve', xT.t[:, :, blk * 128:(blk + 1) * 128], psT3, R=[pb[7]], Wp=[bxT[blk // 4]])
        B.release(m0)
        if stages <= 0.3:
            B.phase_end()
            continue

        NSLAB = 4
        slabs = [B.tile([128, 8, 512], BF16, f"slab{i}") for i in range(NSLAB)]
        plan = [COL_K, COL_K + 512, COL_V, COL_V + 512, COL_CC, COL_CU, COL_Q, COL_Q + 512, COL_QM] + \
               [COL_G + i * 512 for i in range(6)] + [COL_CB, COL_CC, COL_CU]
        if stages <= 1:
            plan = plan[:6]
        st_ = {'issued': 0, 'taken': 0}

        def issue_slab():
            n = st_['issued']
            if n >= len(plan):
                return
            s = slabs[n % NSLAB]
            c0 = plan[n]
            B.dma('pool', s.t[:], w_in[l, :, c0:c0 + 512].rearrange("(kc p) n -> p kc n", p=128), W=[s.b])
            st_['issued'] += 1

        def next_slab(c0):
            n = st_['taken']
            assert plan[n] == c0, (plan[n], c0)
            while st_['issued'] <= n + 1:
                if st_['issued'] >= len(plan):
                    break
                issue_slab()
            st_['taken'] += 1
            return slabs[n % NSLAB]

        pctr = [0]

        def proj_fm(s, j, t):
            bi = pctr[0] % 4
            pctr[0] += 1
            for kc in range(8):
                B.mm(ps[:, bi, :], s.t[:, kc, j * 128:(j + 1) * 128], xT.t[:, kc, t * 512:(t + 1) * 512], kc == 0, kc == 7,
                     R=[s.b, bxT[t]], Wp=pb[bi])
            return bi

        hbuf = [B.tile([128, T], BF16, "hbuf") for _ in range(2)]
        hctr = [0]

        def next_hb():
            hb = hbuf[hctr[0] % 2]
            hctr[0] += 1
            return hb

        ma_ = B.mark()
        cosT = B.tile([128, T], F32, "cos")
        sinT = B.tile([128, T], F32, "sin")
        B.dma('sp', cosT.t[:], COS, R=[bCOS], W=[cosT.b])
        B.dma('sp', sinT.t[:], SIN, R=[bSIN], W=[sinT.b])
        kb_ = [B.tile([128, 512], BF16, "kb") for _ in range(2)]
        t1_ = [B.tile([128, 512], F32, "t1") for _ in range(2)]
        t2_ = [B.tile([128, 512], F32, "t2") for _ in range(2)]
        rctr = [0]

        import os
        RH = int(os.environ.get("RH", "9"))

        def rope_head(s, j, hb):
            for t in range(NT):
                bi = proj_fm(s, j, t)
                r = rctr[0] % 2
                rctr[0] += 1
                if RH < 2:
                    continue
                B.cp('act', kb_[r].t[:], ps[:, bi, :], R=[pb[bi]], W=[kb_[r].b])
                B.mm(ps[:, 4 + r, :], Rm, kb_[r].t[:], True, True, R=[kb_[r].b, cbf.b], Wp=pb[4 + r])
                if RH < 3:
                    continue
                B.tt('dve', t1_[r].t[:], ps[:, bi, :], cosT.t[:, t * 512:(t + 1) * 512], ALU.mult, R=[pb[bi], cosT.b, kb_[r].b], W=[t1_[r].b])
                B.tt('dve', t2_[r].t[:], ps[:, 4 + r, :], sinT.t[:, t * 512:(t + 1) * 512], ALU.mult, R=[pb[4 + r], sinT.b], W=[t2_[r].b])
                B.tt('dve', hb.t[:, t * 512:(t + 1) * 512], t1_[r].t[:], t2_[r].t[:], ALU.add, R=[t1_[r].b, t2_[r].b], Wp=[hb.b])

        for sl in range(2):
            s = next_slab(COL_K + sl * 512)
            for j in range(4):
                hd = sl * 4 + j
                hb = next_hb()
                rope_head(s, j, hb)
                i, hh = hd // 2, hd % 2
                if RH >= 4:
                    B.dma('sp', exinK[i].rearrange("(h p a) c -> h p (a c)", h=2, a=4)[hh], hb.t[:], R=[hb.b], Wp=[bK[i]])
                if hh == 1:
                    B.cc(exinK[i], exoutK[i], R=[bK[i]], W=[boK[i]])
        if stages <= 0.5:
            B.phase_end()
            continue
        vt = [B.tile([128, 512], BF16, "vt") for _ in range(2)]
        vctr = 0
        for half in range(2):
            s = next_slab(COL_V + half * 512)
            for blk in range(NB):
                bi = pctr[0] % 4
                pctr[0] += 1
                for kc in range(8):
                    B.mm(ps[:, bi, :], xT.t[:, kc, blk * 128:(blk + 1) * 128], s.t[:, kc, :], kc == 0, kc == 7, R=[s.b, bxT[blk // 4]], Wp=pb[bi])
                v = vt[vctr % 2]
                vctr += 1
                B.cp('act', v.t[:], ps[:, bi, :], R=[pb[bi]], W=[v.b])
                for pr in range(2):
                    i = half * 2 + pr
                    dst = exinV[i].rearrange("(h tt) (tr d) -> h (tt tr) d", h=2, tr=8)[:, blk * 128:(blk + 1) * 128, :].rearrange("h t d -> t h d")
                    B.dma('sp', dst, v.t[:, pr * 256:(pr + 1) * 256].rearrange("p (h d) -> p h d", h=2), R=[v.b], Wp=[bV[i]])
            for pr in range(2):
                i = half * 2 + pr
                B.cc(exinV[i], exoutV[i], R=[bV[i]], W=[boV[i]])
        if stages <= 0.7:
            B.phase_end()
            continue
        s_cc = next_slab(COL_CC)
        s_cu = next_slab(COL_CU)
        tl = B.tile([128, 2, 4, 4], F32, "tl")
        tlb = B.tile([128, 4, 4], BF16, "tlb")
        xtail = xT.t[:, :, :].rearrange("p k (c t) -> p k c t", c=2)
        for which, s in enumerate((s_cc, s_cu)):
            for chc in range(4):
                c0 = (which * 4 + chc) * 4
                for kc in range(8):
                    B.mm(ps[:, 6, c0:c0 + 4].rearrange("p (c t) -> p c t", c=2),
                         s.t[:, kc, chc * 128:(chc + 1) * 128], xtail[:, kc, :, 2046:2048], kc == 0, kc == 7,
                         R=[s.b, bxT[3], bxT[7]], Wp=pb[6])
        B.cp('dve', tl.t[:].rearrange("p a b c -> p (a b c)"), ps[:, 6, 0:32], R=[pb[6]], W=[tl.b])
        B.tt('dve', tlb.t[:], tl.t[:, 0, :, :], tl.t[:, 1, :, :], ALU.mult, R=[tl.b], W=[tlb.b])
        B.dma('sp', exinT, tlb.t[:].rearrange("p a b -> p (a b)"), R=[tlb.b], W=[bT])
        B.cc(exinT, exoutT, R=[bT], W=[boT])
        if dbg:
            for i in range(4):
                B.dma('sp', dK[i], exinK[i], R=[bK[i]])
                B.dma('sp', dV[i], exinV[i], R=[bV[i]])
            B.dma('sp', dT, exinT, R=[bT])
            for i in range(4):
                B.dma('sp', doK[i], exoutK[i], R=[boK[i]])
                B.dma('sp', doV[i], exoutV[i], R=[boV[i]])
        if stages <= 1:
            B.phase_end()
            continue

        for sl in range(2):
            s = next_slab(COL_Q + sl * 512)
            for j in range(4):
                hd = sl * 4 + j
                hb = next_hb()
                rope_head(s, j, hb)
                B.dma('sp', QT[hd], hb.t[:], R=[hb.b], W=[bQT[hd]])
        B.release(ma_)

        s = next_slab(COL_QM)
        qmb = [B.tile([128, 512], BF16, "qmb") for _ in range(2)]
        pmt = [B.tile([128, 2, 512], BF16, "pmt") for _ in range(2)]
        rl = [B.tile([128, 512], F32, "rl") for _ in range(2)]
        mctr = 0
        for mh in range(4):
            hb = next_hb()
            for t in range(NT):
                bi = proj_fm(s, mh, t)
                r = mctr % 2
                mctr += 1
                B.cp('dve', qmb[r].t[:], ps[:, bi, :], R=[pb[bi]], W=[qmb[r].b])
                for mc in range(2):
                    B.mm(ps[:, 4 + mc, :], KmT.t[:, mh, mc * 128:(mc + 1) * 128], qmb[r].t[:], True, True, R=[KmT.b, qmb[r].b], Wp=pb[4 + mc])
                    B.act(pmt[r].t[:, mc, :], ps[:, 4 + mc, :], AF.Exp, R=[pb[4 + mc]], Wp=[pmt[r].b], scale=128 ** -0.5)
                for mc in range(2):
                    B.mm(ps[:, 6, :], Vm.t[:, mc, mh * 128:(mh + 1) * 128], pmt[r].t[:, mc, :], mc == 0, mc == 1, R=[Vm.b, pmt[r].b], Wp=pb[6])
                bl = pctr[0] % 4
                pctr[0] += 1
                for mc in range(2):
                    B.mm(ps[:, bl, :], ones, pmt[r].t[:, mc, :], mc == 0, mc == 1, R=[cbf.b, pmt[r].b], Wp=pb[bl])
                B.recip(rl[r].t[:], ps[:, bl, :], R=[pb[bl]], W=[rl[r].b])
                B.tt('dve', hb.t[:, t * 512:(t + 1) * 512], ps[:, 6, :], rl[r].t[:], ALU.mult, R=[pb[6], rl[r].b], Wp=[hb.b])
            B.dma('sp', YM[mh], hb.t[:], R=[hb.b], W=[bYM[mh]])
        B.release(ma_)

        for sl in range(6):
            s = next_slab(COL_G + sl * 512)
            for j in range(4):
                gi = sl * 4 + j
                hb = next_hb()
                for t in range(NT):
                    bi = proj_fm(s, j, t)
                    B.act(hb.t[:, t * 512:(t + 1) * 512], ps[:, bi, :], AF.Sigmoid, R=[pb[bi]], Wp=[hb.b])
                B.dma('sp', Gs[gi], hb.t[:], R=[hb.b], W=[bG[gi]])

        s_cb = next_slab(COL_CB)
        s_cc = next_slab(COL_CC)
        s_cu = next_slab(COL_CU)
        tg = B.tile([128, 2, 16], BF16, "tg")
        tgf = B.tile([128, 2, 16], F32, "tgf")
        for r in range(2):
            B.dma('sp', tg.t[:, r, :], exoutT[r * 128:(r + 1) * 128, :], R=[boT], Wp=[tg.b])
        B.cp('dve', tgf.t[:], tg.t[:], R=[tg.b], W=[tgf.b])
        halo = B.tile([128, 2, 4, 2], F32, "halo")
        for c in range(2):
            for k in range(4):
                r_, c_ = k // 2, k % 2
                src = tgf.t[:, r_, :].rearrange("p (a c t) -> p a c t", a=4, c=2)[:, :, c_, :]
                col = 4 + c * 4 + k
                if k == 0:
                    B.ts('dve', halo.t[:, c, :, :], src, ccs.t[:, col:col + 1], None, ALU.mult, None, R=[tgf.b, ccs.b], W=[halo.b] if c == 0 else (), Wp=[halo.b] if c == 1 else ())
                else:
                    B.stt('dve', halo.t[:, c, :, :], src, ccs.t[:, col:col + 1], halo.t[:, c, :, :], ALU.mult, ALU.add,
                          R=[tgf.b, ccs.b, halo.b], W=[halo.b])
        zrow = B.tile([128, 2, 2050], F32, "zrow")
        ccs_ = [B.tile([128, 512], F32, "ccsb") for _ in range(2)]
        yv = [B.tile([128, 512], F32, "yv") for _ in range(2)]
        cctr = 0
        for chc in range(4):
            hb = next_hb()
            for c in range(2):
                B.cp('dve', zrow.t[:, c, 0:2], halo.t[:, c, chc, :], R=[halo.b], Wp=[zrow.b])
            for t in range(NT):
                c, tt_ = t // 4, t % 4
                bc = proj_fm(s_cc, chc, t)
                r = cctr % 2
                cctr += 1
                B.cp('act', ccs_[r].t[:], ps[:, bc, :], R=[pb[bc]], W=[ccs_[r].b])
                bu = proj_fm(s_cu, chc, t)
                B.tt('dve', zrow.t[:, c, 2 + tt_ * 512:2 + (tt_ + 1) * 512], ps[:, bu, :], ccs_[r].t[:], ALU.mult, R=[pb[bu], ccs_[r].b], Wp=[zrow.b])
            for t in range(NT):
                c, tt_ = t // 4, t % 4
                r = cctr % 2
                cctr += 1
                bb = proj_fm(s_cb, chc, t)
                B.ts('dve', yv[r].t[:], zrow.t[:, c, tt_ * 512:tt_ * 512 + 512], cw.t[:, 0, chc:chc + 1], None, ALU.mult, None, R=[zrow.b, cw.b], W=[yv[r].b])
                B.stt('dve', yv[r].t[:], zrow.t[:, c, tt_ * 512 + 1:tt_ * 512 + 513], cw.t[:, 1, chc:chc + 1], yv[r].t[:], ALU.mult, ALU.add, R=[zrow.b, cw.b, yv[r].b], W=[yv[r].b])
                B.stt('dve', yv[r].t[:], zrow.t[:, c, tt_ * 512 + 2:tt_ * 512 + 514], cw.t[:, 2, chc:chc + 1], yv[r].t[:], ALU.mult, ALU.add, R=[zrow.b, cw.b, yv[r].b], W=[yv[r].b])
                B.tt('dve', hb.t[:, t * 512:(t + 1) * 512], ps[:, bb, :], yv[r].t[:], ALU.mult, R=[pb[bb], yv[r].b], Wp=[hb.b])
            B.dma('sp', YC[chc], hb.t[:], R=[hb.b], W=[bYC[chc]])
        B.phase_end()
        if stages <= 2:
            continue

        NBUF = 2
        Kg = [B.tile([128, 3 * 2048], BF16, "Kg") for _ in range(NBUF)]
        Ko = [B.tile([128, T], BF16, "Ko") for _ in range(NBUF)]
        Vg = [B.tile([128, 48, 129], BF16, "Vg") for _ in range(NBUF)]
        Vo = [B.tile([128, 32, 129], BF16, "Vo") for _ in range(NBUF)]
        Qh = [B.tile([128, T], BF16, "Qh") for _ in range(NBUF)]
        yaT = [B.tile([128, T], BF16, "yaT") for _ in range(2)]
        for i in range(NBUF):
            B.memset('dve', Vg[i].t[:, :, 128:129], 1.0, Wp=[Vg[i].b])
            B.memset('dve', Vo[i].t[:, :, 128:129], 1.0, Wp=[Vo[i].b])
        ptile = [B.tile([128, 2, 2, 256], BF16, "pt") for _ in range(3)]
        osb = [B.tile([128, 128], F32, "osb") for _ in range(2)]
        osq = [B.tile([128, 128], F32, "osq") for _ in range(2)]
        ybf = [B.tile([128, 128], BF16, "ybf") for _ in range(2)]
        sm = [B.tile([128, 8], F32, "sm") for _ in range(2)]
        slots = [(0, 0), (1, 0), (1, 1)]

        def load_head(hd):
            i, hh = hd // 2, hd % 2
            bfi = hd % NBUF
            kg, ko, vg, vo, qh = Kg[bfi], Ko[bfi], Vg[bfi], Vo[bfi], Qh[bfi]
            gK = exoutK[i].rearrange("(r h p a) c -> r h p (a c)", r=2, h=2, a=4)
            gV = exoutV[i].rearrange("(r h tt) (tr d) -> r h (tt tr) d", r=2, h=2, tr=8)
            for si, (rk, ch) in enumerate(slots):
                B.dma('sp', kg.t[:, si * 2048:(si + 1) * 2048], gK[rk, hh, :, ch * 2048:(ch + 1) * 2048], R=[boK[i]], Wp=[kg.b])
                B.dma('sp', vg.t[:, si * 16:(si + 1) * 16, 0:128],
                      gV[rk, hh, ch * 2048:(ch + 1) * 2048, :].rearrange("(n p) d -> p n d", p=128), R=[boV[i]], Wp=[vg.b])
            B.dma('sp', ko.t[:], exinK[i].rearrange("(h p a) c -> h p (a c)", h=2, a=4)[hh], R=[bK[i]], W=[ko.b])
            B.dma('sp', vo.t[:, :, 0:128], exinV[i].rearrange("(h tt) (tr d) -> h (tt tr) d", h=2, tr=8)[hh].rearrange("(n p) d -> p n d", p=128),
                  R=[bV[i]], Wp=[vo.b])
            B.dma('sp', qh.t[:], QT[hd], R=[bQT[hd]], W=[qh.b])

        oc = [[B.tile([128, 258], F32, "oc") for _ in range(2)] for _ in range(2)]
        ybf4 = [B.tile([128, 128], BF16, "ybf4") for _ in range(4)]
        stepctr = 0
        cmbctr = 0
        qtctr = 0
        TR_DELAY = 6
        load_head(0)
        for hd in range(8):
            if hd + 1 < 8:
                load_head(hd + 1)
            bfi = hd % NBUF
            kg, ko, vg, vo, qh = Kg[bfi], Ko[bfi], Vg[bfi], Vo[bfi], Qh[bfi]
            ya = yaT[hd % 2]
            items = []
            for c in range(2):
                pref = [(0, 0)] if c == 0 else [(0, 1), (1, 2), (2, 3)]
                for qt in range(8):
                    q0 = c * 2048 + qt * 256
                    steps = []
                    for (si, bcol) in pref:
                        for kb in range(16):
                            steps.append((kg.t[:, si * 2048 + kb * 128: si * 2048 + (kb + 1) * 128], vg.t[:, si * 16 + kb, :], bcol, None, kg.b, vg.b))
                    for kb in range(2 * qt + 2):
                        mk = None
                        if kb == 2 * qt:
                            mk = 0
                        elif kb == 2 * qt + 1:
                            mk = 1
                        steps.append((ko.t[:, c * 2048 + kb * 128: c * 2048 + (kb + 1) * 128], vo.t[:, c * 16 + kb, :], None, mk, ko.b, vo.b))
                    nsteps = len(steps)
                    assert nsteps % 2 == 0
                    for pidx in range(nsteps // 2):
                        items.append({'q0': q0, 'pidx': pidx, 'np': nsteps // 2, 'nsteps': nsteps, 's': (steps[2 * pidx], steps[2 * pidx + 1])})

            def emit_scores(it):
                nonlocal stepctr
                pc = stepctr
                stepctr += 1
                pt = ptile[pc % 3]
                bA = 2 * (pc % 2)
                it['pt'] = pt
                q0 = it['q0']
                bcol = it['s'][0][2]
                assert it['s'][1][2] == bcol
                for j in range(2):
                    kap, vap, _, mk, kbuf, vbuf = it['s'][j]
                    for comp in range(2):
                        B.mm(ps[:, bA + comp, j * 256:(j + 1) * 256], kap[comp * 64:(comp + 1) * 64, :],
                             qh.t[comp * 64:(comp + 1) * 64, q0:q0 + 256], True, True, R=[kbuf, qh.b], Wp=pb[bA + comp])
                bias = 0.0 if bcol is None else ccs.t[:, bcol:bcol + 1]
                if True:
                    B.act(pt.t[:].rearrange("p c j q -> p (c j q)"), ps[:, bA:bA + 2, :].rearrange("p c n -> p (c n)"), AF.Exp,
                          R=[pb[bA], pb[bA + 1], ccs.b], W=[pt.b], bias=bias, scale=0.125)
                else:
                    for comp in range(2):
                        B.act(pt.t[:, comp, :, :].rearrange("p j q -> p (j q)"), ps[:, bA + comp, :], AF.Exp, R=[pb[bA + comp], ccs.b],
                              Wp=[pt.b], bias=bias, scale=0.125)

            pending = []

            def emit_pv(it, now):
                nonlocal cmbctr, qtctr
                pt = it['pt']
                q0 = it['q0']
                pidx, nsteps = it['pidx'], it['nsteps']
                if pidx == 0:
                    for comp in range(2):
                        B.mm(ps[:, 4 + comp, 0:258], cbf.t[:, 4, :], cbf.t[:, 0:3, :].rearrange("p a b -> p (a b)")[:, 0:258], True, False,
                             R=[cbf.b], Wp=pb[4 + comp])
                for j in range(2):
                    mk = it['s'][j][3]
                    if mk is not None:
                        B.tt('dve', pt.t[:, :, j, mk * 128:(mk + 1) * 128], pt.t[:, :, j, mk * 128:(mk + 1) * 128], tri2.t[:], ALU.mult,
                             R=[pt.b, tri2.b], W=[pt.b])
                for j in range(2):
                    kap, vap, _, mk, kbuf, vbuf = it['s'][j]
                    sidx = 2 * pidx + j
                    for comp in range(2):
                        for sub in range(2):
                            if mk == 1 and sub == 0:
                                continue
                            last = (sidx == nsteps - 1) if sub == 1 else (sidx == nsteps - 2)
                            B.mm(ps[:, 4 + comp, sub * 129:(sub + 1) * 129], pt.t[:, comp, j, sub * 128:(sub + 1) * 128], vap, False, last,
                                 R=[pt.b, vbuf], Wp=pb[4 + comp])
                if pidx != it['np'] - 1:
                    return
                par = qtctr % 2
                qtctr += 1
                o_ = oc[par]
                for comp in range(2):
                    B.cp('dve', o_[comp].t[:], ps[:, 4 + comp, 0:258], R=[pb[4 + comp]], W=[o_[comp].b])
                for sub in range(2):
                    k = cmbctr % 2
                    cmbctr += 1
                    o1 = o_[0].t[:, sub * 129:(sub + 1) * 129]
                    o2 = o_[1].t[:, sub * 129:(sub + 1) * 129]
                    b1, b2 = o_[0].b, o_[1].b
                    s_ = sm[k]
                    yb = ybf4[par * 2 + sub]
                    B.recip(s_.t[:, 0:1], o1[:, 128:129], R=[b1], W=[s_.b])
                    B.recip(s_.t[:, 1:2], o2[:, 128:129], R=[b2, s_.b], W=[s_.b])
                    B.tt('dve', s_.t[:, 2:3], s_.t[:, 1:2], lamt.t[:, 0:1], ALU.mult, R=[s_.b, lamt.b], W=[s_.b])
                    B.ts('dve', osb[k].t[:], o1[:, 0:128], s_.t[:, 0:1], None, ALU.mult, None, R=[b1, s_.b], W=[osb[k].b])
                    B.stt('dve', osb[k].t[:], o2[:, 0:128], s_.t[:, 2:3], osb[k].t[:], ALU.mult, ALU.add, R=[b2, s_.b, osb[k].b], W=[osb[k].b])
                    B.tt('dve', osq[k].t[:], osb[k].t[:], osb[k].t[:], ALU.mult, R=[osb[k].b], W=[osq[k].b])
                    B.red(s_.t[:, 3:4], osq[k].t[:], R=[osq[k].b, s_.b], W=[s_.b])
                    B.ts('dve', s_.t[:, 4:5], s_.t[:, 3:4], 1.0 / 128, RMS_EPS, ALU.mult, ALU.add, R=[s_.b], W=[s_.b])
                    pending.append([now + 3, 1, yb, par * 2 + sub, q0 + sub * 128, s_, osb[k]])

            def flush_tr(now, force=False):
                while pending and (force or pending[0][0] <= now):
                    ent = pending.pop(0)
                    _, stage, yb, kk, col, s_, ob = ent
                    if stage == 1:
                        B.act(s_.t[:, 6:7], s_.t[:, 4:5], AF.Ln, R=[s_.b], W=[s_.b])
                        B.act(s_.t[:, 5:6], s_.t[:, 6:7], AF.Exp, R=[s_.b], W=[s_.b], scale=-0.5)
                        B.stt('dve', yb.t[:], ob.t[:], s_.t[:, 5:6], gsc.t[:], ALU.mult, ALU.mult, R=[ob.b, s_.b, gsc.b], W=[yb.b])
                        ent[0] = now + 3
                        ent[1] = 2
                        pending.append(ent)
                        pending.sort(key=lambda e: e[0])
                    else:
                        B.tr(psT[:, kk * 128:(kk + 1) * 128], yb.t[:], ident, R=[yb.b, cbf.b], Wp=pb[7])
                        B.cp('dve', ya.t[:, col:col + 128], psT[:, kk * 128:(kk + 1) * 128], R=[pb[7]], Wp=[ya.b])

            n_it = len(items)
            for i in range(n_it + 1):
                if i < n_it:
                    emit_scores(items[i])
                if i >= 1:
                    emit_pv(items[i - 1], i)
                flush_tr(i)
            flush_tr(0, force=True)
            B.dma('sp', YA[hd], ya.t[:], R=[ya.b], W=[bYA[hd]])
        B.phase_end()
        if stages <= 3:
            continue

        wba_s = B.tile([128, 8, D], BF16, "wba")
        wbc_s = B.tile([128, 4, D], BF16, "wbc")
        wbm_s = B.tile([128, 4, D], BF16, "wbm")
        wo_s = B.tile([128, 8, D], BF16, "wo")
        B.dma('pool', wba_s.t[:], wba[l].rearrange("(kc p) n -> p kc n", p=128), W=[wba_s.b])
        B.dma('pool', wbc_s.t[:], wbc[l].rearrange("(kc p) n -> p kc n", p=128), W=[wbc_s.b])
        B.dma('pool', wbm_s.t[:], wbm[l].rearrange("(kc p) n -> p kc n", p=128), W=[wbm_s.b])
        B.dma('pool', wo_s.t[:], wout[l].rearrange("(kc p) n -> p kc n", p=128), W=[wo_s.b])
        lng = B.tile([128, D], F32, "lng")
        lnb = B.tile([128, D], F32, "lnb")
        B.dma('sp', lng.t[:], bcast_rows(ln1g[l:l + 1, :], 128), W=[lng.b])
        B.dma('sp', lnb.t[:], bcast_rows(ln1b[l:l + 1, :], 128), W=[lnb.b])
        yat = [B.tile([128, 8, 512], BF16, "yat") for _ in range(2)]
        yct = [B.tile([128, 4, 512], BF16, "yct") for _ in range(2)]
        ymt = [B.tile([128, 4, 512], BF16, "ymt") for _ in range(2)]
        gt = [B.tile([128, 3, 512], BF16, "gt") for _ in range(2)]
        mT = [B.tile([128, 8, 512], BF16, "mT") for _ in range(2)]
        ma = [B.tile([128, 512], F32, "ma") for _ in range(2)]
        mb2 = [B.tile([128, 512], F32, "mb2") for _ in range(2)]
        xr = [B.tile([128, D], F32, "xr") for _ in range(2)]
        rr = [B.tile([128, D], F32, "rr") for _ in range(2)]
        x1b = [B.tile([128, D], BF16, "x1b") for _ in range(2)]
        x1T = [B.tile([128, 8, 512], BF16, "x1T") for _ in range(2)]
        st = [B.tile([128, 16], F32, "st") for _ in range(2)]
        rt = [B.tile([128, 16, 8], F32, "rt") for _ in range(2)]
        wtb = [B.tile([128, 16], BF16, "wtb") for _ in range(2)]

        def layer_norm(r_, g_, b_, s_):
            P.add('dve', lambda e: e.bn_stats(out=s_.t[:, 0:6], in_=r_.t[:, 0:512]), reads=[r_.b], writes=[s_.b])
            P.add('dve', lambda e: e.bn_stats(out=s_.t[:, 6:12], in_=r_.t[:, 512:1024]), reads=[r_.b, s_.b], writes=[s_.b])
            P.add('dve', lambda e: e.bn_aggr(out=s_.t[:, 12:14], in_=s_.t[:, 0:12]), reads=[s_.b], writes=[s_.b])
            B.ts('dve', s_.t[:, 14:15], s_.t[:, 13:14], LN_EPS, None, ALU.add, None, R=[s_.b], W=[s_.b])
            B.act(s_.t[:, 15:16], s_.t[:, 14:15], AF.Ln, R=[s_.b], W=[s_.b])
            B.act(s_.t[:, 14:15], s_.t[:, 15:16], AF.Exp, R=[s_.b], W=[s_.b], scale=-0.5)
            B.ts('dve', r_.t[:], r_.t[:], s_.t[:, 12:13], s_.t[:, 14:15], ALU.subtract, ALU.mult, R=[r_.b, s_.b], W=[r_.b])
            B.tt('dve', r_.t[:], r_.t[:], g_.t[:], ALU.mult, R=[r_.b, g_.b], W=[r_.b])
            B.tt('dve', r_.t[:], r_.t[:], b_.t[:], ALU.add, R=[r_.b, b_.b], W=[r_.b])

        gctr2 = 0
        for t in range(NT):
            k = t % 2
            for hd in range(8):
                B.dma('sp', yat[k].t[:, hd, :], YA[hd, :, t * 512:(t + 1) * 512], R=[bYA[hd]], Wp=[yat[k].b])
            for c4 in range(4):
                B.dma('sp', yct[k].t[:, c4, :], YC[c4, :, t * 512:(t + 1) * 512], R=[bYC[c4]], Wp=[yct[k].b])
                B.dma('sp', ymt[k].t[:, c4, :], YM[c4, :, t * 512:(t + 1) * 512], R=[bYM[c4]], Wp=[ymt[k].b])
            for j in range(8):
                jj = j % 2
                g_ = gt[gctr2 % 2]
                gctr2 += 1
                for br in range(3):
                    B.dma('sp', g_.t[:, br, :], Gs[br * 8 + j, :, t * 512:(t + 1) * 512], R=[bG[br * 8 + j]], Wp=[g_.b])
                for kc in range(8):
                    B.mm(ps[:, 0, :], wba_s.t[:, kc, j * 128:(j + 1) * 128], yat[k].t[:, kc, :], kc == 0, kc == 7, R=[wba_s.b, yat[k].b], Wp=pb[0])
                for kc in range(4):
                    B.mm(ps[:, 1, :], wbc_s.t[:, kc, j * 128:(j + 1) * 128], yct[k].t[:, kc, :], kc == 0, kc == 3, R=[wbc_s.b, yct[k].b], Wp=pb[1])
                for kc in range(4):
                    B.mm(ps[:, 2, :], wbm_s.t[:, kc, j * 128:(j + 1) * 128], ymt[k].t[:, kc, :], kc == 0, kc == 3, R=[wbm_s.b, ymt[k].b], Wp=pb[2])
                B.tt('dve', ma[jj].t[:], ps[:, 0, :], g_.t[:, 0, :], ALU.mult, R=[pb[0], g_.b], W=[ma[jj].b])
                B.tt('dve', mb2[jj].t[:], ps[:, 1, :], g_.t[:, 1, :], ALU.mult, R=[pb[1], g_.b], W=[mb2[jj].b])
                B.tt('dve', ma[jj].t[:], ma[jj].t[:], mb2[jj].t[:], ALU.add, R=[ma[jj].b, mb2[jj].b], W=[ma[jj].b])
                B.tt('dve', mb2[jj].t[:], ps[:, 2, :], g_.t[:, 2, :], ALU.mult, R=[pb[2], g_.b], W=[mb2[jj].b])
                B.tt('dve', mT[k].t[:, j, :], ma[jj].t[:], mb2[jj].t[:], ALU.add, R=[ma[jj].b, mb2[jj].b], Wp=[mT[k].b])
            for b4 in range(4):
                blk = t * 4 + b4
                kk = blk % 2
                B.dma('sp', xr[kk].t[:], x_src[blk * 128:(blk + 1) * 128, :], R=[bxs[t]], W=[xr[kk].b])
                for half in range(2):
                    bank = 3 + half
                    for kc in range(8):
                        B.mm(ps[:, bank, :], mT[k].t[:, kc, b4 * 128:(b4 + 1) * 128], wo_s.t[:, kc, half * 512:(half + 1) * 512], kc == 0, kc == 7,
                             R=[mT[k].b, wo_s.b], Wp=pb[bank])
                    B.stt('dve', rr[kk].t[:, half * 512:(half + 1) * 512], xr[kk].t[:, half * 512:(half + 1) * 512], ALPHA, ps[:, bank, :],
                          ALU.mult, ALU.add, R=[xr[kk].b, pb[bank]], Wp=[rr[kk].b])
                layer_norm(rr[kk], lng, lnb, st[kk])
                B.dma('sp', X1[blk * 128:(blk + 1) * 128, :], rr[kk].t[:], R=[rr[kk].b], Wp=[bX1[t]])
                B.cp('act', x1b[kk].t[:], rr[kk].t[:], R=[rr[kk].b], W=[x1b[kk].b])
                for kc in range(8):
                    B.tr(psT[:, kc * 128:(kc + 1) * 128], x1b[kk].t[:, kc * 128:(kc + 1) * 128], ident, R=[x1b[kk].b, cbf.b], Wp=pb[7])
                B.cp('dve', x1T[k].t[:, :, b4 * 128:(b4 + 1) * 128], psT3, R=[pb[7]], Wp=[x1T[k].b])
                for kc in range(8):
                    B.mm(ps[:, 5, 0:16], x1T[k].t[:, kc, b4 * 128:(b4 + 1) * 128], wrt.t[:, kc, :], kc == 0, kc == 7, R=[x1T[k].b, wrt.b], Wp=pb[5])
                R_ = rt[kk]
                rb_ = R_.b
                col = lambda j: R_.t[:, :, j]
                g44 = lambda ap: ap.rearrange("p (g e) -> p g e", g=4)
                B.tt('dve', col(0), ps[:, 5, 0:16], rbias.t[:], ALU.add, R=[pb[5], rbias.b], W=[rb_])
                B.rmax(R_.t[:, 0:4, 1], g44(col(0)), R=[rb_], W=[rb_])
                B.rmax(R_.t[:, 4:5, 1], R_.t[:, 0:4, 1], R=[rb_], W=[rb_])
                B.ts('dve', R_.t[:, 0:4, 2], R_.t[:, 0:4, 1], R_.t[:, 4:5, 1], NEG, ALU.is_lt, ALU.mult, R=[rb_], W=[rb_])
                for g in range(4):
                    B.ts('dve', R_.t[:, g * 4:(g + 1) * 4, 3], R_.t[:, g * 4:(g + 1) * 4, 0], R_.t[:, g:g + 1, 2], None, ALU.add, None, R=[rb_], W=[rb_])
                B.rmax(R_.t[:, 5:6, 1], col(3), R=[rb_], W=[rb_])
                B.ts('dve', col(4), col(3), R_.t[:, 5:6, 1], NEG, ALU.is_ge, ALU.mult, R=[rb_], W=[rb_])
                B.tt('dve', col(4), col(4), col(3), ALU.add, R=[rb_], W=[rb_])
                B.rmax(R_.t[:, 6:7, 1], col(4), R=[rb_], W=[rb_])
                B.ts('dve', col(5), col(3), R_.t[:, 6:7, 1], None, ALU.is_ge, None, R=[rb_], W=[rb_])
                B.ts('dve', col(6), col(3), R_.t[:, 5:6, 1], -80.0, ALU.subtract, ALU.max, R=[rb_], W=[rb_])
                B.act(col(7), col(6), AF.Exp, R=[rb_], W=[rb_])
                B.tt('dve', col(7), col(7), col(5), ALU.mult, R=[rb_], W=[rb_])
                B.red(R_.t[:, 7:8, 1], col(7), R=[rb_], W=[rb_])
                B.recip(R_.t[:, 8:9, 1], R_.t[:, 7:8, 1], R=[rb_], W=[rb_])
                B.ts('dve', wtb[kk].t[:], col(7), R_.t[:, 8:9, 1], None, ALU.mult, None, R=[rb_], W=[wtb[kk].b])
                B.mm(ps[0:16, 6, 0:128], wtb[kk].t[:], ident, True, True, R=[wtb[kk].b, cbf.b], Wp=pb[6])
                B.cp('dve', wT.t[:, blk * 128:(blk + 1) * 128], ps[0:16, 6, 0:128], R=[pb[6]], Wp=[wT.b])
            for kc in range(8):
                B.dma('sp', X1T[kc, :, t * 512:(t + 1) * 512], x1T[k].t[:, kc, :], R=[x1T[k].b], Wp=[bX1T[t]])
        if dbg:
            B.dma('sp', WTD, wT.t[:], R=[wT.b], W=[bWTD])
        B.phase_end()
        if stages <= 4:
            continue

        lng2 = B.tile([128, D], F32, "lng2")
        lnb2 = B.tile([128, D], F32, "lnb2")
        B.dma('sp', lng2.t[:], bcast_rows(ln2g[l:l + 1, :], 128), W=[lng2.b])
        B.dma('sp', lnb2.t[:], bcast_rows(ln2b[l:l + 1, :], 128), W=[lnb2.b])
        NPT = 2048
        xp = B.tile([128, 8, NPT], BF16, "xp")
        facc = B.tile([128, 16, D], F32, "facc")
        bfacc = [Buf() for _ in range(16)]
        wg_ = [B.tile([128, 8, 512], BF16, "wg") for _ in range(2)]
        wu_ = [B.tile([128, 8, 512], BF16, "wu") for _ in range(2)]
        wd_ = [B.tile([128, 4, D], BF16, "wd") for _ in range(2)]
        hT = [B.tile([128, 4, 512], BF16, "hT") for _ in range(2)]
        wbs = [B.tile([128, 512], BF16, "wbs") for _ in range(2)]
        sg = [B.tile([128, 512], F32, "sg") for _ in range(2)]
        tu = [B.tile([128, 512], F32, "tu") for _ in range(2)]
        xr2 = [B.tile([128, D], F32, "xr2") for _ in range(2)]
        st2 = [B.tile([128, 16], F32, "st2") for _ in range(2)]
        gctr = 0
        dctr = 0
        hctr2 = 0
        prev_down = [None]
        for ps_ in range(T // NPT):
            tok0 = ps_ * NPT
            bxp = [Buf() for _ in range(4)]
            for tt_ in range(4):
                for kc in range(8):
                    B.dma('sp', xp.t[:, kc, tt_ * 512:(tt_ + 1) * 512], X1T[kc, :, tok0 + tt_ * 512: tok0 + (tt_ + 1) * 512], R=[bX1T[ps_ * 4 + tt_]], Wp=[bxp[tt_]])
            for ex in range(16):
                w = ex % 2
                B.dma('pool', wg_[w].t[:], weg[l, ex].rearrange("(kc p) n -> p kc n", p=128), W=[wg_[w].b])
                B.dma('pool', wu_[w].t[:], weu[l, ex].rearrange("(kc p) n -> p kc n", p=128), W=[wu_[w].b])
                B.dma('pool', wd_[w].t[:], wed[l, ex].rearrange("(kc p) n -> p kc n", p=128), W=[wd_[w].b])
                for tt_ in range(4):
                    hk = hctr2 % 2
                    hctr2 += 1
                    B.mm(ps[:, 6, :], sel.t[:, ex * 128:(ex + 1) * 128], wT.t[:, tok0 + tt_ * 512: tok0 + (tt_ + 1) * 512], True, True, R=[sel.b, wT.b], Wp=pb[6])
                    B.cp('act', wbs[hk].t[:], ps[:, 6, :], R=[pb[6]], W=[wbs[hk].b])
                    for fc in range(4):
                        g2 = gctr % 2
                        gctr += 1
                        bg, bu = g2 * 2, g2 * 2 + 1
                        for kc in range(8):
                            B.mm(ps[:, bg, :], wg_[w].t[:, kc, fc * 128:(fc + 1) * 128], xp.t[:, kc, tt_ * 512:(tt_ + 1) * 512], kc == 0, kc == 7,
                                 R=[wg_[w].b, bxp[tt_]], Wp=pb[bg])
                        for kc in range(8):
                            B.mm(ps[:, bu, :], wu_[w].t[:, kc, fc * 128:(fc + 1) * 128], xp.t[:, kc, tt_ * 512:(tt_ + 1) * 512], kc == 0, kc == 7,
                                 R=[wu_[w].b, bxp[tt_]], Wp=pb[bu])
                        B.act(sg[g2].t[:], ps[:, bg, :], AF.Silu, R=[pb[bg]], W=[sg[g2].b])
                        B.tt('dve', tu[g2].t[:], ps[:, bu, :], sg[g2].t[:], ALU.mult, R=[pb[bu], sg[g2].b], W=[tu[g2].b])
                        B.tt('dve', hT[hk].t[:, fc, :], tu[g2].t[:], wbs[hk].t[:], ALU.mult, R=[tu[g2].b, wbs[hk].b], Wp=[hT[hk].b])
                    def down(tt_=tt_, hk=hk, w=w, ex=ex):
                        nonlocal dctr
                        for b4 in range(4):
                            lb = tt_ * 4 + b4
                            for half in range(2):
                                bank = 4 + (dctr % 2)
                                dctr += 1
                                for fc in range(4):
                                    B.mm(ps[:, bank, :], hT[hk].t[:, fc, b4 * 128:(b4 + 1) * 128], wd_[w].t[:, fc, half * 512:(half + 1) * 512], fc == 0, fc == 3,
                                         R=[hT[hk].b, wd_[w].b], Wp=pb[bank])
                                dst = facc.t[:, lb, half * 512:(half + 1) * 512]
                                if ex == 0:
                                    B.cp('dve', dst, ps[:, bank, :], R=[pb[bank]], Wp=[bfacc[lb]])
                                else:
                                    B.tt('dve', dst, dst, ps[:, bank, :], ALU.add, R=[pb[bank], bfacc[lb]], Wp=[bfacc[lb]])
                    if prev_down[0] is not None:
                        prev_down[0]()
                    prev_down[0] = down
            if prev_down[0] is not None:
                prev_down[0]()
                prev_down[0] = None
            for lb in range(16):
                blk = ps_ * 16 + lb
                kk = lb % 2
                B.dma('sp', xr2[kk].t[:], X1[blk * 128:(blk + 1) * 128, :], R=[bX1[blk // 4]], W=[xr2[kk].b])
                B.stt('dve', xr2[kk].t[:], xr2[kk].t[:], ALPHA, facc.t[:, lb, :], ALU.mult, ALU.add, R=[xr2[kk].b, bfacc[lb]], W=[xr2[kk].b])
                layer_norm(xr2[kk], lng2, lnb2, st2[kk])
                B.dma('sp', y_dst[blk * 128:(blk + 1) * 128, :], xr2[kk].t[:], R=[xr2[kk].b], Wp=[by[blk // 4]])
        B.phase_end()

    P.barrier()
    print("nops", P.nops, {e: len(P.ops[e]) for e in ENGS})
    P.emit()
    return nc


def host_consts():
    ident = np.eye(128, dtype=np.float32)
    Rm = np.zeros((128, 128), np.float32)
    for m in range(128):
        if (m % 64) < 32:
            Rm[m + 32, m] = -1.0
        else:
            Rm[m - 32, m] = 1.0
    tri = (np.arange(128)[None, :] >= np.arange(128)[:, None]).astype(np.float32)
    ones = np.ones((128, 128), np.float32)
    cbf = np.stack([ident, Rm, tri, ones, np.zeros_like(ones)], axis=1).astype(ml_dtypes.bfloat16)
    sel = np.zeros((16, 16, 128), np.float32)
    for e in range(16):
        sel[e, e, :] = 1.0
    sel = sel.reshape(16, 16 * 128).astype(ml_dtypes.bfloat16)
    inv_freq = (10000.0 ** (-np.arange(0, 64, 2, dtype=np.float32) / 64)).astype(np.float32)
    invf = inv_freq[np.arange(128) % 32].reshape(128, 1).astype(np.float32)
    return cbf, sel, invf


def core_tokens(h):
    qa, qb = (0, 3) if h == 0 else (1, 2)
    return np.concatenate([np.arange(qa * 2048, (qa + 1) * 2048), np.arange(qb * 2048, (qb + 1) * 2048)])


def core_cc(h):
    cc = np.zeros((128, 16), np.float32)
    if h == 0:
        cc[:, 0] = NEG
        cc[:, 8 + 3] = 1.0
    else:
        cc[:, 3] = NEG
        cc[:, 4 + 0] = 1.0
        cc[:, 8 + 2] = 1.0
    return cc


_NC_CACHE = {}


def make_in_maps(inputs):
    cbf, sel, invf = host_consts()
    x = np.asarray(inputs['x'])
    mem = np.asarray(inputs['mem'])
    pos = np.asarray(inputs['positions'])
    shared = {}
    for k in ['w_in', 'lambda_q1', 'lambda_k1', 'lambda_q2', 'lambda_k2', 'diff_subln_g', 'conv_w', 'w_mem_kv',
              'w_br_attn', 'w_br_conv', 'w_br_mem', 'w_out', 'ln1_g', 'ln1_b', 'ln2_g', 'ln2_b', 'w_router',
              'w_exp_gate', 'w_exp_up', 'w_exp_down']:
        shared[k] = np.ascontiguousarray(np.asarray(inputs[k]), dtype=np.float32)
    shared['router_bias'] = np.ascontiguousarray(np.asarray(inputs['router_bias'], dtype=np.float32).reshape(1, 16))
    shared['cbf'] = cbf
    shared['sel'] = sel
    shared['invf'] = invf
    in_maps = []
    for c in range(8):
        b, h = c // 2, c % 2
        tok = core_tokens(h)
        m = dict(shared)
        m['x'] = np.ascontiguousarray(x[b, tok, :], dtype=np.float32)
        m['pos'] = np.ascontiguousarray(pos[b, tok].reshape(1, T).astype(np.int32))
        m['mem'] = np.ascontiguousarray(mem[b], dtype=np.float32)
        m['cc'] = core_cc(h)
        in_maps.append(m)
    return in_maps


def kernel(**inputs):
    in_maps = make_in_maps(inputs)
    if 'nc' not in _NC_CACHE:
        _NC_CACHE['nc'] = build_program()
    nc = _NC_CACHE['nc']
    res = run_bass_kernel_spmd(nc, in_maps, core_ids=list(range(8)))
    out = np.zeros((4, SEQ, D), np.float32)
    for c in range(8):
        b, h = c // 2, c % 2
        out[b, core_tokens(h), :] = np.asarray(res.results[c]['y'], dtype=np.float32)
    return out
```
